# Optimizing a Trainium2 kernel written in Bass

```python
import math
import jax
import jax.numpy as jnp
from jax import lax
import numpy as np

D_MODEL = 1024
BATCH = 4
SEQ = 8192
DEPTH = 2

NSA_HEADS = 8
NSA_GROUPS = 2
NSA_HPG = NSA_HEADS // NSA_GROUPS
HEAD_DIM = 64
NSA_WIDTH = NSA_HEADS * HEAD_DIM
NSA_KV = NSA_GROUPS * HEAD_DIM
CMP_STRIDE = 16
CMP_LEN = 2 * CMP_STRIDE
CMP_HIDDEN = 256
SEL_BLOCK = 64
SEL_TOPN = 16
WINDOW = 512
Q_BLOCK = 128
SEL_FORCE = 1e9

RWKV_HEADS = 8
RWKV_HEAD = 64
RWKV_WIDTH = RWKV_HEADS * RWKV_HEAD
LORA_W = 64
LORA_A = 64
LORA_V = 32
LORA_G = 128
GN_EPS = 64e-5

REL_BUCKETS = 32
REL_MAX_DIST = 1024

MEM_LEN = 256
XATTN_HEADS = 4
XATTN_HEAD = 128
XATTN_WIDTH = XATTN_HEADS * XATTN_HEAD

N_EXPERTS = 16
N_EXPERT_GROUPS = 4
EXPERTS_PER_GROUP = N_EXPERTS // N_EXPERT_GROUPS
TOP_K = 2
EXPERT_FF = 512
MOE_BLOCK = 128

DN_ALPHA = (2 * DEPTH) ** 0.25
DN_BETA = (8 * DEPTH) ** -0.25
LN_EPS = 1e-5
NEG_INF = -1e30

NSA_COLS = NSA_WIDTH + 6 * NSA_KV + 3 * NSA_HEADS
RWKV_COLS = 3 * RWKV_WIDTH + LORA_W + LORA_A + LORA_G
GATE_COLS = 2 * D_MODEL
IN_COLS = NSA_COLS + RWKV_COLS + GATE_COLS

kernel_name = 'hybrid_nsa_rwkv7_grouped_moe_deepnorm'


def _split(z, sizes):
    return jnp.split(z, np.cumsum(sizes)[:-1].tolist(), axis=-1)


def layer_norm(x, g, b):
    xf = x.astype(jnp.float32)
    mu = jnp.mean(xf, axis=-1, keepdims=True)
    var = jnp.mean(jnp.square(xf - mu), axis=-1, keepdims=True)
    return ((xf - mu) * lax.rsqrt(var + LN_EPS) * g + b).astype(x.dtype)


def masked_softmax(s, mask):
    s = jnp.where(mask, s.astype(jnp.float32), NEG_INF)
    m = jnp.max(s, axis=-1, keepdims=True)
    p = jnp.where(mask, jnp.exp(s - m), 0.0)
    return p / jnp.maximum(jnp.sum(p, axis=-1, keepdims=True), 1e-30)


def t5_bucket(dist):
    n = jnp.maximum(dist, 0)
    max_exact = REL_BUCKETS // 2
    log_ratio = jnp.log(jnp.maximum(n, 1).astype(jnp.float32) / max_exact) / math.log(REL_MAX_DIST / max_exact)
    large = jnp.minimum(max_exact + (log_ratio * (REL_BUCKETS - max_exact)).astype(jnp.int32), REL_BUCKETS - 1)
    return jnp.where(n < max_exact, n, large)


def token_shift_mix(z, mu):
    z_prev = jnp.pad(z[:, :-1], ((0, 0), (1, 0), (0, 0)))
    return z + mu * (z_prev - z)


def _compress(z, pe, w1, w2):
    B, T, G, dh = z.shape
    n_cmp = T // CMP_STRIDE - 1
    ch = z.reshape(B, T // CMP_STRIDE, CMP_STRIDE, G, dh)
    blocks = jnp.concatenate([ch[:, :-1], ch[:, 1:]], axis=2) + pe[:, None, :]
    flat = jnp.moveaxis(blocks, 3, 2).reshape(B, n_cmp, G, CMP_LEN * dh)
    return jax.nn.gelu(flat @ w1) @ w2


def _gather_blocks(blocks, idx):
    return jax.vmap(jax.vmap(lambda b, i: b[i]))(blocks, idx)


def nsa_attention(q, kc, vc, ks, vs, kw, vw, gate_logits, rel_table, pe_k, pe_v, w1k, w2k, w1v, w2v):
    B, T, _ = q.shape
    G, Hg, dh = NSA_GROUPS, NSA_HPG, HEAD_DIM
    scale = HEAD_DIM ** -0.5
    f32 = jnp.float32
    q = q.reshape(B, T, G, Hg, dh)
    kv = lambda z: z.reshape(B, T, G, dh)
    k_cmp = _compress(kv(kc), pe_k, w1k, w2k)
    v_cmp = _compress(kv(vc), pe_v, w1v, w2v)
    n_cmp = k_cmp.shape[1]
    n_sel = T // SEL_BLOCK
    n_top = min(SEL_TOPN, n_sel)
    k_blk = kv(ks).reshape(B, n_sel, SEL_BLOCK, G, dh).transpose(0, 3, 1, 2, 4)
    v_blk = kv(vs).reshape(B, n_sel, SEL_BLOCK, G, dh).transpose(0, 3, 1, 2, 4)
    pad = ((0, 0), (WINDOW, 0), (0, 0), (0, 0))
    k_win = jnp.pad(kv(kw), pad)
    v_win = jnp.pad(kv(vw), pad)
    gates = jax.nn.sigmoid(gate_logits.astype(f32)).reshape(B, T, G, Hg, 3)
    cmp_start = jnp.arange(n_cmp) * CMP_STRIDE
    cmp_end = cmp_start + CMP_LEN - 1
    sel_start = jnp.arange(n_sel) * SEL_BLOCK
    overlap = ((cmp_start[:, None] <= sel_start[None, :] + SEL_BLOCK - 1)
               & (cmp_end[:, None] >= sel_start[None, :])).astype(f32)
    tbl_g = rel_table.reshape(REL_BUCKETS, G, Hg).transpose(1, 0, 2).astype(f32)

    def bias_2d(dist):
        return rel_table[t5_bucket(dist)].reshape(dist.shape + (G, Hg)).transpose(2, 3, 0, 1).astype(f32)

    def block(c):
        t0 = c * Q_BLOCK
        tq = t0 + jnp.arange(Q_BLOCK)
        qc = lax.dynamic_slice_in_dim(q, t0, Q_BLOCK, axis=1)
        gc = lax.dynamic_slice_in_dim(gates, t0, Q_BLOCK, axis=1)
        mask_c = cmp_end[None, :] <= tq[:, None]
        s_c = jnp.einsum('bqghd,bngd->bghqn', qc, k_cmp).astype(f32) * scale + bias_2d(tq[:, None] - cmp_end[None, :])
        p_c = masked_softmax(s_c, mask_c)
        o_c = jnp.einsum('bghqn,bngd->bqghd', p_c, v_cmp.astype(f32))
        imp = jnp.einsum('bghqn,nj->bgqj', p_c, overlap)
        jq = tq // SEL_BLOCK
        jb = jnp.arange(n_sel)
        valid = jb[None, :] <= jq[:, None]
        forced = (jb[None, :] == 0) | (jb[None, :] == jq[:, None]) | (jb[None, :] == jq[:, None] - 1)
        imp = jnp.where(forced, SEL_FORCE, jnp.where(valid, imp, -SEL_FORCE))
        top_v, top_i = lax.top_k(imp, n_top)
        sel_ok = top_v >= 0.0
        k_sel = _gather_blocks(k_blk, top_i).reshape(B, G, Q_BLOCK, n_top * SEL_BLOCK, dh)
        v_sel = _gather_blocks(v_blk, top_i).reshape(B, G, Q_BLOCK, n_top * SEL_BLOCK, dh)
        kpos = (top_i[..., None] * SEL_BLOCK + jnp.arange(SEL_BLOCK)).reshape(B, G, Q_BLOCK, n_top * SEL_BLOCK)
        mask_s = (kpos <= tq[:, None]) & jnp.repeat(sel_ok, SEL_BLOCK, axis=-1)
        bias_s = jax.vmap(lambda t, b: t[b], in_axes=(0, 1), out_axes=1)(tbl_g, t5_bucket(tq[:, None] - kpos))
        bias_s = jnp.moveaxis(bias_s, -1, 2)
        s_s = jnp.einsum('bqghd,bgqkd->bghqk', qc, k_sel).astype(f32) * scale + bias_s
        p_s = masked_softmax(s_s, mask_s[:, :, None])
        o_s = jnp.einsum('bghqk,bgqkd->bqghd', p_s, v_sel.astype(f32))
        kwc = lax.dynamic_slice_in_dim(k_win, t0, Q_BLOCK + WINDOW, axis=1)
        vwc = lax.dynamic_slice_in_dim(v_win, t0, Q_BLOCK + WINDOW, axis=1)
        kpos_w = t0 - WINDOW + jnp.arange(Q_BLOCK + WINDOW)
        dist_w = tq[:, None] - kpos_w[None, :]
        mask_w = (dist_w >= 0) & (dist_w < WINDOW) & (kpos_w[None, :] >= 0)
        s_w = jnp.einsum('bqghd,bkgd->bghqk', qc, kwc).astype(f32) * scale + bias_2d(dist_w)
        p_w = masked_softmax(s_w, mask_w)
        o_w = jnp.einsum('bghqk,bkgd->bqghd', p_w, vwc.astype(f32))
        o = gc[..., 0:1] * o_c + gc[..., 1:2] * o_s + gc[..., 2:3] * o_w
        return o.reshape(B, Q_BLOCK, NSA_WIDTH).astype(q.dtype)

    out = lax.map(block, jnp.arange(T // Q_BLOCK))
    return jnp.moveaxis(out, 0, 1).reshape(B, T, NSA_WIDTH)


def rwkv7_time_mix(r, k, v, zw, za, zg, w0, w2, a0, a2, g2, k_k, k_a, r_k, gn_g, gn_b):
    B, T, C = r.shape
    H, N = RWKV_HEADS, RWKV_HEAD
    f32 = jnp.float32
    r, k, v = r.astype(f32), k.astype(f32), v.astype(f32)
    log_w = -jax.nn.softplus(-(w0 + jnp.tanh(zw) @ w2).astype(f32)) - 0.5
    decay = jnp.exp(-jnp.exp(log_w))
    a = jax.nn.sigmoid((a0 + za @ a2).astype(f32))
    g = (jax.nn.sigmoid(zg) @ g2).astype(f32)
    kk = (k * k_k).reshape(B, T, H, N)
    kk = kk / jnp.maximum(jnp.sqrt(jnp.sum(kk * kk, axis=-1, keepdims=True)), 1e-12)
    k = k * (1.0 + (a - 1.0) * k_a)
    heads = lambda t: jnp.moveaxis(t.reshape(B, T, H, N), 1, 0)

    def step(S, inp):
        r_t, w_t, k_t, v_t, kk_t, a_t = inp
        S = (S * w_t[:, :, None, :]
             - jnp.einsum('bhij,bhj->bhi', S, kk_t)[..., None] * (kk_t * a_t)[:, :, None, :]
             + v_t[..., None] * k_t[:, :, None, :])
        return S, jnp.einsum('bhij,bhj->bhi', S, r_t)

    S0 = jnp.zeros((B, H, N, N), f32)
    _, o = lax.scan(step, S0, (heads(r), heads(decay), heads(k), heads(v), jnp.moveaxis(kk, 1, 0), heads(a)))
    o = jnp.moveaxis(o, 0, 1)
    mu = jnp.mean(o, axis=-1, keepdims=True)
    var = jnp.mean(jnp.square(o - mu), axis=-1, keepdims=True)
    o = ((o - mu) * lax.rsqrt(var + GN_EPS)).reshape(B, T, C) * gn_g + gn_b
    bonus = jnp.sum((r * k).reshape(B, T, H, N) * r_k, axis=-1, keepdims=True) * v.reshape(B, T, H, N)
    return (o + bonus.reshape(B, T, C)) * g


def memory_cross_attention(x, mem, wq, wk, wv, wo):
    B, T, _ = x.shape
    M = mem.shape[1]
    q = (x @ wq).reshape(B, T, XATTN_HEADS, XATTN_HEAD)
    k = (mem @ wk).reshape(B, M, XATTN_HEADS, XATTN_HEAD)
    v = (mem @ wv).reshape(B, M, XATTN_HEADS, XATTN_HEAD)
    s = jnp.einsum('bqhd,bkhd->bhqk', q, k).astype(jnp.float32) * XATTN_HEAD ** -0.5
    p = jax.nn.softmax(s, axis=-1)
    o = jnp.einsum('bhqk,bkhd->bqhd', p, v.astype(jnp.float32)).astype(x.dtype)
    return o.reshape(B, T, XATTN_WIDTH) @ wo


def grouped_moe(x, router_w, router_bias, w_gate, w_up, w_down):
    B, T, D = x.shape
    n_tok = B * T
    xf = x.reshape(n_tok, D)
    s = jax.nn.sigmoid((xf @ router_w).astype(jnp.float32))
    s_sel = s + router_bias.astype(jnp.float32)
    group_score = jnp.sum(lax.top_k(s_sel.reshape(n_tok, N_EXPERT_GROUPS, EXPERTS_PER_GROUP), 2)[0], axis=-1)
    group = jnp.argmax(group_score, axis=-1)
    in_group = (jnp.arange(N_EXPERTS) // EXPERTS_PER_GROUP)[None, :] == group[:, None]
    _, e_idx = lax.top_k(jnp.where(in_group, s_sel, NEG_INF), TOP_K)
    s_top = jnp.take_along_axis(s, e_idx, axis=-1)
    gate = s_top / jnp.sum(s_top, axis=-1, keepdims=True)
    n_asg = n_tok * TOP_K
    e_flat = e_idx.reshape(-1)
    order = jnp.argsort(e_flat)
    e_sorted = e_flat[order]
    tok_sorted = order // TOP_K
    gate_sorted = gate.reshape(-1)[order]
    counts = jnp.bincount(e_flat, length=N_EXPERTS)
    padded = (counts + MOE_BLOCK - 1) // MOE_BLOCK * MOE_BLOCK
    dest = (jnp.cumsum(padded) - padded)[e_sorted] + jnp.arange(n_asg) - (jnp.cumsum(counts) - counts)[e_sorted]
    n_rows = n_asg + N_EXPERTS * MOE_BLOCK
    n_blocks = n_rows // MOE_BLOCK
    rows = jnp.zeros((n_rows, D), x.dtype).at[dest].set(xf[tok_sorted])
    block_expert = jnp.minimum(jnp.searchsorted(jnp.cumsum(padded), jnp.arange(n_blocks) * MOE_BLOCK, side='right'), N_EXPERTS - 1)

    def expert_block(args):
        xb, e = args
        return (jax.nn.silu(xb @ w_gate[e]) * (xb @ w_up[e])) @ w_down[e]

    y_rows = lax.map(expert_block, (rows.reshape(n_blocks, MOE_BLOCK, D), block_expert)).reshape(n_rows, D)
    y = jax.ops.segment_sum(y_rows[dest] * gate_sorted[:, None], tok_sorted, num_segments=n_tok)
    return y.astype(x.dtype).reshape(B, T, D)


def setup_inputs(seed: int = 0) -> dict:
    key = jax.random.key(seed)
    keys = iter(jax.random.split(key, 64))
    f32 = jnp.float32
    L, D = DEPTH, D_MODEL

    def nrm(shape, scale):
        return jax.random.normal(next(keys), shape, f32) * scale

    def gain(shape):
        return 1.0 + nrm(shape, 0.02)

    def unif(shape, lo, hi):
        return jax.random.uniform(next(keys), shape, f32, lo, hi)

    return {
        'x': nrm((BATCH, SEQ, D), 1.0),
        'mem': nrm((BATCH, MEM_LEN, D), 1.0),
        'rel_table': nrm((REL_BUCKETS, NSA_HEADS), 0.5),
        'router_w': nrm((D, N_EXPERTS), D ** -0.5),
        'router_bias': nrm((N_EXPERTS,), 0.01),
        'w_in': nrm((L, D, IN_COLS), D ** -0.5),
        'cmp_pe_k': nrm((L, CMP_LEN, HEAD_DIM), 0.1),
        'cmp_pe_v': nrm((L, CMP_LEN, HEAD_DIM), 0.1),
        'cmp_w1k': nrm((L, CMP_LEN * HEAD_DIM, CMP_HIDDEN), (CMP_LEN * HEAD_DIM) ** -0.5),
        'cmp_w2k': nrm((L, CMP_HIDDEN, HEAD_DIM), CMP_HIDDEN ** -0.5),
        'cmp_w1v': nrm((L, CMP_LEN * HEAD_DIM, CMP_HIDDEN), (CMP_LEN * HEAD_DIM) ** -0.5),
        'cmp_w2v': nrm((L, CMP_HIDDEN, HEAD_DIM), CMP_HIDDEN ** -0.5),
        'rwkv_mu': unif((L, RWKV_COLS), 0.0, 1.0),
        'rwkv_w0': unif((L, RWKV_WIDTH), -6.0, -1.0),
        'rwkv_w2': nrm((L, LORA_W, RWKV_WIDTH), 0.1),
        'rwkv_a0': nrm((L, RWKV_WIDTH), 0.1),
        'rwkv_a2': nrm((L, LORA_A, RWKV_WIDTH), LORA_A ** -0.5),
        'rwkv_g2': nrm((L, LORA_G, RWKV_WIDTH), LORA_G ** -0.5),
        'rwkv_kk': 0.85 + nrm((L, RWKV_WIDTH), 0.02),
        'rwkv_ka': gain((L, RWKV_WIDTH)),
        'rwkv_rk': nrm((L, RWKV_HEADS, RWKV_HEAD), 0.1),
        'rwkv_gn_g': gain((L, RWKV_WIDTH)),
        'rwkv_gn_b': nrm((L, RWKV_WIDTH), 0.02),
        'rwkv_v0': nrm((L - 1, RWKV_WIDTH), 0.1),
        'rwkv_v1': nrm((L - 1, RWKV_WIDTH, LORA_V), RWKV_WIDTH ** -0.5),
        'rwkv_v2': nrm((L - 1, LORA_V, RWKV_WIDTH), LORA_V ** -0.5),
        'p_nsa': nrm((L, NSA_WIDTH, D), NSA_WIDTH ** -0.5),
        'p_rwkv': nrm((L, RWKV_WIDTH, D), RWKV_WIDTH ** -0.5),
        'w_out': nrm((L, D, D), D ** -0.5 * DN_BETA),
        'ln1_g': gain((L, D)),
        'ln1_b': nrm((L, D), 0.02),
        'xq_w': nrm((L, D, XATTN_WIDTH), D ** -0.5),
        'xk_w': nrm((L, D, XATTN_WIDTH), D ** -0.5),
        'xv_w': nrm((L, D, XATTN_WIDTH), D ** -0.5 * DN_BETA),
        'xo_w': nrm((L, XATTN_WIDTH, D), XATTN_WIDTH ** -0.5 * DN_BETA),
        'ln2_g': gain((L, D)),
        'ln2_b': nrm((L, D), 0.02),
        'moe_w_gate': nrm((L, N_EXPERTS, D, EXPERT_FF), D ** -0.5),
        'moe_w_up': nrm((L, N_EXPERTS, D, EXPERT_FF), D ** -0.5),
        'moe_w_down': nrm((L, N_EXPERTS, EXPERT_FF, D), EXPERT_FF ** -0.5 * DN_BETA),
        'ln3_g': gain((L, D)),
        'ln3_b': nrm((L, D), 0.02),
    }


def reference(x, mem, rel_table, router_w, router_bias, w_in, cmp_pe_k, cmp_pe_v, cmp_w1k, cmp_w2k, cmp_w1v, cmp_w2v,
              rwkv_mu, rwkv_w0, rwkv_w2, rwkv_a0, rwkv_a2, rwkv_g2, rwkv_kk, rwkv_ka, rwkv_rk, rwkv_gn_g, rwkv_gn_b,
              rwkv_v0, rwkv_v1, rwkv_v2, p_nsa, p_rwkv, w_out, ln1_g, ln1_b, xq_w, xk_w, xv_w, xo_w, ln2_g, ln2_b,
              moe_w_gate, moe_w_up, moe_w_down, ln3_g, ln3_b):
    v_first = None
    for l in range(DEPTH):
        z_nsa, z_rwkv, z_gate = _split(x @ w_in[l], (NSA_COLS, RWKV_COLS, GATE_COLS))
        q, kc, vc, ks, vs, kw, vw, g_nsa = _split(z_nsa, (NSA_WIDTH,) + (NSA_KV,) * 6 + (3 * NSA_HEADS,))
        o_nsa = nsa_attention(q, kc, vc, ks, vs, kw, vw, g_nsa, rel_table, cmp_pe_k[l], cmp_pe_v[l],
                              cmp_w1k[l], cmp_w2k[l], cmp_w1v[l], cmp_w2v[l])
        r, k, v, zw, za, zg = _split(token_shift_mix(z_rwkv, rwkv_mu[l]), (RWKV_WIDTH,) * 3 + (LORA_W, LORA_A, LORA_G))
        if l == 0:
            v_first = v
        else:
            v = v + (v_first - v) * jax.nn.sigmoid(rwkv_v0[l - 1] + (v @ rwkv_v1[l - 1]) @ rwkv_v2[l - 1])
        o_rwkv = rwkv7_time_mix(r, k, v, zw, za, zg, rwkv_w0[l], rwkv_w2[l], rwkv_a0[l], rwkv_a2[l], rwkv_g2[l],
                                rwkv_kk[l], rwkv_ka[l], rwkv_rk[l], rwkv_gn_g[l], rwkv_gn_b[l]).astype(x.dtype)
        gate_nsa, gate_rwkv = _split(jax.nn.sigmoid(z_gate), (D_MODEL, D_MODEL))
        y = (gate_nsa * (o_nsa @ p_nsa[l]) + gate_rwkv * (o_rwkv @ p_rwkv[l])) @ w_out[l]
        x = layer_norm(DN_ALPHA * x + y, ln1_g[l], ln1_b[l])
        y = memory_cross_attention(x, mem, xq_w[l], xk_w[l], xv_w[l], xo_w[l])
        x = layer_norm(DN_ALPHA * x + y, ln2_g[l], ln2_b[l])
        y = grouped_moe(x, router_w, router_bias, moe_w_gate[l], moe_w_up[l], moe_w_down[l])
        x = layer_norm(DN_ALPHA * x + y, ln3_g[l], ln3_b[l])
    return x
```

```python
import numpy as np
from contextlib import ExitStack
import concourse.bass as bass
import concourse.mybir as mybir
from concourse.bass_utils import run_bass_kernel_spmd

F32 = mybir.dt.float32
BF16 = mybir.dt.bfloat16
AF = mybir.ActivationFunctionType
ALU = mybir.AluOpType
AX = mybir.AxisListType

ENGS = ("pe", "act", "dve", "pool", "sp")
NDS = 20


class Buf:
    def __init__(self, h, name):
        self.h = h
        self.name = name
        self.w = None
        self.r = {}

    def __getitem__(self, idx):
        return V(self, self.h[idx])

    def ap(self):
        return V(self, self.h[:])


class V:
    def __init__(self, buf, ap):
        self.buf = buf
        self.ap = ap

    def __getitem__(self, idx):
        return V(self.buf, self.ap[idx])

    def rearrange(self, *a, **k):
        return V(self.buf, self.ap.rearrange(*a, **k))

    def bitcast(self, dt):
        return V(self.buf, self.ap.bitcast(dt))

    def to_broadcast(self, shape):
        return V(self.buf, self.ap.to_broadcast(list(shape)))

    def unsqueeze(self, ax):
        return V(self.buf, self.ap.unsqueeze(ax))

    def partition_broadcast(self, n):
        return V(self.buf, self.ap.partition_broadcast(n))

    def bc(self, shape):
        ap = self.ap
        while len(ap.shape) < len(shape):
            ap = ap.unsqueeze(len(ap.shape))
        return V(self.buf, ap.to_broadcast(list(shape)))

    @property
    def shape(self):
        return self.ap.shape


WRITE_KW = ("out", "accum_out", "out_max", "out_indices")


class KB:
    def __init__(self, nc, es):
        self.nc = nc
        self.es = es
        self.q = {e: [] for e in ENGS}
        self.cnt = {e: 0 for e in ENGS}
        self.sem = {}
        for e in ENGS:
            if e == "sp":
                continue
            self.sem[e] = es.enter_context(nc.semaphore("s_" + e))
        self.dsem = {}
        self.dval = {}
        self.dn = {}
        for e in ("sp", "pool", "act"):
            self.dsem[e] = [es.enter_context(nc.semaphore(f"d_{e}{i}")) for i in range(NDS)]
            self.dval[e] = [0] * NDS
            self.dn[e] = 0
        self.seen = {e: {} for e in ENGS}
        self.semobj = {}
        self.nbuf = 0

    def sb(self, shape, dt=F32, name=None):
        self.nbuf += 1
        name = name or f"t{self.nbuf}"
        h = self.es.enter_context(self.nc.sbuf_tensor("sb_" + name, list(shape), dt))
        return Buf(h, name)

    def ps(self, shape, dt=F32, name=None):
        self.nbuf += 1
        name = name or f"p{self.nbuf}"
        h = self.es.enter_context(self.nc.psum_tensor(name, list(shape), dt))
        return Buf(h, name)

    def sub(self, view, name, bank=None):
        b = Buf(view.ap if isinstance(view, V) else view, name)
        b.bank = bank
        return b

    def banklock(self, name):
        return Buf(None, name)

    def dram(self, name, shape, dt=F32, kind="Internal"):
        h = self.nc.dram_tensor(name, list(shape), dt, kind=kind)
        return Buf(h.ap(), name)

    def _need(self, eng, tok, waits):
        if tok is None:
            return
        key, val, teng = tok
        if teng == eng and eng in ("pe", "sp"):
            return
        if self.seen[eng].get(key, 0) >= val:
            return
        waits[key] = max(waits.get(key, 0), val)

    def _deps(self, eng, reads, writes, same_ok=False):
        waits = {}
        for b in reads:
            self._need(eng, b.w, waits)
        for b in writes:
            self._need(eng, b.w, waits)
            for key, (val, teng) in b.r.items():
                if teng == eng:
                    continue
                self._need(eng, (key, val, teng), waits)
        for key, val in waits.items():
            self.seen[eng][key] = val
        return [(self.semobj[key], val) for key, val in waits.items()]

    def _mark(self, tok, reads, writes):
        key, val, eng = tok
        for b in reads:
            b.r[key] = (val, eng)
        for b in writes:
            b.w = tok
            b.r = {}

    def op(self, eng, meth, *args, **kw):
        reads, writes = [], []
        extra_r = kw.pop("_reads", [])
        extra_w = kw.pop("_writes", [])
        a2 = []
        for a in args:
            if isinstance(a, V):
                (writes if not a2 and not writes else reads).append(a.buf)
                a2.append(a.ap)
            else:
                a2.append(a)
        k2 = {}
        for k, v in kw.items():
            if isinstance(v, V):
                (writes if k in WRITE_KW else reads).append(v.buf)
                k2[k] = v.ap
            else:
                k2[k] = v
        reads += [b.buf if isinstance(b, V) else b for b in extra_r]
        writes += [b.buf if isinstance(b, V) else b for b in extra_w]
        waits = self._deps(eng, reads, writes)
        banks = []
        for b in reads + writes:
            bk = getattr(b, "bank", None)
            if bk is not None and bk not in banks:
                banks.append(bk)
        for bk in banks:
            if bk.w is not None and bk.w[2] != eng and self.seen[eng].get(bk.w[0], 0) < bk.w[1]:
                self.seen[eng][bk.w[0]] = bk.w[1]
                waits.append((self.semobj[bk.w[0]], bk.w[1]))
        self.cnt[eng] += 1
        n = self.cnt[eng]
        sem = self.sem[eng]
        key = "c_" + eng
        self.semobj[key] = sem
        self._mark((key, n, eng), reads, writes)
        for bk in banks:
            bk.w = (key, n, eng)

        def emit(e, meth=meth, a2=a2, k2=k2, waits=waits, sem=sem):
            for s, v in waits:
                e.wait_ge(s, v)
            getattr(e, meth)(*a2, **k2).then_inc(sem, 1)

        self.q[eng].append(emit)

    def pe(self, meth, *a, **k):
        self.op("pe", meth, *a, **k)

    def act(self, meth, *a, **k):
        self.op("act", meth, *a, **k)

    def dve(self, meth, *a, **k):
        self.op("dve", meth, *a, **k)

    def pool(self, meth, *a, **k):
        self.op("pool", meth, *a, **k)

    def dma(self, out, in_, q="sp", **kw):
        reads, writes = [in_.buf], [out.buf]
        i = self.dn[q] % NDS
        self.dn[q] += 1
        sem = self.dsem[q][i]
        key = f"d_{q}{i}"
        self.semobj[key] = sem
        waits = self._deps(q, reads, writes)
        prev = self.dval[q][i]
        if prev > 0 and self.seen[q].get(key, 0) < prev:
            waits.append((sem, prev))
            self.seen[q][key] = prev
        self.dval[q][i] = prev + 16
        val = prev + 16
        self._mark((key, val, "dma_" + q), reads, writes)
        oap, iap = out.ap, in_.ap

        def emit(e, waits=waits, sem=sem, oap=oap, iap=iap, kw=kw):
            for s, v in waits:
                e.wait_ge(s, v)
            e.dma_start(out=oap, in_=iap, **kw).then_inc(sem, 16)

        self.q[q].append(emit)

    def wait_all(self, eng, bufs):
        waits = {}
        for b in bufs:
            self._need(eng, b.w, waits)
        ws = [(self.semobj[k], v) for k, v in waits.items()]

        def emit(e, ws=ws):
            for s, v in ws:
                e.wait_ge(s, v)

        self.q[eng].append(emit)

    def finish(self):
        nc = self.nc
        with nc.Block() as block:
            @block.tensor
            def _(e):
                for f in self.q["pe"]:
                    f(e)

            @block.scalar
            def _(e):
                for f in self.q["act"]:
                    f(e)

            @block.vector
            def _(e):
                for f in self.q["dve"]:
                    f(e)

            @block.gpsimd
            def _(e):
                for f in self.q["pool"]:
                    f(e)

            @block.sync
            def _(e):
                for f in self.q["sp"]:
                    f(e)


K1_NT = 4096
K1_NCOL = 5144
K1_NCH = (K1_NCOL + 127)//128

def build_k1():
    nc = bass.Bass("TRN2", target_bir_lowering=False)
    es = ExitStack()
    with es:
        kb = KB(nc, es)
        xT = kb.dram("xT", [1024, K1_NT], F32, kind="ExternalInput")
        w = kb.dram("w", [1024, K1_NCOL], F32, kind="ExternalInput")
        zT = kb.dram("zT", [K1_NCH*128, K1_NT], F32, kind="ExternalOutput")
        xb = kb.sb([128, 8, K1_NT], BF16, "xb")
        wb = kb.sb([128, 8, K1_NCH*128], BF16, "wb")
        stg = [kb.sb([128, 2048], F32, f"stg{i}") for i in range(3)]
        si = 0
        ceng = ["dve", "pool", "act"]
        if K1_NCOL < K1_NCH*128: kb.dve("memset", wb[:, :, K1_NCOL:K1_NCH*128], 0.0)
        for k in range(8):
            for c0 in range(0, K1_NT, 2048):
                s = stg[si % 3];
                kb.dma(s[:, :], xT[k*128:(k+1)*128, c0:c0+2048])
                e = ceng[si % 3]; si += 1
                if e == "act":
                    kb.act("copy", out=xb[:, k, c0:c0+2048], in_=s[:, :])
                else:
                    kb.op(e, "tensor_copy", out=xb[:, k, c0:c0+2048], in_=s[:, :])
            for c0 in range(0, K1_NCOL, 2048):
                cw = min(2048, K1_NCOL - c0)
                s = stg[si % 3]
                kb.dma(s[:, :cw], w[k*128:(k+1)*128, c0:c0+cw])
                e = ceng[si % 3]; si += 1
                if e == "act":
                    kb.act("copy", out=wb[:, k, c0:c0+cw], in_=s[:, :cw])
                else:
                    kb.op(e, "tensor_copy", out=wb[:, k, c0:c0+cw], in_=s[:, :cw])
        pss = [kb.ps([128, 512], F32, f"ps{i}") for i in range(4)]
        outs = [kb.sb([128, 512], F32, f"ob{i}") for i in range(4)]
        it = 0
        for ct in range(K1_NCH):
            for tt in range(K1_NT//512):
                ps = pss[it % 4]; ob = outs[it % 4]
                for k in range(8):
                    kb.pe("matmul", ps[:, :], lhsT=wb[:, k, ct*128:(ct+1)*128], rhs=xb[:, k, tt*512:(tt+1)*512], start=(k == 0), stop=(k == 7))
                if it % 2 == 0:
                    kb.act("copy", out=ob[:, :], in_=ps[:, :])
                else:
                    kb.dve("tensor_copy", out=ob[:, :], in_=ps[:, :])
                kb.dma(zT[ct*128:(ct+1)*128, tt*512:(tt+1)*512], ob[:, :], q="pool")
                it += 1
        kb.wait_all("pool", [zT])
        kb.finish()
    return nc


NEG = -30000.0
NCLS_C, NCLS_W, NCLS_S = 24, 5, 9
NCLS = NCLS_C + NCLS_W + NCLS_S


def t5_bucket_np(dist):
    n = np.maximum(dist, 0)
    max_exact = 16
    lr = np.log(np.maximum(n, 1).astype(np.float32) / np.float32(max_exact)) / np.float32(np.log(1024 / 16))
    large = np.minimum(max_exact + (lr.astype(np.float32) * np.float32(16)).astype(np.int32), 31)
    return np.where(n < max_exact, n, large)


def nsa_consts(T, rel_table, g):
    NJ = T // 64
    NKC = T // 128
    NCc = (T // 16 + 127) // 128
    tbl = rel_table.reshape(32, 2, 4)[:, g, :]
    ni = np.arange(128)[:, None]
    qi = np.arange(128)[None, :]
    strips = np.zeros((4, 128, NCLS, 128), np.float32)
    for m in range(NCLS_C):
        dist = 128 * m + qi - 16 * ni - 31
        b = t5_bucket_np(dist)
        for h in range(4):
            strips[h, :, m, :] = np.where(dist >= 0, tbl[b, h], np.float32(NEG))
    for m in range(NCLS_W):
        dist = 128 * m + qi - ni
        b = t5_bucket_np(dist)
        for h in range(4):
            strips[h, :, NCLS_C + m, :] = np.where((dist >= 0) & (dist < 512), tbl[b, h], np.float32(NEG))
    for m in range(NCLS_S):
        dist = 128 * m + qi - ni
        b = t5_bucket_np(dist)
        for h in range(4):
            strips[h, :, NCLS_C + NCLS_W + m, :] = np.where(dist >= 0, tbl[b, h], np.float32(NEG))
    n = np.arange(NCc * 128)
    cs = n * 16
    ce = cs + 31
    ss = np.arange(NJ) * 64
    ov = ((cs[:, None] <= ss[None, :] + 63) & (ce[:, None] >= ss[None, :])).astype(np.float32)
    ov[n >= T // 16 - 1] = 0.0
    jr = np.arange(2 * NJ)[None, :] - NJ
    hi = (np.arange(128)[:, None] >= 64).astype(np.int64)
    valid = jr <= hi
    f1 = jr == hi
    f2 = jr == hi - 1
    vm = (valid & ~f1 & ~f2).astype(np.float32)
    va = np.where(f1, 2e9, np.where(f2, 3e9, np.where(valid, 0.0, -1e9))).astype(np.float32)
    es = np.zeros((NJ, NKC, 128), np.float32)
    for kc in range(NKC):
        es[kc, kc, 0:64] = 1.0
        es[NJ // 2 + kc, kc, 64:128] = 1.0
    return {
        "strips": np.ascontiguousarray(strips.reshape(4, 128, NCLS * 128)),
        "ov": np.ascontiguousarray(ov.reshape(NCc, 128, NJ)),
        "vmva": np.ascontiguousarray(np.concatenate([vm, va], 1)),
        "esel": np.ascontiguousarray(es.reshape(NJ, NKC * 128)),
        "ident": np.eye(128, dtype=np.float32),
    }


def build_k2a(T, stage=9, nq=None, cs=99):
    NJ = T // 64
    NKC = T // 128
    NQ = T // 128
    NCc = (T // 16 + 127) // 128
    NCP = NCc * 128
    VW = 65 + NJ
    nc = bass.Bass("TRN2", target_bir_lowering=False)
    es = ExitStack()
    with es:
        kb = KB(nc, es)
        EI = "ExternalInput"
        qTd = kb.dram("qT", [256, T], F32, kind=EI)
        kcBd = kb.dram("kcB", [2048, NCP], F32, kind=EI)
        vcBd = kb.dram("vcB", [2048, NCP], F32, kind=EI)
        ksTd = kb.dram("ksT", [64, T], F32, kind=EI)
        kwTd = kb.dram("kwT", [64, T], F32, kind=EI)
        vsd = kb.dram("vs", [T, 64], F32, kind=EI)
        vwd = kb.dram("vw", [T, 64], F32, kind=EI)
        gld = kb.dram("gl", [T, 12], F32, kind=EI)
        w1kd = kb.dram("w1k", [2048, 256], F32, kind=EI)
        w1vd = kb.dram("w1v", [2048, 256], F32, kind=EI)
        w2kd = kb.dram("w2k", [256, 64], F32, kind=EI)
        w2vd = kb.dram("w2v", [256, 64], F32, kind=EI)
        pekd = kb.dram("pek", [2048], F32, kind=EI)
        pevd = kb.dram("pev", [2048], F32, kind=EI)
        strd = kb.dram("strips", [4, 128, NCLS * 128], F32, kind=EI)
        ovd = kb.dram("ov", [NCc, 128, NJ], F32, kind=EI)
        vmvad = kb.dram("vmva", [128, 4 * NJ], F32, kind=EI)
        eseld = kb.dram("esel", [NJ, NKC * 128], F32, kind=EI)
        identd = kb.dram("ident", [128, 128], F32, kind=EI)
        oout = kb.dram("o", [T, 256], F32, kind="ExternalOutput")

        LK = [kb.banklock(f"lk{i}") for i in range(8)]
        bST = [es.enter_context(nc.psum_tensor(f"bst{i}", [128, 512], F32)) for i in range(3)]
        bC = es.enter_context(nc.psum_tensor("bC", [128, 512], F32))
        bW = es.enter_context(nc.psum_tensor("bW", [128, 512], F32))
        bS = es.enter_context(nc.psum_tensor("bS", [128, 512], F32))
        bT = es.enter_context(nc.psum_tensor("bT", [128, 1024], BF16))
        bM = es.enter_context(nc.psum_tensor("bM", [128, 512], F32))
        ST = [kb.sub(bST[i][:, 0:128], f"ST{i}", LK[i]) for i in range(3)]
        hidp = [kb.sub(bST[i][:, :], f"hid{i}", LK[i]) for i in range(2)]
        accC = [kb.sub(bC[:, i * 256:i * 256 + VW], f"accC{i}", LK[3]) for i in range(2)]
        accW = [kb.sub(bW[:, h * 65:(h + 1) * 65], f"accW{h}", LK[4]) for h in range(4)]
        accS = [kb.sub(bS[:, h * 65:(h + 1) * 65], f"accS{h}", LK[5]) for h in range(4)]
        tpp = kb.sub(bT[:, 0:128], "tpp", LK[6])
        misc = kb.sub(bM[:, :], "misc", LK[7])

        stg = [kb.sb([128, 2048], F32, f"stg{i}") for i in range(2)]
        sti = [0]

        def load_cast(dst, src, np_=128, w=None, eng="dve"):
            w = w or dst.shape[-1]
            s = stg[sti[0] % 2]; sti[0] += 1
            kb.dma(s[0:np_, 0:w], src)
            if eng == "act":
                kb.act("copy", out=dst, in_=s[0:np_, 0:w])
            else:
                kb.op(eng, "tensor_copy", out=dst, in_=s[0:np_, 0:w])

        identb = kb.sb([128, 128], BF16, "identb")
        load_cast(identb[:, :], identd[:, :])
        strips = kb.sb([128, 4, NCLS * 128], BF16, "strips")
        for h in range(4 if cs >= 1 else 0):
            for c0 in range(0, NCLS * 128, 2048):
                w = min(2048, NCLS * 128 - c0)
                load_cast(strips[:, h, c0:c0 + w], strd[h, :, c0:c0 + w], eng=("dve" if (c0 // 2048) % 2 == 0 else "pool"))
        esel = kb.sb([NJ, NKC * 128], BF16, "esel")
        for c0 in range(0, NKC * 128 if cs >= 2 else 0, 2048):
            w = min(2048, NKC * 128 - c0)
            load_cast(esel[:, c0:c0 + w], eseld[:, c0:c0 + w], np_=NJ)
        vmva = kb.sb([128, 4 * NJ], F32, "vmva")
        if cs >= 3: kb.dma(vmva[:, :], vmvad[:, :])
        ksT = kb.sb([64, T], BF16, "ksT"); kwT = kb.sb([64, T], BF16, "kwT")
        for c0 in range(0, T if cs >= 4 else 0, 2048):
            load_cast(ksT[:, c0:c0 + 2048], ksTd[:, c0:c0 + 2048], np_=64, eng="pool")
            load_cast(kwT[:, c0:c0 + 2048], kwTd[:, c0:c0 + 2048], np_=64, eng="dve")
        VsA = kb.sb([128, NKC, 65], BF16, "VsA"); VwA = kb.sb([128, NKC, 65], BF16, "VwA")
        if cs >= 5:
            kb.dve("memset", VsA[:, :, 64:65], 1.0)
            kb.dve("memset", VwA[:, :, 64:65], 1.0)
        for (dst, srcd) in (((VsA, vsd), (VwA, vwd)) if cs >= 6 else ()):
            src = srcd.ap().rearrange("(c p) d -> p c d", p=128)
            for c0 in range(0, NKC, 32):
                cw = min(32, NKC - c0)
                s = stg[sti[0] % 2]; sti[0] += 1
                sv = s[:, 0:cw * 64].rearrange("p (c d) -> p c d", d=64)
                kb.dma(sv, src[:, c0:c0 + cw, :])
                kb.dve("tensor_copy", out=dst[:, c0:c0 + cw, 0:64], in_=sv)
        VcA = kb.sb([128, NCc, VW], BF16, "VcA")
        if cs >= 7: kb.dve("memset", VcA[:, :, 64:65], 1.0)
        for c in range(NCc if cs >= 8 else 0):
            load_cast(VcA[:, c, 65:VW], ovd[c, :, :], w=NJ)
        kcT = kb.sb([64, NCP], BF16, "kcT")

        w1b = kb.sb([128, 16, 256], BF16, "w1b")
        w2b = kb.sb([128, 2, 64], BF16, "w2b")
        peb = kb.sb([128, 16], BF16, "peb")
        pbias = kb.sb([128, 2], F32, "pbias")
        xin = kb.sb([128, 512], BF16, "xin")
        hsb = kb.sb([128, 2, 512], F32, "hsb")
        tg = kb.sb([128, 512], F32, "tg")
        gT = kb.sb([128, 2, 512], BF16, "gT")
        for which in range(2 if stage >= 1 else 0):
            Bd, w1d, w2d, ped = ((kcBd, w1kd, w2kd, pekd), (vcBd, w1vd, w2vd, pevd))[which]
            w1v_ = w1d.ap().rearrange("(c p) h -> p c h", p=128)
            for c0 in range(0, 16, 8):
                s = stg[sti[0] % 2]; sti[0] += 1
                sv = s[:, 0:2048].rearrange("p (c h) -> p c h", h=256)
                kb.dma(sv, w1v_[:, c0:c0 + 8, :])
                kb.dve("tensor_copy", out=w1b[:, c0:c0 + 8, :], in_=sv)
            s = stg[sti[0] % 2]; sti[0] += 1
            sv = s[:, 0:128].rearrange("p (c d) -> p c d", d=64)
            kb.dma(sv, w2d.ap().rearrange("(c p) d -> p c d", p=128))
            kb.dve("tensor_copy", out=w2b[:, :, :], in_=sv)
            s = stg[sti[0] % 2]; sti[0] += 1
            kb.dma(s[:, 0:16], ped.ap().rearrange("(c p) -> p c", p=128), allow_slow_non_contiguous=True)
            kb.dve("tensor_copy", out=peb[:, :], in_=s[:, 0:16])
            for hc in range(2):
                for c in range(16):
                    kb.pe("matmul", misc[:, 0:1], lhsT=w1b[:, c, hc * 128:(hc + 1) * 128], rhs=peb[:, c:c + 1], start=(c == 0), stop=(c == 15))
                kb.dve("tensor_copy", out=pbias[:, hc:hc + 1], in_=misc[:, 0:1])
            for nb in range(0, NCP, 512):
                nw = min(512, NCP - nb)
                for c in range(16):
                    load_cast(xin[:, 0:nw], Bd[c * 128:(c + 1) * 128, nb:nb + nw], w=nw, eng=("dve" if c % 2 else "pool"))
                    for hc in range(2):
                        kb.pe("matmul", hidp[hc][:, 0:nw], lhsT=w1b[:, c, hc * 128:(hc + 1) * 128], rhs=xin[:, 0:nw], start=(c == 0), stop=(c == 15))
                for hc in range(2):
                    hv = hsb[:, hc, 0:nw]
                    kb.act("activation", out=hv, in_=hidp[hc][:, 0:nw], func=AF.Identity, bias=pbias[:, hc:hc + 1], scale=1.0)
                    kb.dve("tensor_tensor", out=tg[:, 0:nw], in0=hv, in1=hv, op=ALU.mult)
                    kb.dve("tensor_scalar", out=tg[:, 0:nw], in0=tg[:, 0:nw], scalar1=0.044715, scalar2=1.0, op0=ALU.mult, op1=ALU.add)
                    kb.dve("tensor_tensor", out=tg[:, 0:nw], in0=tg[:, 0:nw], in1=hv, op=ALU.mult)
                    kb.act("activation", out=tg[:, 0:nw], in_=tg[:, 0:nw], func=AF.Sigmoid, scale=1.5957691216057308)
                    kb.dve("tensor_tensor", out=gT[:, hc, 0:nw], in0=tg[:, 0:nw], in1=hv, op=ALU.mult)
                if which == 0:
                    for hc in range(2):
                        kb.pe("matmul", misc[0:64, 0:nw], lhsT=w2b[:, hc, :], rhs=gT[:, hc, 0:nw], start=(hc == 0), stop=(hc == 1))
                    kb.act("copy", out=kcT[:, nb:nb + nw], in_=misc[0:64, 0:nw])
                else:
                    for cc in range(nw // 128):
                        for hc in range(2):
                            kb.pe("matmul", misc[:, 0:64], lhsT=gT[:, hc, cc * 128:(cc + 1) * 128], rhs=w2b[:, hc, :], start=(hc == 0), stop=(hc == 1))
                        kb.act("copy", out=VcA[:, nb // 128 + cc, 0:64], in_=misc[:, 0:64])

        qf = [kb.sb([64, 4, 128], F32, f"qf{i}") for i in range(2)]
        qb = [kb.sb([64, 4, 128], BF16, f"qb{i}") for i in range(2)]
        glt = [kb.sb([128, 12], F32, f"glt{i}") for i in range(2)]
        gs = kb.sb([128, 4, 3], F32, "gs")
        PT = [kb.sb([128, 128], BF16, f"PT{i}") for i in range(6)]
        rd = kb.sb([128, 12], F32, "rd")
        cf = kb.sb([128, 12], F32, "cf")
        ocs = kb.sb([128, 4, 64], F32, "ocs")
        imp = kb.sb([128, NJ], F32, "imp")
        imp2 = kb.sb([128, NJ], F32, "imp2")
        impw = kb.sb([128, NJ], F32, "impw")
        mx = kb.sb([128, 16], F32, "mx")
        c1 = kb.sb([128, NJ], F32, "c1")
        negp = kb.sb([128, NJ], BF16, "negp")
        negT = kb.sb([NJ, 128], BF16, "negT")
        OUTt = [kb.sb([128, 4, 64], F32, f"OUTt{i}") for i in range(2)]
        qsrc = qTd.ap().rearrange("(h d) t -> d h t", h=4)
        cnt = {"st": 0, "pt": 0}

        def attend(h, qbt, kT_chunk, strip_cls, acc, vaug, first, last, extra=None):
            st = ST[cnt["st"] % 3]; cnt["st"] += 1
            pt = PT[cnt["pt"] % 6]; cnt["pt"] += 1
            kb.pe("matmul", st[:, :], lhsT=kT_chunk, rhs=qbt[:, h, :], start=True, stop=False)
            if extra is not None:
                kb.pe("matmul", st[:, :], lhsT=extra, rhs=negT[:, :], start=False, stop=False)
            kb.pe("matmul", st[:, :], lhsT=identb[:, :], rhs=strips[:, h, strip_cls * 128:(strip_cls + 1) * 128], start=False, stop=True)
            kb.act("activation", out=pt[:, :], in_=st[:, :], func=AF.Exp)
            kb.pe("matmul", acc, lhsT=pt[:, :], rhs=vaug, start=first, stop=last)

        for qt in range((NQ if nq is None else nq) if stage >= 2 else 0):
            q0 = qt * 128
            i = qt % 2
            kb.dma(qf[i][:, :, :], qsrc[:, :, q0:q0 + 128])
            kb.act("mul", out=qb[i][:, :, :], in_=qf[i][:, :, :], mul=0.125)
            kb.dma(glt[i][:, :], gld[q0:q0 + 128, :])
            kb.act("activation", out=gs[:, :, :].rearrange("p h c -> p (h c)"), in_=glt[i][:, :], func=AF.Sigmoid)
            ncj = min(NCc - 1, (q0 + 96) // 16 // 128) + 1
            for h in range(4):
                acc = accC[h % 2]
                for cj in range(ncj):
                    m = min(qt - 16 * cj, NCLS_C - 1)
                    attend(h, qb[i], kcT[:, cj * 128:(cj + 1) * 128], m, acc[:, :], VcA[:, cj, :], cj == 0, cj == ncj - 1)
                if stage < 3: continue
                kb.dve("tensor_scalar", out=rd[:, 3 * h:3 * h + 1], in0=acc[:, 64:65], scalar1=1e-30, scalar2=None, op0=ALU.max)
                kb.dve("reciprocal", out=rd[:, 3 * h:3 * h + 1], in_=rd[:, 3 * h:3 * h + 1])
                kb.act("copy", out=ocs[:, h, :], in_=acc[:, 0:64])
                if h == 0:
                    kb.dve("tensor_scalar", out=imp[:, :], in0=acc[:, 65:VW], scalar1=rd[:, 0:1], scalar2=None, op0=ALU.mult)
                else:
                    kb.dve("scalar_tensor_tensor", out=imp[:, :], in0=acc[:, 65:VW], scalar=rd[:, 3 * h:3 * h + 1], in1=imp[:, :], op0=ALU.mult, op1=ALU.add)
            if stage < 4: continue
            j0 = NJ - 2 * qt
            kb.dve("tensor_tensor", out=imp2[:, :], in0=imp[:, :], in1=vmva[:, j0:j0 + NJ], op=ALU.mult)
            kb.dve("tensor_tensor", out=imp2[:, :], in0=imp2[:, :], in1=vmva[:, 2 * NJ + j0:2 * NJ + j0 + NJ], op=ALU.add)
            kb.dve("memset", imp2[:, 0:1], 1e9)
            kb.dve("max", out=mx[:, 0:8], in_=imp2[:, :])
            kb.dve("match_replace", out=impw[:, :], in_to_replace=mx[:, 0:8], in_values=imp2[:, :], imm_value=-3e9)
            kb.dve("max", out=mx[:, 8:16], in_=impw[:, :])
            kb.dve("tensor_scalar", out=c1[:, :], in0=imp2[:, :], scalar1=mx[:, 15:16], scalar2=None, op0=ALU.is_ge)
            kb.dve("tensor_scalar", out=impw[:, :], in0=imp2[:, :], scalar1=0.0, scalar2=None, op0=ALU.is_ge)
            kb.dve("tensor_tensor", out=c1[:, :], in0=c1[:, :], in1=impw[:, :], op=ALU.mult)
            if stage < 5: continue
            kb.dve("tensor_scalar", out=negp[:, :].rearrange("q (jj c) -> q c jj", jj=2), in0=c1[:, :].rearrange("q (c jj) -> q c jj", jj=2),
                   scalar1=-1.0, scalar2=-NEG, op0=ALU.add, op1=ALU.mult)
            kb.pe("transpose", out=tpp[0:NJ, :], in_=negp[:, :], identity=identb[:, :])
            kb.act("copy", out=negT[:, :], in_=tpp[0:NJ, :])
            if stage < 6: continue
            for h in range(4):
                kcs = [kc for kc in range(qt - 4, qt + 1) if kc >= 0]
                for n_, kc in enumerate(kcs):
                    attend(h, qb[i], kwT[:, kc * 128:(kc + 1) * 128], NCLS_C + (qt - kc), accW[h][:, :], VwA[:, kc, :], n_ == 0, n_ == len(kcs) - 1)
                for kc in range(qt + 1 if stage >= 7 else 0):
                    ms = min(qt - kc, NCLS_S - 1)
                    attend(h, qb[i], ksT[:, kc * 128:(kc + 1) * 128], NCLS_C + NCLS_W + ms, accS[h][:, :], VsA[:, kc, :], kc == 0, kc == qt,
                           extra=esel[:, kc * 128:(kc + 1) * 128])
            if stage < 8: continue
            for h in range(4):
                kb.dve("reciprocal", out=rd[:, 3 * h + 1:3 * h + 2], in_=accS[h][:, 64:65])
                kb.dve("reciprocal", out=rd[:, 3 * h + 2:3 * h + 3], in_=accW[h][:, 64:65])
            kb.dve("tensor_tensor", out=cf[:, :], in0=rd[:, :], in1=gs[:, :, :].rearrange("p h c -> p (h c)"), op=ALU.mult)
            ot = OUTt[i]
            for h in range(4):
                kb.dve("tensor_scalar", out=ot[:, h, :], in0=ocs[:, h, :], scalar1=cf[:, 3 * h:3 * h + 1], scalar2=None, op0=ALU.mult)
                kb.dve("scalar_tensor_tensor", out=ot[:, h, :], in0=accS[h][:, 0:64], scalar=cf[:, 3 * h + 1:3 * h + 2], in1=ot[:, h, :], op0=ALU.mult, op1=ALU.add)
                kb.dve("scalar_tensor_tensor", out=ot[:, h, :], in0=accW[h][:, 0:64], scalar=cf[:, 3 * h + 2:3 * h + 3], in1=ot[:, h, :], op0=ALU.mult, op1=ALU.add)
            kb.dma(oout[q0:q0 + 128, :], ot[:, :, :].rearrange("p h c -> p (h c)"), q="pool")
        kb.wait_all("pool", [oout])
        kb.finish()
    return nc


GN_EPS = 64e-5
TS = 256
NCH = TS // 64
HN = 4 * NCH
NCM = 512 + 512 + 256 + 256 + 4 * TS + 64


def rwkv_consts():
    su = np.triu(np.ones((64, 64), np.float32), 1)
    ui = np.triu(np.ones((64, 64), np.float32), 0)
    sl = np.tril(np.ones((64, 64), np.float32), -1)
    ident = np.eye(64, dtype=np.float32)
    mb = np.tile(np.concatenate([su, -ui], 1), (1, 4))
    mk = np.tile(np.concatenate([su, ui], 1), (1, 4))
    ml = np.tile(sl, (1, 4))
    i4 = np.tile(ident, (1, 4))
    rm = np.ones((64, 4 * TS), np.float32)
    rm[:, ::64] = 0.0
    ones = np.ones((64, 64), np.float32)
    return np.ascontiguousarray(np.concatenate([mb, mk, ml, i4, rm, ones], 1))


def build_k2b(T, layer1=False):
    ntile = T // TS
    nc = bass.Bass("TRN2", target_bir_lowering=False)
    es = ExitStack()
    with es:
        kb = KB(nc, es)
        EI = "ExternalInput"
        zr = kb.dram("zr", [256, T], F32, kind=EI)
        zk = kb.dram("zk", [256, T], F32, kind=EI)
        zv = kb.dram("zv", [256, T], F32, kind=EI)
        zw = kb.dram("zw", [64, T], F32, kind=EI)
        za = kb.dram("za", [64, T], F32, kind=EI)
        zg = kb.dram("zg", [128, T], F32, kind=EI)
        prd = kb.dram("pr", [64, 34], F32, kind=EI)
        mugd = kb.dram("mug", [128, 1], F32, kind=EI)
        w2d = kb.dram("w2", [64, 256], F32, kind=EI)
        a2d = kb.dram("a2", [64, 256], F32, kind=EI)
        g2d = kb.dram("g2", [128, 256], F32, kind=EI)
        gnd = kb.dram("gn", [2, 256], F32, kind=EI)
        cmd = kb.dram("cm", [64, NCM], F32, kind=EI)
        if layer1:
            zvad = kb.dram("zva", [512, T], F32, kind=EI)
            vfd = kb.dram("vfirst", [256, T], F32, kind=EI)
            v1d = kb.dram("v1", [512, 32], F32, kind=EI)
            v2d = kb.dram("v2", [32, 256], F32, kind=EI)
            pvd = kb.dram("pv", [64, 12], F32, kind=EI)
        oout = kb.dram("o", [T, 256], F32, kind="ExternalOutput")
        vout = kb.dram("vs", [256, T], F32, kind="ExternalOutput")

        cm = kb.sb([64, NCM], F32, "cm")
        kb.dma(cm[:, :], cmd[:, :])
        Mb = cm[:, 0:512].rearrange("p (h c) -> p h c", h=4)
        Mk = cm[:, 512:1024].rearrange("p (h c) -> p h c", h=4)
        Ml = cm[:, 1024:1280].rearrange("p (h c) -> p h c", h=4)
        I4 = cm[:, 1280:1536].rearrange("p (h c) -> p h c", h=4)
        rmask = cm[:, 1536:1536 + 4 * TS]
        ones = cm[:, 1536 + 4 * TS:1600 + 4 * TS]
        identb = kb.sb([64, 64], BF16, "identb")
        kb.dve("tensor_copy", out=identb[:, :], in_=cm[:, 1280:1344])
        pr = kb.sb([64, 34], F32, "pr")
        kb.dma(pr[:, :], prd[:, :])
        P = lambda i: pr[:, 4 * i:4 * i + 4]
        mu_r, mu_k, mu_v, w0, a0, k_k, k_a, r_k = [P(i) for i in range(8)]
        mug = kb.sb([128, 1], F32, "mug")
        kb.dma(mug[:, :], mugd[:, :])
        omka = kb.sb([64, 4], F32, "omka")
        kb.dve("tensor_scalar", out=omka[:, :], in0=k_a, scalar1=-1.0, scalar2=1.0, op0=ALU.mult, op1=ALU.add)
        stg = kb.sb([128, 256], F32, "wstg")
        w2b = kb.sb([64, 256], BF16, "w2b")
        a2b = kb.sb([64, 256], BF16, "a2b")
        g2b = kb.sb([128, 256], BF16, "g2b")
        kb.dma(stg[0:64, :], w2d[:, :]); kb.dve("tensor_copy", out=w2b[:, :], in_=stg[0:64, :])
        kb.dma(stg[0:64, :], a2d[:, :]); kb.dve("tensor_copy", out=a2b[:, :], in_=stg[0:64, :])
        kb.dma(stg[:, :], g2d[:, :]); kb.dve("tensor_copy", out=g2b[:, :], in_=stg[:, :])
        gng = kb.sb([64, 256], F32, "gng")
        gnb = kb.sb([64, 256], F32, "gnb")
        kb.dma(gng[:, :], gnd[0:1, :].partition_broadcast(64))
        kb.dma(gnb[:, :], gnd[1:2, :].partition_broadcast(64))

        if layer1:
            pv = kb.sb([64, 12], F32, "pv"); kb.dma(pv[:, :], pvd[:, :])
            v1b = kb.sb([64, 8, 32], BF16, "v1b")
            kb.dma(stg[0:64, :].rearrange("p (h l) -> p h l", l=32), v1d.ap().rearrange("(h j) l -> j h l", h=8))
            kb.dve("tensor_copy", out=v1b[:, :, :], in_=stg[0:64, :].rearrange("p (h l) -> p h l", l=32))
            v2b = kb.sb([32, 256], BF16, "v2b")
            kb.dma(stg[0:32, :], v2d[:, :]); kb.dve("tensor_copy", out=v2b[:, :], in_=stg[0:32, :])
            VA0 = kb.sb([64, 8, TS + 1], F32, "VA0"); vsa = kb.sb([64, 8, TS], F32, "vsa"); vsab = kb.sb([64, 8, TS], BF16, "vsab")
            vf = kb.sb([64, 4, TS], F32, "vf"); latb = kb.sb([32, TS], BF16, "latb"); sgv = kb.sb([64, 4, TS], F32, "sgv")
        bk = [es.enter_context(nc.psum_tensor(f"bank{i}", [128, 512], F32)) for i in (0, 2, 3, 4, 5, 6, 7)]
        bkT = es.enter_context(nc.psum_tensor("bankT", [128, 1024], BF16))
        LK = [kb.banklock(f"lk{i}") for i in range(8)]
        sc = kb.sub(bk[0][0:64, 0:TS], "sc", LK[0])
        pB = kb.sub(bk[1][0:64, :].rearrange("p (h c) -> p h c", h=4), "pB", LK[1])
        pK = kb.sub(bk[2][0:64, :].rearrange("p (h c) -> p h c", h=4), "pK", LK[2])
        pL = kb.sub(bk[3][0:64, 0:256].rearrange("p (h c) -> p h c", h=4), "pL", LK[3])
        psQ = kb.sub(bk[3][0:64, 256:512].rearrange("p (h c) -> p h c", h=4), "psQ", LK[3])
        psNL = kb.sub(bk[4][0:64, 0:256].rearrange("p (h c) -> p h c", h=4), "psNL", LK[4])
        psG = kb.sub(bk[4][0:64, 256:512], "psG", LK[4])
        pW = kb.sub(bk[5][0:64, 0:256].rearrange("p (h c) -> p h c", h=4), "pW", LK[5])
        pU = kb.sub(bk[5][0:64, 256:512].rearrange("p (h c) -> p h c", h=4), "pU", LK[5])
        pO = kb.sub(bk[6][0:64, 0:256].rearrange("p (h c) -> p h c", h=4), "pO", LK[6])
        pdS = kb.sub(bk[6][0:64, 256:512].rearrange("p (h c) -> p h c", h=4), "pdS", LK[6])
        tp = kb.sub(bkT[0:64, :].rearrange("p (b c) -> p b c", b=16), "tp", LK[7])

        S = kb.sb([64, 4, 64], F32, "S")
        Sb = kb.sb([64, 4, 64], BF16, "Sb")
        kb.dve("memset", S[:, :, :], 0.0)
        kb.dve("memset", Sb[:, :, :], 0.0)

        def two(shape, dt, name):
            return [kb.sb(shape, dt, f"{name}{i}") for i in range(2)]

        R0 = two([64, 4, TS + 1], F32, "R0"); K0 = two([64, 4, TS + 1], F32, "K0"); V0 = two([64, 4, TS + 1], F32, "V0")
        W0 = two([64, TS + 1], F32, "W0"); A0 = two([64, TS + 1], F32, "A0"); G0 = two([128, TS + 1], F32, "G0")
        d3 = kb.sb([64, 4, TS], F32, "d3"); t1 = d3
        rs = kb.sb([64, 4, TS], F32, "rs"); ks = kb.sb([64, 4, TS], F32, "ks"); vs = kb.sb([64, 4, TS], F32, "vs_")
        d1 = kb.sb([128, TS], F32, "d1")
        tzw = kb.sb([64, TS], BF16, "tzw"); zab = kb.sb([64, TS], BF16, "zab")
        sgb = two([128, TS], BF16, "sgb")
        sig = kb.sb([64, 4, TS], F32, "sig"); av = kb.sb([64, 4, TS], F32, "av")
        ell = sig; Gc = kb.sb([64, 4, TS], F32, "Gc")
        kk = kb.sb([64, 4, TS], F32, "kk"); sq = kb.sb([64, 4, TS], F32, "sq")
        lnss = kb.sb([64, TS], F32, "lnss")
        rn = kb.sb([64, 4, TS], F32, "rn")
        kap = kb.sb([64, 4, TS], F32, "kap")
        kp = kb.sb([64, 4, TS], F32, "kp"); bet = kb.sb([64, 4, TS], F32, "bet")
        pd = kk
        eG = kb.sb([64, 4, TS], F32, "eG"); eGm = eG
        enG = kb.sb([64, 4, TS], F32, "enG"); edG = enG
        gC = two([64, HN], F32, "gC")
        KR = two([64, HN, 2, 64], BF16, "KR")
        KT = two([64, HN, 64], BF16, "KT"); BT = two([64, HN, 64], BF16, "BT")
        FM = two([64, 4, HN, 64], BF16, "FM")
        OUT = two([64, NCH, 256], F32, "OUT")

        TM = two([64, 16, 64], BF16, "TM")
        Nf = two([64, 4, 64], F32, "Nf"); Lf = two([64, 4, 64], F32, "Lf")
        Qm = kb.sb([64, 4, 64], F32, "Qm")
        NArb = two([64, 4, 64], BF16, "NArb"); AK = two([64, 4, 128], BF16, "AK")
        TT = two([64, 4, 64], BF16, "TT")
        Wb = kb.sb([64, 4, 64], BF16, "Wb"); Ub = kb.sb([64, 4, 64], BF16, "Ub")
        osb = kb.sb([64, 4, 64], F32, "osb"); osq = kb.sb([64, 4, 64], F32, "osq")
        st = kb.sb([64, 24], F32, "st")
        on = kb.sb([64, 4, 64], F32, "on"); bon = kb.sb([64, 4, 64], F32, "bon")

        B3 = [64, 4, TS]
        f32v = lambda b: b[:, :, :].rearrange("p h (n c) -> p (h n) c", c=64)

        def shift3(X0, zd, mu, out, t0, i):
            src = zd.ap().rearrange("(h j) t -> j h t", h=4)
            if t0 == 0:
                kb.pool("memset", X0[i][:, :, 0:1], 0.0)
                kb.dma(X0[i][:, :, 1:TS + 1], src[:, :, 0:TS])
            else:
                kb.dma(X0[i][:, :, :], src[:, :, t0 - 1:t0 + TS])
            kb.pool("tensor_tensor", out=d3[:, :, :], in0=X0[i][:, :, 0:TS], in1=X0[i][:, :, 1:TS + 1], op=ALU.subtract)
            kb.pool("tensor_tensor", out=d3[:, :, :], in0=d3[:, :, :], in1=mu.bc(B3), op=ALU.mult)
            kb.pool("tensor_tensor", out=out[:, :, :], in0=d3[:, :, :], in1=X0[i][:, :, 1:TS + 1], op=ALU.add)

        def shift1(X0, zd, mucol, np_, t0, i):
            if t0 == 0:
                kb.pool("memset", X0[i][:, 0:1], 0.0)
                kb.dma(X0[i][:, 1:TS + 1], zd[:, 0:TS])
            else:
                kb.dma(X0[i][:, :], zd[:, t0 - 1:t0 + TS])
            kb.dve("tensor_tensor", out=d1[0:np_, :], in0=X0[i][:, 0:TS], in1=X0[i][:, 1:TS + 1], op=ALU.subtract)
            kb.dve("scalar_tensor_tensor", out=d1[0:np_, :], in0=d1[0:np_, :], scalar=mucol, in1=X0[i][:, 1:TS + 1], op0=ALU.mult, op1=ALU.add)

        for tt in range(ntile):
            t0 = tt * TS
            i = tt % 2
            shift3(R0, zr, mu_r, rs, t0, i)
            shift3(K0, zk, mu_k, ks, t0, i)
            if not layer1:
                shift3(V0, zv, mu_v, vs, t0, i)
            else:
                srcv = zvad.ap().rearrange("(h j) t -> j h t", h=8)
                if t0 == 0:
                    kb.pool("memset", VA0[:, :, 0:1], 0.0)
                    kb.dma(VA0[:, :, 1:TS + 1], srcv[:, :, 0:TS])
                else:
                    kb.dma(VA0[:, :, :], srcv[:, :, t0 - 1:t0 + TS])
                B8 = [64, 8, TS]
                kb.pool("tensor_tensor", out=vsa[:, :, :], in0=VA0[:, :, 0:TS], in1=VA0[:, :, 1:TS + 1], op=ALU.subtract)
                kb.pool("tensor_tensor", out=vsa[:, :, :], in0=vsa[:, :, :], in1=pv[:, 0:8].bc(B8), op=ALU.mult)
                kb.pool("tensor_tensor", out=vsa[:, :, :], in0=vsa[:, :, :], in1=VA0[:, :, 1:TS + 1], op=ALU.add)
                kb.dve("tensor_copy", out=vsab[:, :, :], in_=vsa[:, :, :])
                kb.dma(vf[:, :, :], vfd.ap().rearrange("(h j) t -> j h t", h=4)[:, :, t0:t0 + TS])
                for h8 in range(8):
                    kb.pe("matmul", sc[0:32, :], lhsT=v1b[:, h8, :], rhs=vsab[:, h8, :], start=(h8 == 0), stop=(h8 == 7))
                kb.act("copy", out=latb[:, :], in_=sc[0:32, :])
                for h in range(4):
                    kb.pe("matmul", sc[:, :], lhsT=v2b[:, h * 64:(h + 1) * 64], rhs=latb[:, :], start=True, stop=True)
                    kb.act("activation", out=sgv[:, h, :], in_=sc[:, :], func=AF.Sigmoid, bias=pv[:, 8 + h:9 + h], scale=1.0)
                kb.dve("tensor_tensor", out=vf[:, :, :], in0=vf[:, :, :], in1=vsa[:, 0:4, :], op=ALU.subtract)
                kb.dve("tensor_tensor", out=vf[:, :, :], in0=vf[:, :, :], in1=sgv[:, :, :], op=ALU.mult)
                kb.dve("tensor_tensor", out=vs[:, :, :], in0=vsa[:, 0:4, :], in1=vf[:, :, :], op=ALU.add)
            kb.dma(vout.ap().rearrange("(h j) t -> j h t", h=4)[:, :, t0:t0 + TS], vs[:, :, :], q="pool")
            shift1(W0, zw, pr[:, 32:33], 64, t0, i)
            kb.act("activation", out=tzw[:, :], in_=d1[0:64, :], func=AF.Tanh)
            shift1(A0, za, pr[:, 33:34], 64, t0, i)
            kb.act("copy", out=zab[:, :], in_=d1[0:64, :])
            shift1(G0, zg, mug[:, 0:1], 128, t0, i)
            kb.act("activation", out=sgb[i][:, :], in_=d1[:, :], func=AF.Sigmoid)
            for h in range(4):
                kb.pe("matmul", sc[:, :], lhsT=w2b[:, h * 64:(h + 1) * 64], rhs=tzw[:, :], start=True, stop=True)
                kb.act("activation", out=sig[:, h, :], in_=sc[:, :], func=AF.Sigmoid, bias=w0[:, h:h + 1], scale=1.0)
            for h in range(4):
                kb.pe("matmul", sc[:, :], lhsT=a2b[:, h * 64:(h + 1) * 64], rhs=zab[:, :], start=True, stop=True)
                kb.act("activation", out=av[:, h, :], in_=sc[:, :], func=AF.Sigmoid, bias=a0[:, h:h + 1], scale=1.0)
            kb.dve("tensor_scalar", out=ell[:, :, :], in0=sig[:, :, :], scalar1=-0.6065306597126334, scalar2=None, op0=ALU.mult)
            kb.dve("tensor_tensor_scan", out=Gc[:, :, :].rearrange("p h t -> p (h t)"), data0=rmask,
                   data1=ell[:, :, :].rearrange("p h t -> p (h t)"), initial=0.0, op0=ALU.mult, op1=ALU.add)
            kb.dve("tensor_tensor", out=kk[:, :, :], in0=ks[:, :, :], in1=k_k.bc(B3), op=ALU.mult)
            kb.dve("tensor_tensor", out=sq[:, :, :], in0=kk[:, :, :], in1=kk[:, :, :], op=ALU.mult)
            for h in range(4):
                kb.pe("matmul", sc[:, :], lhsT=ones, rhs=sq[:, h, :], start=True, stop=True)
                kb.act("activation", out=lnss[:, :], in_=sc[:, :], func=AF.Ln)
                kb.act("activation", out=rn[:, h, :], in_=lnss[:, :], func=AF.Exp, scale=-0.5)
            kb.dve("tensor_tensor", out=kap[:, :, :], in0=kk[:, :, :], in1=rn[:, :, :], op=ALU.mult)
            kb.dve("tensor_tensor", out=t1[:, :, :], in0=av[:, :, :], in1=k_a.bc(B3), op=ALU.mult)
            kb.dve("tensor_tensor", out=t1[:, :, :], in0=t1[:, :, :], in1=omka[:, :].bc(B3), op=ALU.add)
            kb.dve("tensor_tensor", out=kp[:, :, :], in0=ks[:, :, :], in1=t1[:, :, :], op=ALU.mult)
            kb.dve("tensor_tensor", out=bet[:, :, :], in0=kap[:, :, :], in1=av[:, :, :], op=ALU.mult)
            kb.pool("tensor_tensor", out=pd[:, :, :], in0=rs[:, :, :], in1=kp[:, :, :], op=ALU.mult)
            kb.pool("tensor_tensor", out=pd[:, :, :], in0=pd[:, :, :], in1=r_k.bc(B3), op=ALU.mult)
            fm = FM[i]
            kb.pool("tensor_copy", out=fm[:, 3, :, :], in_=f32v(pd))
            G3 = f32v(Gc)
            kr = KR[i]
            kb.act("activation", out=eG[:, :, :], in_=Gc[:, :, :], func=AF.Exp)
            kb.dve("tensor_tensor", out=kr[:, :, 1, :], in0=f32v(rs), in1=f32v(eG), op=ALU.mult)
            kb.act("activation", out=enG[:, :, :], in_=Gc[:, :, :], func=AF.Exp, scale=-1.0)
            kb.dve("tensor_tensor", out=KT[i][:, :, :], in0=f32v(kp), in1=f32v(enG), op=ALU.mult)
            kb.dve("tensor_tensor", out=BT[i][:, :, :], in0=f32v(bet), in1=f32v(enG), op=ALU.mult)
            kb.dve("tensor_tensor", out=t1[:, :, :], in0=Gc[:, :, :], in1=ell[:, :, :], op=ALU.subtract)
            kb.act("activation", out=eGm[:, :, :], in_=t1[:, :, :], func=AF.Exp)
            kb.dve("tensor_tensor", out=kr[:, :, 0, :], in0=f32v(kap), in1=f32v(eGm), op=ALU.mult)
            kb.dve("tensor_tensor", out=f32v(sq), in0=G3[:, :, 63:64].to_broadcast([64, HN, 64]), in1=G3, op=ALU.subtract)
            kb.act("activation", out=edG[:, :, :], in_=sq[:, :, :], func=AF.Exp)
            kb.act("activation", out=gC[i][:, :], in_=G3[:, :, 63:64].rearrange("p a c -> p (a c)"), func=AF.Exp)
            kb.pool("tensor_copy", out=fm[:, 0, :, :], in_=f32v(vs))
            kb.pool("tensor_tensor", out=fm[:, 1, :, :], in0=f32v(kp), in1=f32v(edG), op=ALU.mult)
            kb.dve("scalar_tensor_tensor", out=fm[:, 2, :, :], in0=f32v(bet), scalar=-1.0, in1=f32v(edG), op0=ALU.mult, op1=ALU.mult)

            for n in range(NCH):
                ci = (tt * NCH + n) % 2
                for blk in range(4):
                    for h in range(4):
                        kb.pe("transpose", out=tp[:, blk * 4 + h, :], in_=fm[:, blk, h * NCH + n, :], identity=identb[:, :])
                tm = TM[ci]
                kb.act("copy", out=tm[:, :, :], in_=tp[:, :, :])
                for h in range(4):
                    hn = h * NCH + n
                    krhs = kr[:, hn, :, :].rearrange("p a c -> p (a c)")
                    kb.pe("matmul", pB[:, h, :], lhsT=BT[i][:, hn, :], rhs=krhs, start=True, stop=True)
                    kb.pe("matmul", pK[:, h, :], lhsT=KT[i][:, hn, :], rhs=krhs, start=True, stop=True)
                    kb.pe("matmul", pL[:, h, :], lhsT=kr[:, hn, 0, :], rhs=BT[i][:, hn, :], start=True, stop=True)
                Nc, Lc = Nf[0], Lf[0]
                kb.dve("tensor_tensor", out=Nc[:, :, :], in0=pB[:, :, 0:64], in1=Mb[:, :, 0:64], op=ALU.mult)
                kb.dve("tensor_tensor", out=NArb[ci][:, :, :], in0=pB[:, :, 64:128], in1=Mb[:, :, 64:128], op=ALU.mult)
                kb.dve("tensor_tensor", out=AK[ci][:, :, :], in0=pK[:, :, :], in1=Mk, op=ALU.mult)
                kb.dve("tensor_tensor", out=Lc[:, :, :], in0=pL[:, :, :], in1=Ml, op=ALU.mult)
                kb.dve("tensor_tensor", out=Qm[:, :, :], in0=I4, in1=Nc[:, :, :], op=ALU.subtract)
                for j in range(5):
                    Ln, Nn = Lf[(j + 1) % 2], Nf[(j + 1) % 2]
                    for h in range(4):
                        kb.pe("matmul", psNL[:, h, :], lhsT=Nc[:, h, :], rhs=Lc[:, h, :], start=True, stop=True)
                    kb.act("copy", out=Ln[:, :, :], in_=psNL[:, :, :])
                    if j < 4:
                        for h in range(4):
                            kb.pe("matmul", psNL[:, h, :], lhsT=Lc[:, h, :], rhs=Nc[:, h, :], start=True, stop=True)
                        kb.dve("tensor_copy", out=Nn[:, :, :], in_=psNL[:, :, :])
                    for h in range(4):
                        kb.pe("matmul", psQ[:, h, :], lhsT=Ln[:, h, :], rhs=Qm[:, h, :], start=True, stop=True)
                    kb.dve("tensor_tensor", out=Qm[:, :, :], in0=Qm[:, :, :], in1=psQ[:, :, :], op=ALU.add)
                    Lc, Nc = Ln, Nn
                kb.act("copy", out=TT[ci][:, :, :], in_=Qm[:, :, :])
                for h in range(4):
                    hn = h * NCH + n
                    kb.pe("matmul", pW[:, h, :], lhsT=kr[:, hn, 0, :], rhs=Sb[:, h, :], start=True, stop=False)
                    kb.pe("matmul", pW[:, h, :], lhsT=AK[ci][:, h, 0:64], rhs=tm[:, h, :], start=False, stop=True)
                kb.act("copy", out=Wb[:, :, :], in_=pW[:, :, :])
                for h in range(4):
                    kb.pe("matmul", pU[:, h, :], lhsT=TT[ci][:, h, :], rhs=Wb[:, h, :], start=True, stop=True)
                kb.dve("tensor_copy", out=Ub[:, :, :], in_=pU[:, :, :])
                for h in range(4):
                    hn = h * NCH + n
                    kb.pe("matmul", pO[:, h, :], lhsT=kr[:, hn, 1, :], rhs=Sb[:, h, :], start=True, stop=False)
                    kb.pe("matmul", pO[:, h, :], lhsT=AK[ci][:, h, 64:128], rhs=tm[:, h, :], start=False, stop=False)
                    kb.pe("matmul", pO[:, h, :], lhsT=NArb[ci][:, h, :], rhs=Ub[:, h, :], start=False, stop=True)
                for h in range(4):
                    kb.pe("matmul", pdS[:, h, :], lhsT=tm[:, 4 + h, :], rhs=tm[:, h, :], start=True, stop=False)
                    kb.pe("matmul", pdS[:, h, :], lhsT=tm[:, 8 + h, :], rhs=Ub[:, h, :], start=False, stop=True)
                gcv = gC[i][:, :].rearrange("p (h n) -> p h n", h=4)[:, :, n:n + 1].to_broadcast([64, 4, 64])
                kb.dve("tensor_tensor", out=S[:, :, :], in0=S[:, :, :], in1=gcv, op=ALU.mult)
                kb.dve("tensor_tensor", out=S[:, :, :], in0=S[:, :, :], in1=pdS[:, :, :], op=ALU.add)
                kb.act("copy", out=Sb[:, :, :], in_=S[:, :, :])
                kb.pe("matmul", psG[:, :], lhsT=sgb[i][:, n * 64:(n + 1) * 64], rhs=g2b[:, :], start=True, stop=True)
                kb.act("copy", out=osb[:, :, :], in_=pO[:, :, :])
                kb.pool("tensor_tensor", out=osq[:, :, :], in0=osb[:, :, :], in1=osb[:, :, :], op=ALU.mult)
                kb.dve("tensor_reduce", out=st[:, 0:4], in_=osb[:, :, :], axis=AX.X, op=ALU.add)
                kb.dve("tensor_reduce", out=st[:, 4:8], in_=osq[:, :, :], axis=AX.X, op=ALU.add)
                kb.dve("tensor_reduce", out=st[:, 20:24], in_=tm[:, 12:16, :], axis=AX.X, op=ALU.add)
                kb.dve("tensor_scalar", out=st[:, 0:4], in0=st[:, 0:4], scalar1=1.0 / 64, scalar2=None, op0=ALU.mult)
                kb.dve("tensor_tensor", out=st[:, 8:12], in0=st[:, 0:4], in1=st[:, 0:4], op=ALU.mult)
                kb.dve("scalar_tensor_tensor", out=st[:, 12:16], in0=st[:, 4:8], scalar=1.0 / 64, in1=st[:, 8:12], op0=ALU.mult, op1=ALU.subtract)
                kb.dve("tensor_scalar", out=st[:, 12:16], in0=st[:, 12:16], scalar1=GN_EPS, scalar2=None, op0=ALU.add)
                kb.act("activation", out=st[:, 16:20], in_=st[:, 12:16], func=AF.Sqrt)
                kb.dve("reciprocal", out=st[:, 16:20], in_=st[:, 16:20])
                S3 = [64, 4, 64]
                kb.dve("tensor_tensor", out=on[:, :, :], in0=osb[:, :, :], in1=st[:, 0:4].bc(S3), op=ALU.subtract)
                kb.dve("tensor_tensor", out=on[:, :, :], in0=on[:, :, :], in1=st[:, 16:20].bc(S3), op=ALU.mult)
                kb.pool("tensor_tensor", out=on[:, :, :], in0=on[:, :, :], in1=gng[:, :].rearrange("p (h c) -> p h c", h=4), op=ALU.mult)
                kb.pool("tensor_tensor", out=on[:, :, :], in0=on[:, :, :], in1=gnb[:, :].rearrange("p (h c) -> p h c", h=4), op=ALU.add)
                kb.pool("tensor_tensor", out=bon[:, :, :], in0=tm[:, 0:4, :], in1=st[:, 20:24].bc(S3), op=ALU.mult)
                kb.pool("tensor_tensor", out=on[:, :, :], in0=on[:, :, :], in1=bon[:, :, :], op=ALU.add)
                kb.dve("tensor_tensor", out=OUT[i][:, n, :], in0=on[:, :, :].rearrange("p h c -> p (h c)"), in1=psG[:, :], op=ALU.mult)
            kb.dma(oout[t0:t0 + TS, :].rearrange("(n t) c -> t n c", t=64), OUT[i][:, :, :], q="pool")
        kb.wait_all("pool", [oout, vout])
        kb.finish()
    return nc


DN_ALPHA = 4 ** 0.25
LN_EPS = 1e-5


def layer_norm_fm(kb, v, gb, out_dram_cols, W, psM, psV, tmp, onesf, x1b=None, out=None):
    sq, mean_sb, msq, rstd = tmp["sq"], tmp["mean"], tmp["msq"], tmp["rstd"]
    for dc in range(8):
        kb.pe("matmul", psM[:, 0:W], lhsT=onesf[:, :], rhs=v[:, dc, 0:W], start=(dc == 0), stop=(dc == 7))
    for dc in range(8):
        s = sq[dc % 2]
        kb.act("activation", out=s[:, 0:W], in_=v[:, dc, 0:W], func=AF.Square)
        kb.pe("matmul", psV[:, 0:W], lhsT=onesf[:, :], rhs=s[:, 0:W], start=(dc == 0), stop=(dc == 7))
    kb.act("copy", out=mean_sb[:, 0:W], in_=psM[:, 0:W])
    kb.dve("tensor_tensor", out=msq[:, 0:W], in0=mean_sb[:, 0:W], in1=mean_sb[:, 0:W], op=ALU.mult)
    kb.dve("tensor_tensor", out=msq[:, 0:W], in0=psV[:, 0:W], in1=msq[:, 0:W], op=ALU.subtract)
    kb.dve("tensor_scalar", out=msq[:, 0:W], in0=msq[:, 0:W], scalar1=LN_EPS, scalar2=None, op0=ALU.add)
    kb.act("activation", out=rstd[:, 0:W], in_=msq[:, 0:W], func=AF.Sqrt)
    kb.dve("reciprocal", out=rstd[:, 0:W], in_=rstd[:, 0:W])
    for dc in range(8):
        kb.dve("tensor_tensor", out=v[:, dc, 0:W], in0=v[:, dc, 0:W], in1=mean_sb[:, 0:W], op=ALU.subtract)
        kb.pool("tensor_tensor", out=v[:, dc, 0:W], in0=v[:, dc, 0:W], in1=rstd[:, 0:W], op=ALU.mult)
        kb.act("activation", out=v[:, dc, 0:W], in_=v[:, dc, 0:W], func=AF.Identity, scale=gb[:, dc:dc + 1], bias=gb[:, 8 + dc:9 + dc])
        if x1b is not None:
            kb.pool("tensor_copy", out=x1b[:, dc, 0:W], in_=v[:, dc, 0:W])
        if out is not None:
            kb.dma(out[dc * 128:(dc + 1) * 128, out_dram_cols[0]:out_dram_cols[1]], v[:, dc, 0:W], q="pool")


class Stager:
    def __init__(self, kb, w=2048, n=2):
        self.kb = kb
        self.b = [kb.sb([128, w], F32, f"stg{i}") for i in range(n)]
        self.i = 0
        self.w = w

    def load_cast(self, dst, src, np_=128, eng=None):
        kb = self.kb
        s = self.b[self.i % len(self.b)]
        eng = eng or ("dve", "pool")[self.i % 2]
        self.i += 1
        shp = list(dst.shape)
        n = int(np.prod(shp[1:]))
        sv = s[0:np_, 0:n]
        if len(shp) == 3:
            sv = sv.rearrange("p (a b) -> p a b", b=shp[2])
        kb.dma(sv, src)
        if eng == "act":
            kb.act("copy", out=dst, in_=sv)
        else:
            kb.op(eng, "tensor_copy", out=dst, in_=sv)


def build_k3a(NT, stage=9):
    W = 512
    ntile = NT // W
    nc = bass.Bass("TRN2", target_bir_lowering=False)
    es = ExitStack()
    with es:
        kb = KB(nc, es)
        EI = "ExternalInput"
        xTd = kb.dram("xT", [1024, NT], F32, kind=EI)
        onTd = kb.dram("onT", [512, NT], F32, kind=EI)
        orTd = kb.dram("orT", [512, NT], F32, kind=EI)
        zgTd = kb.dram("zgT", [2048, NT], F32, kind=EI)
        pnd = kb.dram("p_nsa", [512, 1024], F32, kind=EI)
        prd = kb.dram("p_rwkv", [512, 1024], F32, kind=EI)
        wod = kb.dram("w_out", [1024, 1024], F32, kind=EI)
        xqd = kb.dram("xq", [1024, 512], F32, kind=EI)
        xkd = kb.dram("xk", [1024, 512], F32, kind=EI)
        xvd = kb.dram("xv", [1024, 512], F32, kind=EI)
        xod = kb.dram("xo", [512, 1024], F32, kind=EI)
        memTd = kb.dram("memT", [1024, 256], F32, kind=EI)
        lnd = kb.dram("ln", [128, 32], F32, kind=EI)
        x2Td = kb.dram("x2T", [1024, NT], F32, kind="ExternalOutput")

        stg = Stager(kb, 2048, 2)
        pn = kb.sb([128, 4, 1024], BF16, "pn"); prw = kb.sb([128, 4, 1024], BF16, "prw")
        wo = kb.sb([128, 8, 1024], BF16, "wo"); xq = kb.sb([128, 8, 512], BF16, "xq_")
        xk = kb.sb([128, 8, 512], BF16, "xk_"); xv = kb.sb([128, 8, 512], BF16, "xv_")
        xo = kb.sb([128, 4, 1024], BF16, "xo_"); memb = kb.sb([128, 8, 256], BF16, "memb")
        for (dst, srcd, nk, ncol) in ((pn, pnd, 4, 1024), (prw, prd, 4, 1024), (wo, wod, 8, 1024), (xq, xqd, 8, 512), (xk, xkd, 8, 512),
                                      (xv, xvd, 8, 512), (xo, xod, 4, 1024), (memb, memTd, 8, 256)):
            src = srcd.ap().rearrange("(k p) c -> p k c", p=128)
            kstep = 2048 // ncol
            for k0 in range(0, nk, kstep):
                kw = min(kstep, nk - k0)
                stg.load_cast(dst[:, k0:k0 + kw, :], src[:, k0:k0 + kw, :])
        ln = kb.sb([128, 32], F32, "ln_"); kb.dma(ln[:, :], lnd[:, :])
        onesf = kb.sb([128, 128], F32, "onesf"); kb.dve("memset", onesf[:, :], 1.0 / 1024)
        onesb = kb.sb([128, 128], BF16, "onesb"); kb.dve("memset", onesb[:, :], 1.0)

        P = [kb.ps([128, 512], F32, f"bank{i}") for i in range(8)]
        pA, pB_, pY, pM, pV, pQ, pO, pD = P
        KT = kb.sb([128, 4, 256], BF16, "KT"); Vm = kb.sb([128, 2, 512], BF16, "Vm")
        for h in range(4):
            for k in range(8):
                kb.pe("matmul", pQ[:, 0:256], lhsT=xk[:, k, h * 128:(h + 1) * 128], rhs=memb[:, k, :], start=(k == 0), stop=(k == 7))
            kb.act("copy", out=KT[:, h, :], in_=pQ[:, 0:256])
        for mc in range(2):
            for k in range(8):
                kb.pe("matmul", pO[:, :], lhsT=memb[:, k, mc * 128:(mc + 1) * 128], rhs=xv[:, k, :], start=(k == 0), stop=(k == 7))
            kb.act("copy", out=Vm[:, mc, :], in_=pO[:, :])

        xt = kb.sb([128, 8, W], F32, "xt"); vv = kb.sb([128, 8, W], F32, "vv")
        onb = kb.sb([128, 4, W], BF16, "onb"); orb = kb.sb([128, 4, W], BF16, "orb")
        zgn = [kb.sb([128, W], F32, f"zgn{i}") for i in range(2)]; zgr = [kb.sb([128, W], F32, f"zgr{i}") for i in range(2)]
        m1 = kb.sb([128, W], F32, "m1"); m2 = kb.sb([128, W], F32, "m2")
        mT = kb.sb([128, 8, W], BF16, "mT"); x1b = kb.sb([128, 8, W], BF16, "x1b")
        qTb = kb.sb([128, 4, W], BF16, "qTb"); PTm = kb.sb([128, 2, W], BF16, "PTm")
        rden = kb.sb([128, W], F32, "rden"); oTn = kb.sb([128, 4, W], BF16, "oTn")
        tmp = {"sq": [kb.sb([128, W], F32, f"sq{i}") for i in range(2)], "mean": kb.sb([128, W], F32, "mean"),
               "msq": kb.sb([128, W], F32, "msq"), "rstd": kb.sb([128, W], F32, "rstd")}

        for tt in range(ntile if stage >= 1 else 0):
            c0, c1 = tt * W, (tt + 1) * W
            for k in range(8):
                kb.dma(xt[:, k, :], xTd[k * 128:(k + 1) * 128, c0:c1])
            stg.load_cast(onb[:, :, :], onTd.ap().rearrange("(k p) t -> p k t", p=128)[:, :, c0:c1])
            stg.load_cast(orb[:, :, :], orTd.ap().rearrange("(k p) t -> p k t", p=128)[:, :, c0:c1])
            for dc in range(8):
                gn, gr = zgn[dc % 2], zgr[dc % 2]
                kb.dma(gn[:, :], zgTd[dc * 128:(dc + 1) * 128, c0:c1])
                kb.dma(gr[:, :], zgTd[1024 + dc * 128:1024 + (dc + 1) * 128, c0:c1])
                kb.act("activation", out=gn[:, :], in_=gn[:, :], func=AF.Sigmoid)
                kb.act("activation", out=gr[:, :], in_=gr[:, :], func=AF.Sigmoid)
                for kc in range(4):
                    kb.pe("matmul", pA[:, :], lhsT=pn[:, kc, dc * 128:(dc + 1) * 128], rhs=onb[:, kc, :], start=(kc == 0), stop=(kc == 3))
                for kc in range(4):
                    kb.pe("matmul", pB_[:, :], lhsT=prw[:, kc, dc * 128:(dc + 1) * 128], rhs=orb[:, kc, :], start=(kc == 0), stop=(kc == 3))
                kb.dve("tensor_tensor", out=m1[:, :], in0=pA[:, :], in1=gn[:, :], op=ALU.mult)
                kb.dve("tensor_tensor", out=m2[:, :], in0=pB_[:, :], in1=gr[:, :], op=ALU.mult)
                kb.pool("tensor_tensor", out=mT[:, dc, :], in0=m1[:, :], in1=m2[:, :], op=ALU.add)
            for dc in range(8):
                for k in range(8):
                    kb.pe("matmul", pY[:, :], lhsT=wo[:, k, dc * 128:(dc + 1) * 128], rhs=mT[:, k, :], start=(k == 0), stop=(k == 7))
                kb.dve("scalar_tensor_tensor", out=vv[:, dc, :], in0=xt[:, dc, :], scalar=DN_ALPHA, in1=pY[:, :], op0=ALU.mult, op1=ALU.add)
            if stage < 2: continue
            layer_norm_fm(kb, vv, ln[:, 0:16], None, W, pM, pV, tmp, onesf, x1b=x1b)
            if stage < 3: continue
            for h in range(4):
                for k in range(8):
                    kb.pe("matmul", pQ[:, :], lhsT=xq[:, k, h * 128:(h + 1) * 128], rhs=x1b[:, k, :], start=(k == 0), stop=(k == 7))
                kb.act("mul", out=qTb[:, h, :], in_=pQ[:, :], mul=128 ** -0.5)
            if stage < 4: continue
            for h in range(4):
                for mc in range(2 if stage >= 4 else 0):
                    kb.pe("matmul", pA[:, :] if mc == 0 else pB_[:, :], lhsT=KT[:, h, mc * 128:(mc + 1) * 128], rhs=qTb[:, h, :], start=True, stop=True)
                    kb.act("activation", out=PTm[:, mc, :], in_=(pA if mc == 0 else pB_)[:, :], func=AF.Exp)
                if stage < 5: continue
                for mc in range(2):
                    kb.pe("matmul", pO[:, :], lhsT=Vm[:, mc, h * 128:(h + 1) * 128], rhs=PTm[:, mc, :], start=(mc == 0), stop=(mc == 1))
                for mc in range(2):
                    kb.pe("matmul", pD[:, :], lhsT=onesb[:, :], rhs=PTm[:, mc, :], start=(mc == 0), stop=(mc == 1))
                kb.dve("reciprocal", out=rden[:, :], in_=pD[:, :])
                kb.dve("tensor_tensor", out=oTn[:, h, :], in0=pO[:, :], in1=rden[:, :], op=ALU.mult)
            if stage < 6: continue
            for dc in range(8):
                for h in range(4):
                    kb.pe("matmul", pY[:, :], lhsT=xo[:, h, dc * 128:(dc + 1) * 128], rhs=oTn[:, h, :], start=(h == 0), stop=(h == 3))
                kb.dve("scalar_tensor_tensor", out=vv[:, dc, :], in0=vv[:, dc, :], scalar=DN_ALPHA, in1=pY[:, :], op0=ALU.mult, op1=ALU.add)
            layer_norm_fm(kb, vv, ln[:, 16:32], (c0, c1), W, pM, pV, tmp, onesf, out=x2Td)
        kb.wait_all("pool", [x2Td])
        kb.finish()
    return nc


def moe_consts():
    oh = np.zeros((16, 16, 128), np.float32)
    for e in range(16):
        oh[e, e, :] = 1.0
    return {"oh16": np.ascontiguousarray(oh.reshape(16, 16 * 128)), "identf": np.eye(128, dtype=np.float32)}


def build_k3b(NT):
    HT = min(2048, NT)
    nhalf = NT // HT
    W = 512
    WL = 256
    nc = bass.Bass("TRN2", target_bir_lowering=False)
    es = ExitStack()
    with es:
        kb = KB(nc, es)
        EI = "ExternalInput"
        x2Td = kb.dram("x2T", [1024, NT], F32, kind=EI)
        rwd = kb.dram("rw", [1024, 16], F32, kind=EI)
        rbd = kb.dram("rb", [1, 16], F32, kind=EI)
        wgd = kb.dram("wg", [16, 1024, 512], F32, kind=EI)
        wud = kb.dram("wu", [16, 1024, 512], F32, kind=EI)
        wdd = kb.dram("wd", [16, 512, 1024], F32, kind=EI)
        lnd = kb.dram("ln", [128, 16], F32, kind=EI)
        ohd = kb.dram("oh16", [16, 16 * 128], F32, kind=EI)
        idd = kb.dram("identf", [128, 128], F32, kind=EI)
        x3Td = kb.dram("x3T", [1024, NT], F32, kind="ExternalOutput")

        stg = Stager(kb, 1024, 2)
        rw = kb.sb([128, 8, 16], F32, "rw_"); kb.dma(rw[:, :, :], rwd.ap().rearrange("(k p) e -> p k e", p=128))
        rb = kb.sb([128, 16], F32, "rb_"); kb.dma(rb[:, :], rbd[0:1, :].partition_broadcast(128))
        ln = kb.sb([128, 16], F32, "ln_"); kb.dma(ln[:, :], lnd[:, :])
        oh16 = kb.sb([16, 16, 128], F32, "oh16_"); kb.dma(oh16[:, :, :], ohd.ap().rearrange("r (e c) -> r e c", c=128))
        identf = kb.sb([128, 128], F32, "identf_"); kb.dma(identf[:, :], idd[:, :])
        onesf = kb.sb([128, 128], F32, "onesf"); kb.dve("memset", onesf[:, :], 1.0 / 1024)

        P = [kb.ps([128, 512], F32, f"bank{i}") for i in range(8)]
        pR, pGb, pHg0, pHu0, pHg1, pHu1, pY0, pY1 = P

        x2b = kb.sb([128, 8, HT], BF16, "x2b"); yacc = kb.sb([128, 8, HT], F32, "yacc")
        gateT = kb.sb([16, HT], F32, "gateT")
        wgb = [kb.sb([128, 8, 512], BF16, f"wgb{i}") for i in range(2)]
        wub = [kb.sb([128, 8, 512], BF16, f"wub{i}") for i in range(2)]
        wdb = [kb.sb([128, 4, 1024], BF16, f"wdb{i}") for i in range(2)]
        hT = kb.sb([128, 4, W], BF16, "hT")
        sg = [kb.sb([128, W], F32, f"sg{i}") for i in range(2)]; hu = [kb.sb([128, W], F32, f"hu{i}") for i in range(2)]
        gbs = kb.sb([128, W], F32, "gbs")
        xr = [kb.sb([128, 8, 128], F32, f"xr{i}") for i in range(2)]
        r = {n: kb.sb([128, 16], F32, "r_" + n) for n in ("s", "ssel", "eq", "s2", "msel", "oh1", "oh2", "st", "gate", "t16")}
        q = {n: kb.sb([128, 4], F32, "q_" + n) for n in ("m1", "m2", "gsc", "ing", "t")}
        c = {n: kb.sb([128, 1], F32, "c_" + n) for n in ("gmax", "e1", "e2", "ssum")}
        xt = kb.sb([128, 8, WL], F32, "xt")
        tmp = {"sq": [kb.sb([128, WL], F32, f"sq{i}") for i in range(2)], "mean": kb.sb([128, WL], F32, "mean"),
               "msq": kb.sb([128, WL], F32, "msq"), "rstd": kb.sb([128, WL], F32, "rstd")}
        G44 = [128, 4, 4]
        v44 = lambda b: b[:, :].rearrange("p (g e) -> p g e", e=4)

        for hf in range(nhalf):
            h0 = hf * HT
            for k in range(8):
                cs_ = min(1024, HT)
                for c0 in range(0, HT, cs_):
                    stg.load_cast(x2b[:, k, c0:c0 + cs_], x2Td[k * 128:(k + 1) * 128, h0 + c0:h0 + c0 + cs_])
            for rt in range(HT // 128):
                xrt = xr[rt % 2]
                kb.dma(xrt[:, :, :], x2Td.ap().rearrange("(k p) t -> p k t", p=128)[:, :, h0 + rt * 128:h0 + (rt + 1) * 128])
                for k in range(8):
                    kb.pe("matmul", pR[:, 0:16], lhsT=xrt[:, k, :], rhs=rw[:, k, :], start=(k == 0), stop=(k == 7))
                kb.act("activation", out=r["s"][:, :], in_=pR[:, 0:16], func=AF.Sigmoid)
                kb.dve("tensor_tensor", out=r["ssel"][:, :], in0=r["s"][:, :], in1=rb[:, :], op=ALU.add)
                kb.dve("tensor_reduce", out=q["m1"][:, :], in_=v44(r["ssel"]), axis=AX.X, op=ALU.max)
                kb.dve("tensor_tensor", out=v44(r["eq"]), in0=v44(r["ssel"]), in1=q["m1"][:, :].bc(G44), op=ALU.is_equal)
                kb.dve("scalar_tensor_tensor", out=r["s2"][:, :], in0=r["eq"][:, :], scalar=-1e9, in1=r["ssel"][:, :], op0=ALU.mult, op1=ALU.add)
                kb.dve("tensor_reduce", out=q["m2"][:, :], in_=v44(r["s2"]), axis=AX.X, op=ALU.max)
                kb.dve("tensor_tensor", out=q["gsc"][:, :], in0=q["m1"][:, :], in1=q["m2"][:, :], op=ALU.add)
                kb.dve("tensor_reduce", out=c["gmax"][:, :], in_=q["gsc"][:, :], axis=AX.X, op=ALU.max)
                kb.dve("tensor_scalar", out=q["ing"][:, :], in0=q["gsc"][:, :], scalar1=c["gmax"][:, 0:1], scalar2=None, op0=ALU.is_equal)
                kb.dve("tensor_scalar", out=q["t"][:, :], in0=q["ing"][:, :], scalar1=1e9, scalar2=-1e9, op0=ALU.mult, op1=ALU.add)
                kb.dve("tensor_tensor", out=v44(r["msel"]), in0=v44(r["ssel"]), in1=q["ing"][:, :].bc(G44), op=ALU.mult)
                kb.dve("tensor_tensor", out=v44(r["msel"]), in0=v44(r["msel"]), in1=q["t"][:, :].bc(G44), op=ALU.add)
                kb.dve("tensor_reduce", out=c["e1"][:, :], in_=r["msel"][:, :], axis=AX.X, op=ALU.max)
                kb.dve("tensor_scalar", out=r["oh1"][:, :], in0=r["msel"][:, :], scalar1=c["e1"][:, 0:1], scalar2=None, op0=ALU.is_equal)
                kb.dve("scalar_tensor_tensor", out=r["s2"][:, :], in0=r["oh1"][:, :], scalar=-2e9, in1=r["msel"][:, :], op0=ALU.mult, op1=ALU.add)
                kb.dve("tensor_reduce", out=c["e2"][:, :], in_=r["s2"][:, :], axis=AX.X, op=ALU.max)
                kb.dve("tensor_scalar", out=r["oh2"][:, :], in0=r["s2"][:, :], scalar1=c["e2"][:, 0:1], scalar2=None, op0=ALU.is_equal)
                kb.dve("tensor_tensor", out=r["oh1"][:, :], in0=r["oh1"][:, :], in1=r["oh2"][:, :], op=ALU.add)
                kb.dve("tensor_tensor", out=r["st"][:, :], in0=r["s"][:, :], in1=r["oh1"][:, :], op=ALU.mult)
                kb.dve("tensor_reduce", out=c["ssum"][:, :], in_=r["st"][:, :], axis=AX.X, op=ALU.add)
                kb.dve("reciprocal", out=c["ssum"][:, :], in_=c["ssum"][:, :])
                kb.dve("tensor_scalar", out=r["gate"][:, :], in0=r["st"][:, :], scalar1=c["ssum"][:, 0:1], scalar2=None, op0=ALU.mult)
                kb.pe("transpose", out=pGb[0:16, 0:128], in_=r["gate"][:, :], identity=identf[:, :])
                kb.act("copy", out=gateT[:, rt * 128:(rt + 1) * 128], in_=pGb[0:16, 0:128])
            it = 0
            for e in range(16):
                wi = e % 2
                for k0 in range(0, 8, 2):
                    stg.load_cast(wgb[wi][:, k0:k0 + 2, :], wgd[e].rearrange("(k p) f -> p k f", p=128)[:, k0:k0 + 2, :])
                    stg.load_cast(wub[wi][:, k0:k0 + 2, :], wud[e].rearrange("(k p) f -> p k f", p=128)[:, k0:k0 + 2, :])
                for k0 in range(4):
                    stg.load_cast(wdb[wi][:, k0:k0 + 1, :], wdd[e].rearrange("(k p) d -> p k d", p=128)[:, k0:k0 + 1, :])
                for tt in range(HT // W):
                    c0, c1 = tt * W, (tt + 1) * W
                    kb.pe("matmul", pGb[:, :], lhsT=oh16[:, e, :], rhs=gateT[:, c0:c1], start=True, stop=True)
                    kb.act("copy", out=gbs[:, :], in_=pGb[:, :])
                    for fc in range(4):
                        pHg, pHu = (pHg0, pHu0) if it % 2 == 0 else (pHg1, pHu1)
                        sgi, hui = sg[it % 2], hu[it % 2]
                        it += 1
                        for k in range(8):
                            kb.pe("matmul", pHg[:, :], lhsT=wgb[wi][:, k, fc * 128:(fc + 1) * 128], rhs=x2b[:, k, c0:c1], start=(k == 0), stop=(k == 7))
                        for k in range(8):
                            kb.pe("matmul", pHu[:, :], lhsT=wub[wi][:, k, fc * 128:(fc + 1) * 128], rhs=x2b[:, k, c0:c1], start=(k == 0), stop=(k == 7))
                        kb.act("activation", out=sgi[:, :], in_=pHg[:, :], func=AF.Silu)
                        kb.dve("tensor_tensor", out=hui[:, :], in0=pHu[:, :], in1=sgi[:, :], op=ALU.mult)
                        kb.pool("tensor_tensor", out=hT[:, fc, :], in0=hui[:, :], in1=gbs[:, :], op=ALU.mult)
                    for dc in range(8):
                        pY = pY0 if dc % 2 == 0 else pY1
                        for fc in range(4):
                            kb.pe("matmul", pY[:, :], lhsT=wdb[wi][:, fc, dc * 128:(dc + 1) * 128], rhs=hT[:, fc, :], start=(fc == 0), stop=(fc == 3))
                        if e == 0:
                            kb.dve("tensor_copy", out=yacc[:, dc, c0:c1], in_=pY[:, :])
                        else:
                            kb.dve("tensor_tensor", out=yacc[:, dc, c0:c1], in0=yacc[:, dc, c0:c1], in1=pY[:, :], op=ALU.add)
            for tt in range(HT // WL):
                c0, c1 = tt * WL, (tt + 1) * WL
                for k in range(8):
                    kb.dma(xt[:, k, :], x2Td[k * 128:(k + 1) * 128, h0 + c0:h0 + c1])
                for dc in range(8):
                    kb.dve("scalar_tensor_tensor", out=xt[:, dc, :], in0=xt[:, dc, :], scalar=DN_ALPHA, in1=yacc[:, dc, c0:c1], op0=ALU.mult, op1=ALU.add)
                layer_norm_fm(kb, xt, ln[:, 0:16], (h0 + c0, h0 + c1), WL, pHg0, pHu0, tmp, onesf, out=x3Td)
        kb.wait_all("pool", [x3Td])
        kb.finish()
    return nc


_PROGS = {}


def _prog(key, fn):
    if key not in _PROGS:
        _PROGS[key] = fn()
    return _PROGS[key]


def _run(nc, maps):
    res = run_bass_kernel_spmd(nc, maps, core_ids=list(range(8)))
    return res.results


def _c(a):
    return np.ascontiguousarray(a, dtype=np.float32)


def _lnpack(g, b):
    return np.concatenate([g.reshape(8, 128).T, b.reshape(8, 128).T], 1)


def kernel(**inp):
    inp = {k: np.asarray(v) for k, v in inp.items()}
    x = inp["x"]
    B, T, D = x.shape
    NTc = (B * T) // 8
    HPB = T // NTc
    NSA_C = 1304
    RW0 = NSA_C
    GATE0 = NSA_C + 1792
    xT = [_c(x[c // HPB, (c % HPB) * NTc:(c % HPB + 1) * NTc].T) for c in range(8)]
    NCc = (T // 16 + 127) // 128
    vfirst = None
    mconst = moe_consts()
    rconst = rwkv_consts()
    nconst = [nsa_consts(T, inp["rel_table"], g) for g in range(2)]

    def cblocks(kc):
        blocks = kc.reshape(T // 16, 16, 64)
        Bm = np.concatenate([blocks[:-1], blocks[1:]], axis=1).reshape(T // 16 - 1, 2048).T
        out = np.zeros((2048, NCc * 128), np.float32)
        out[:, :T // 16 - 1] = Bm
        return out

    for l in range(2):
        nc1 = _prog("k1", build_k1)
        w_in = _c(inp["w_in"][l])
        r1 = _run(nc1, [{"xT": xT[c], "w": w_in} for c in range(8)])
        zT = [r1[c]["zT"] for c in range(8)]
        del r1
        nc2a = _prog(("k2a", T), lambda: build_k2a(T))
        maps = []
        for c in range(8):
            b, g = c // 2, c % 2
            znT = np.concatenate([zT[b * HPB + h][0:NSA_C] for h in range(HPB)], axis=1)
            zz = _c(znT.T)
            d = {
                "qT": _c(znT[g * 256:(g + 1) * 256]),
                "kcB": cblocks(zz[:, 512 + g * 64:512 + (g + 1) * 64]),
                "vcB": cblocks(zz[:, 640 + g * 64:640 + (g + 1) * 64]),
                "ksT": _c(znT[768 + g * 64:768 + (g + 1) * 64]),
                "vs": _c(zz[:, 896 + g * 64:896 + (g + 1) * 64]),
                "kwT": _c(znT[1024 + g * 64:1024 + (g + 1) * 64]),
                "vw": _c(zz[:, 1152 + g * 64:1152 + (g + 1) * 64]),
                "gl": _c(zz[:, 1280 + g * 12:1280 + (g + 1) * 12]),
                "w1k": _c(inp["cmp_w1k"][l]), "w1v": _c(inp["cmp_w1v"][l]), "w2k": _c(inp["cmp_w2k"][l]), "w2v": _c(inp["cmp_w2v"][l]),
                "pek": _c(inp["cmp_pe_k"][l].reshape(-1)), "pev": _c(inp["cmp_pe_v"][l].reshape(-1)),
            }
            d.update(nconst[g])
            maps.append(d)
        r2a = _run(nc2a, maps)
        o_nsa = [np.concatenate([r2a[2 * b + g]["o"] for g in range(2)], axis=1) for b in range(B)]
        del r2a, maps
        nc2b = _prog(("k2b", T, l > 0), lambda: build_k2b(T, layer1=(l > 0)))
        mu = inp["rwkv_mu"][l]
        maps = []
        for c in range(8):
            b, g = c // 2, c % 2
            zrT = np.concatenate([zT[b * HPB + h][RW0:RW0 + 1792] for h in range(HPB)], axis=1)
            sl = slice(g * 256, (g + 1) * 256)
            hv = lambda v: v[sl].reshape(4, 64).T
            pr = np.zeros((64, 34), np.float32)
            plist = [mu[0:512], mu[512:1024], mu[1024:1536], inp["rwkv_w0"][l], inp["rwkv_a0"][l], inp["rwkv_kk"][l], inp["rwkv_ka"][l],
                     inp["rwkv_rk"][l].reshape(-1)]
            for p_, v in enumerate(plist):
                pr[:, 4 * p_:4 * p_ + 4] = hv(v)
            pr[:, 32] = mu[1536:1600]
            pr[:, 33] = mu[1600:1664]
            d = {
                "zr": _c(zrT[0:512][sl]), "zk": _c(zrT[512:1024][sl]), "zv": _c(zrT[1024:1536][sl]),
                "zw": _c(zrT[1536:1600]), "za": _c(zrT[1600:1664]), "zg": _c(zrT[1664:1792]),
                "pr": pr, "mug": _c(mu[1664:1792].reshape(128, 1)),
                "w2": _c(inp["rwkv_w2"][l][:, sl]), "a2": _c(inp["rwkv_a2"][l][:, sl]), "g2": _c(inp["rwkv_g2"][l][:, sl]),
                "gn": _c(np.stack([inp["rwkv_gn_g"][l][sl], inp["rwkv_gn_b"][l][sl]])),
                "cm": rconst,
            }
            if l > 0:
                perm = np.concatenate([np.arange(g * 256, (g + 1) * 256), np.arange((1 - g) * 256, (2 - g) * 256)])
                muv = mu[1024:1536][perm]
                pv = np.zeros((64, 12), np.float32)
                pv[:, 0:8] = muv.reshape(8, 64).T
                pv[:, 8:12] = hv(inp["rwkv_v0"][l - 1])
                d.update({"zva": _c(zrT[1024:1536][perm]), "vfirst": vfirst[c], "v1": _c(inp["rwkv_v1"][l - 1][perm]),
                          "v2": _c(inp["rwkv_v2"][l - 1][:, sl]), "pv": pv})
            maps.append(d)
        r2b = _run(nc2b, maps)
        o_rw = [np.concatenate([r2b[2 * b + g]["o"] for g in range(2)], axis=1) for b in range(B)]
        if l == 0:
            vfirst = [_c(r2b[c]["vs"]) for c in range(8)]
        del r2b, maps
        nc3a = _prog(("k3a", NTc), lambda: build_k3a(NTc))
        lnab = _c(np.concatenate([_lnpack(inp["ln1_g"][l], inp["ln1_b"][l]), _lnpack(inp["ln2_g"][l], inp["ln2_b"][l])], 1))
        maps = []
        for c in range(8):
            b, h = c // HPB, c % HPB
            tok = slice(h * NTc, (h + 1) * NTc)
            maps.append({"xT": xT[c], "onT": _c(o_nsa[b][tok].T), "orT": _c(o_rw[b][tok].T), "zgT": _c(zT[c][GATE0:GATE0 + 2048]),
                         "p_nsa": _c(inp["p_nsa"][l]), "p_rwkv": _c(inp["p_rwkv"][l]), "w_out": _c(inp["w_out"][l]),
                         "xq": _c(inp["xq_w"][l]), "xk": _c(inp["xk_w"][l]), "xv": _c(inp["xv_w"][l]), "xo": _c(inp["xo_w"][l]),
                         "memT": _c(inp["mem"][b].T), "ln": lnab})
        r3a = _run(nc3a, maps)
        x2T = [r3a[c]["x2T"] for c in range(8)]
        del r3a, maps, zT
        nc3b = _prog(("k3b", NTc), lambda: build_k3b(NTc))
        ln3 = _c(_lnpack(inp["ln3_g"][l], inp["ln3_b"][l]))
        maps = [dict({"x2T": x2T[c], "rw": _c(inp["router_w"]), "rb": _c(inp["router_bias"].reshape(1, 16)),
                      "wg": _c(inp["moe_w_gate"][l]), "wu": _c(inp["moe_w_up"][l]), "wd": _c(inp["moe_w_down"][l]), "ln": ln3}, **mconst)
                for c in range(8)]
        r3b = _run(nc3b, maps)
        xT = [r3b[c]["x3T"] for c in range(8)]
        del r3b, maps
    out = np.zeros((B, T, D), np.float32)
    for c in range(8):
        out[c // HPB, (c % HPB) * NTc:(c % HPB + 1) * NTc] = xT[c].T
    return out
```

```python
import numpy as np
from contextlib import ExitStack
import concourse.bass as bass
import concourse.mybir as mybir
from concourse.bass_utils import run_bass_kernel_spmd

F32 = mybir.dt.float32
BF16 = mybir.dt.bfloat16
AF = mybir.ActivationFunctionType
ALU = mybir.AluOpType
AX = mybir.AxisListType

ENGS = ("pe", "act", "dve", "pool", "sp")
NDS = 20


class Buf:
    def __init__(self, h, name):
        self.h = h
        self.name = name
        self.w = None
        self.r = {}

    def __getitem__(self, idx):
        return V(self, self.h[idx])

    def ap(self):
        return V(self, self.h[:])

    def rearrange(self, *a, **k):
        return self.ap().rearrange(*a, **k)


class V:
    def __init__(self, buf, ap):
        self.buf = buf
        self.ap = ap

    def __getitem__(self, idx):
        return V(self.buf, self.ap[idx])

    def rearrange(self, *a, **k):
        return V(self.buf, self.ap.rearrange(*a, **k))

    def bitcast(self, dt):
        return V(self.buf, self.ap.bitcast(dt))

    def to_broadcast(self, shape):
        return V(self.buf, self.ap.to_broadcast(list(shape)))

    def unsqueeze(self, ax):
        return V(self.buf, self.ap.unsqueeze(ax))

    def partition_broadcast(self, n):
        return V(self.buf, self.ap.partition_broadcast(n))

    def bc(self, shape):
        ap = self.ap
        while len(ap.shape) < len(shape):
            ap = ap.unsqueeze(len(ap.shape))
        return V(self.buf, ap.to_broadcast(list(shape)))

    @property
    def shape(self):
        return self.ap.shape


WRITE_KW = ("out", "accum_out", "out_max", "out_indices")


class KB:
    def __init__(self, nc, es):
        self.nc = nc
        self.es = es
        self.q = {e: [] for e in ENGS}
        self.cnt = {e: 0 for e in ENGS}
        self.sem = {}
        for e in ENGS:
            if e == "sp":
                continue
            self.sem[e] = es.enter_context(nc.semaphore("s_" + e))
        self.dsem = {}
        self.dval = {}
        self.dn = {}
        for e in ("sp", "pool", "act"):
            self.dsem[e] = [es.enter_context(nc.semaphore(f"d_{e}{i}")) for i in range(NDS)]
            self.dval[e] = [0] * NDS
            self.dn[e] = 0
        self.seen = {e: {} for e in ENGS}
        self.semobj = {}
        self.nbuf = 0

    def sb(self, shape, dt=F32, name=None):
        self.nbuf += 1
        name = name or f"t{self.nbuf}"
        h = self.es.enter_context(self.nc.sbuf_tensor("sb_" + getattr(self, "pfx", "") + name, list(shape), dt))
        return Buf(h, name)

    def ps(self, shape, dt=F32, name=None):
        self.nbuf += 1
        name = name or f"p{self.nbuf}"
        h = self.es.enter_context(self.nc.psum_tensor(getattr(self, "pfx", "") + name, list(shape), dt))
        return Buf(h, name)

    def pst(self, name, shape, dt=F32):
        return self.es.enter_context(self.nc.psum_tensor(getattr(self, "pfx", "") + name, list(shape), dt))

    def sub(self, view, name, bank=None):
        b = Buf(view.ap if isinstance(view, V) else view, name)
        b.bank = bank
        return b

    def banklock(self, name):
        return Buf(None, name)

    def dram(self, name, shape, dt=F32, kind="Internal"):
        h = self.nc.dram_tensor(name, list(shape), dt, kind=kind)
        return Buf(h.ap(), name)

    def _need(self, eng, tok, waits):
        if tok is None:
            return
        key, val, teng = tok
        if teng == eng and eng in ("pe", "sp"):
            return
        if self.seen[eng].get(key, 0) >= val:
            return
        waits[key] = max(waits.get(key, 0), val)

    def _deps(self, eng, reads, writes, same_ok=False):
        waits = {}
        for b in reads:
            self._need(eng, b.w, waits)
        for b in writes:
            self._need(eng, b.w, waits)
            for key, (val, teng) in b.r.items():
                if teng == eng:
                    continue
                self._need(eng, (key, val, teng), waits)
        for key, val in waits.items():
            self.seen[eng][key] = val
        return [(self.semobj[key], val) for key, val in waits.items()]

    def _mark(self, tok, reads, writes):
        key, val, eng = tok
        for b in reads:
            b.r[key] = (val, eng)
        for b in writes:
            b.w = tok
            b.r = {}

    def op(self, eng, meth, *args, **kw):
        reads, writes = [], []
        extra_r = kw.pop("_reads", [])
        extra_w = kw.pop("_writes", [])
        a2 = []
        for a in args:
            if isinstance(a, V):
                (writes if not a2 and not writes else reads).append(a.buf)
                a2.append(a.ap)
            else:
                a2.append(a)
        k2 = {}
        for k, v in kw.items():
            if isinstance(v, V):
                (writes if k in WRITE_KW else reads).append(v.buf)
                k2[k] = v.ap
            else:
                k2[k] = v
        reads += [b.buf if isinstance(b, V) else b for b in extra_r]
        writes += [b.buf if isinstance(b, V) else b for b in extra_w]
        waits = self._deps(eng, reads, writes)
        banks = []
        for b in reads + writes:
            bk = getattr(b, "bank", None)
            if bk is not None and bk not in banks:
                banks.append(bk)
        for bk in banks:
            if bk.w is not None and bk.w[2] != eng and self.seen[eng].get(bk.w[0], 0) < bk.w[1]:
                self.seen[eng][bk.w[0]] = bk.w[1]
                waits.append((self.semobj[bk.w[0]], bk.w[1]))
        self.cnt[eng] += 1
        n = self.cnt[eng]
        sem = self.sem[eng]
        key = "c_" + eng
        self.semobj[key] = sem
        self._mark((key, n, eng), reads, writes)
        for bk in banks:
            bk.w = (key, n, eng)

        def emit(e, meth=meth, a2=a2, k2=k2, waits=waits, sem=sem):
            for s, v in waits:
                e.wait_ge(s, v)
            getattr(e, meth)(*a2, **k2).then_inc(sem, 1)

        self.q[eng].append(emit)

    def pe(self, meth, *a, **k):
        self.op("pe", meth, *a, **k)

    def act(self, meth, *a, **k):
        self.op("act", meth, *a, **k)

    def dve(self, meth, *a, **k):
        self.op("dve", meth, *a, **k)

    def pool(self, meth, *a, **k):
        self.op("pool", meth, *a, **k)

    def dma(self, out, in_, q="sp", **kw):
        reads, writes = [in_.buf], [out.buf]
        i = self.dn[q] % NDS
        self.dn[q] += 1
        sem = self.dsem[q][i]
        key = f"d_{q}{i}"
        self.semobj[key] = sem
        waits = self._deps(q, reads, writes)
        prev = self.dval[q][i]
        if prev > 0 and self.seen[q].get(key, 0) < prev:
            waits.append((sem, prev))
            self.seen[q][key] = prev
        self.dval[q][i] = prev + 16
        val = prev + 16
        self._mark((key, val, "dma_" + q), reads, writes)
        oap, iap = out.ap, in_.ap

        def emit(e, waits=waits, sem=sem, oap=oap, iap=iap, kw=kw):
            for s, v in waits:
                e.wait_ge(s, v)
            e.dma_start(out=oap, in_=iap, **kw).then_inc(sem, 16)

        self.q[q].append(emit)

    def wait_all(self, eng, bufs):
        waits = {}
        for b in bufs:
            self._need(eng, b.w, waits)
        ws = [(self.semobj[k], v) for k, v in waits.items()]

        def emit(e, ws=ws):
            for s, v in ws:
                e.wait_ge(s, v)

        self.q[eng].append(emit)

    def barrier(self):
        for x in ENGS:
            ws = []
            for y in ENGS:
                if y == "sp" or y == x:
                    continue
                key = "c_" + y
                if self.cnt[y] > 0 and self.seen[x].get(key, 0) < self.cnt[y]:
                    self.seen[x][key] = self.cnt[y]
                    ws.append((self.sem[y], self.cnt[y]))
            for qn in ("sp", "pool", "act"):
                for i in range(NDS):
                    v = self.dval[qn][i]
                    key = f"d_{qn}{i}"
                    if v > 0 and self.seen[x].get(key, 0) < v:
                        self.seen[x][key] = v
                        ws.append((self.dsem[qn][i], v))

            def emit(e, ws=ws):
                for s_, v in ws:
                    e.wait_ge(s_, v)

            self.q[x].append(emit)

    def flush(self):
        self.finish()
        self.q = {e: [] for e in ENGS}

    def finish(self):
        nc = self.nc
        with nc.Block() as block:
            @block.tensor
            def _(e):
                for f in self.q["pe"]:
                    f(e)

            @block.scalar
            def _(e):
                for f in self.q["act"]:
                    f(e)

            @block.vector
            def _(e):
                for f in self.q["dve"]:
                    f(e)

            @block.gpsimd
            def _(e):
                for f in self.q["pool"]:
                    f(e)

            @block.sync
            def _(e):
                for f in self.q["sp"]:
                    f(e)


NEG = -30000.0
CR_C, CR_W, CR_S = (-3, 26), (-3, 7), (-3, 11)
NCLS_C, NCLS_W, NCLS_S = CR_C[1] - CR_C[0] + 1, CR_W[1] - CR_W[0] + 1, CR_S[1] - CR_S[0] + 1
OFF_C, OFF_W, OFF_S = 0, NCLS_C, NCLS_C + NCLS_W
NCLS = NCLS_C + NCLS_W + NCLS_S


def t5_bucket_np(dist):
    n = np.maximum(dist, 0)
    max_exact = 16
    lr = np.log(np.maximum(n, 1).astype(np.float32) / np.float32(max_exact)) / np.float32(np.log(1024 / 16))
    large = np.minimum(max_exact + (lr.astype(np.float32) * np.float32(16)).astype(np.int32), 31)
    return np.where(n < max_exact, n, large)


def nsa_consts(T, rel_table, g):
    NJ = T // 64
    NKC = T // 128
    NCc = (T // 16 + 127) // 128
    tbl = rel_table.reshape(32, 2, 4)[:, g, :]
    ni = np.arange(128)[:, None]
    qi = np.arange(128)[None, :]
    strips = np.zeros((4, 128, NCLS, 128), np.float32)
    for ci, m in enumerate(range(CR_C[0], CR_C[1] + 1)):
        dist = 128 * m + qi - 16 * ni - 31
        b = t5_bucket_np(dist)
        for h in range(4):
            strips[h, :, OFF_C + ci, :] = np.where(dist >= 0, tbl[b, h], np.float32(NEG))
    for ci, m in enumerate(range(CR_W[0], CR_W[1] + 1)):
        dist = 128 * m + qi - ni
        b = t5_bucket_np(dist)
        for h in range(4):
            strips[h, :, OFF_W + ci, :] = np.where((dist >= 0) & (dist < 512), tbl[b, h], np.float32(NEG))
    for ci, m in enumerate(range(CR_S[0], CR_S[1] + 1)):
        dist = 128 * m + qi - ni
        b = t5_bucket_np(dist)
        for h in range(4):
            strips[h, :, OFF_S + ci, :] = np.where(dist >= 0, tbl[b, h], np.float32(NEG))
    n = np.arange(NCc * 128)
    cs = n * 16
    ce = cs + 31
    ss = np.arange(NJ) * 64
    ov = ((cs[:, None] <= ss[None, :] + 63) & (ce[:, None] >= ss[None, :])).astype(np.float32)
    ov[n >= T // 16 - 1] = 0.0
    jr = np.arange(2 * NJ)[None, :] - NJ
    hi = (np.arange(128)[:, None] >= 64).astype(np.int64)
    valid = jr <= hi
    f1 = jr == hi
    f2 = jr == hi - 1
    vm = (valid & ~f1 & ~f2).astype(np.float32)
    va = np.where(f1, 2e9, np.where(f2, 3e9, np.where(valid, 0.0, -1e9))).astype(np.float32)
    es = np.zeros((NJ, NKC, 128), np.float32)
    for kc in range(NKC):
        es[kc, kc, 0:64] = 1.0
        es[NJ // 2 + kc, kc, 64:128] = 1.0
    return {
        "strips": np.ascontiguousarray(strips.reshape(4, 128, NCLS * 128)),
        "ov": np.ascontiguousarray(ov.reshape(NCc, 128, NJ)),
        "vmva": np.ascontiguousarray(np.concatenate([vm, va], 1)),
        "esel": np.ascontiguousarray(es.reshape(NJ, NKC * 128)),
        "ident": np.eye(128, dtype=np.float32),
    }


GN_EPS = 64e-5
TS = 256
NCH = TS // 64
HN = 4 * NCH
NCM = 512 + 512 + 256 + 256 + 4 * TS + 64


def rwkv_consts():
    su = np.triu(np.ones((64, 64), np.float32), 1)
    ui = np.triu(np.ones((64, 64), np.float32), 0)
    sl = np.tril(np.ones((64, 64), np.float32), -1)
    ident = np.eye(64, dtype=np.float32)
    mb = np.tile(np.concatenate([su, -ui], 1), (1, 4))
    mk = np.tile(np.concatenate([su, ui], 1), (1, 4))
    ml = np.tile(sl, (1, 4))
    i4 = np.tile(ident, (1, 4))
    rm = np.ones((64, 4 * TS), np.float32)
    rm[:, ::64] = 0.0
    ones = np.ones((64, 64), np.float32)
    return np.ascontiguousarray(np.concatenate([mb, mk, ml, i4, rm, ones], 1))


DN_ALPHA = 4 ** 0.25
LN_EPS = 1e-5


def layer_norm_fm(kb, v, gb, out_dram_cols, W, psM, psV, tmp, onesf, x1b=None, out=None):
    sq, mean_sb, msq, rstd = tmp["sq"], tmp["mean"], tmp["msq"], tmp["rstd"]
    for dc in range(8):
        kb.pe("matmul", psM[:, 0:W], lhsT=onesf[:, :], rhs=v[:, dc, 0:W], start=(dc == 0), stop=(dc == 7))
    for dc in range(8):
        s = sq[dc % 2]
        kb.act("activation", out=s[:, 0:W], in_=v[:, dc, 0:W], func=AF.Square)
        kb.pe("matmul", psV[:, 0:W], lhsT=onesf[:, :], rhs=s[:, 0:W], start=(dc == 0), stop=(dc == 7))
    kb.act("copy", out=mean_sb[:, 0:W], in_=psM[:, 0:W])
    kb.dve("tensor_tensor", out=msq[:, 0:W], in0=mean_sb[:, 0:W], in1=mean_sb[:, 0:W], op=ALU.mult)
    kb.dve("tensor_tensor", out=msq[:, 0:W], in0=psV[:, 0:W], in1=msq[:, 0:W], op=ALU.subtract)
    kb.dve("tensor_scalar", out=msq[:, 0:W], in0=msq[:, 0:W], scalar1=LN_EPS, scalar2=None, op0=ALU.add)
    kb.act("activation", out=rstd[:, 0:W], in_=msq[:, 0:W], func=AF.Sqrt)
    kb.dve("reciprocal", out=rstd[:, 0:W], in_=rstd[:, 0:W])
    for dc in range(8):
        kb.dve("tensor_tensor", out=v[:, dc, 0:W], in0=v[:, dc, 0:W], in1=mean_sb[:, 0:W], op=ALU.subtract)
        kb.pool("tensor_tensor", out=v[:, dc, 0:W], in0=v[:, dc, 0:W], in1=rstd[:, 0:W], op=ALU.mult)
        kb.act("activation", out=v[:, dc, 0:W], in_=v[:, dc, 0:W], func=AF.Identity, scale=gb[:, dc:dc + 1], bias=gb[:, 8 + dc:9 + dc])
        if x1b is not None:
            kb.pool("tensor_copy", out=x1b[:, dc, 0:W], in_=v[:, dc, 0:W])
        if out is not None:
            kb.dma(out[dc * 128:(dc + 1) * 128, out_dram_cols[0]:out_dram_cols[1]], v[:, dc, 0:W], q="pool")


class Stager:
    def __init__(self, kb, w=2048, n=2):
        self.kb = kb
        self.b = [kb.sb([128, w], F32, f"stg{i}") for i in range(n)]
        self.i = 0
        self.w = w

    def load_cast(self, dst, src, np_=128, eng=None):
        kb = self.kb
        s = self.b[self.i % len(self.b)]
        eng = eng or ("dve", "pool")[self.i % 2]
        self.i += 1
        shp = list(dst.shape)
        n = int(np.prod(shp[1:]))
        sv = s[0:np_, 0:n]
        if len(shp) == 3:
            sv = sv.rearrange("p (a b) -> p a b", b=shp[2])
        kb.dma(sv, src)
        if eng == "act":
            kb.act("copy", out=dst, in_=sv)
        else:
            kb.op(eng, "tensor_copy", out=dst, in_=sv)


def moe_consts():
    oh = np.zeros((16, 16, 128), np.float32)
    for e in range(16):
        oh[e, e, :] = 1.0
    return {"oh16": np.ascontiguousarray(oh.reshape(16, 16 * 128)), "identf": np.eye(128, dtype=np.float32)}


def stage_k1(kb, T, l, xsrc, D, ZT, ZV):
    HALF = min(4096, T)
    NCOL = 5144
    NCHK = 41
    wd = D["w_in"][l]
    wvd = D["w_v"][l]
    wb = kb.sb([128, 8, NCHK * 128], BF16, "wb")
    wvb = kb.sb([128, 8, 280], BF16, "wvb")
    xb = kb.sb([128, 8, HALF], BF16, "xb")
    stg = [kb.sb([128, 2048], F32, f"stg{i}") for i in range(3)]
    ceng = ["dve", "pool", "act"]
    si = [0]

    def load_cast(dst, src, w):
        s = stg[si[0] % 3]
        e = ceng[si[0] % 3]
        si[0] += 1
        kb.dma(s[:, :w], src)
        if e == "act":
            kb.act("copy", out=dst, in_=s[:, :w])
        else:
            kb.op(e, "tensor_copy", out=dst, in_=s[:, :w])

    kb.dve("memset", wb[:, :, NCOL:NCHK * 128], 0.0)
    for k in range(8):
        for c0 in range(0, NCOL, 2048):
            cw = min(2048, NCOL - c0)
            load_cast(wb[:, k, c0:c0 + cw], wd[k * 128:(k + 1) * 128, c0:c0 + cw], cw)
        load_cast(wvb[:, k, :], wvd[k * 128:(k + 1) * 128, :], 280)
    pss = [kb.ps([128, 512], F32, f"ps{i}") for i in range(4)]
    psv = [kb.ps([128, 512], F32, f"psv{i}") for i in range(2)]
    outs = [kb.sb([128, 512], F32, f"ob{i}") for i in range(4)]
    ovs = [kb.sb([128, 280], F32, f"ov{i}") for i in range(2)]
    it = 0
    for th in range(T // HALF):
        h0 = th * HALF
        for k in range(8):
            for c0 in range(0, HALF, 2048):
                load_cast(xb[:, k, c0:c0 + 2048], xsrc[k * 128:(k + 1) * 128, h0 + c0:h0 + c0 + 2048], 2048)
        for ct in range(NCHK):
            for tt in range(HALF // 512):
                ps = pss[it % 4]; ob = outs[it % 4]
                for k in range(8):
                    kb.pe("matmul", ps[:, :], lhsT=wb[:, k, ct * 128:(ct + 1) * 128], rhs=xb[:, k, tt * 512:(tt + 1) * 512], start=(k == 0), stop=(k == 7))
                if it % 2 == 0:
                    kb.act("copy", out=ob[:, :], in_=ps[:, :])
                else:
                    kb.dve("tensor_copy", out=ob[:, :], in_=ps[:, :])
                kb.dma(ZT[ct * 128:(ct + 1) * 128, h0 + tt * 512:h0 + (tt + 1) * 512], ob[:, :], q="pool")
                it += 1
        for tk in range(HALF // 128):
            ps = psv[tk % 2]; ov = ovs[tk % 2]
            for k in range(8):
                kb.pe("matmul", ps[:, 0:280], lhsT=xb[:, k, tk * 128:(tk + 1) * 128], rhs=wvb[:, k, :], start=(k == 0), stop=(k == 7))
            kb.act("copy", out=ov[:, :], in_=ps[:, 0:280])
            kb.dma(ZV[h0 + tk * 128:h0 + (tk + 1) * 128, :], ov[:, :], q="pool")


def stage_k2a(kb, T, g, l, ZT, ZV, D, onT):
    nc, es = kb.nc, kb.es
    stage, nq, cs = 9, None, 99
    NJ = T // 64
    NKC = T // 128
    NQ = T // 128
    NCc = (T // 16 + 127) // 128
    NCP = NCc * 128
    VW = 65 + NJ
    vb = g * 140
    qTd_v = ZT[g * 256:(g + 1) * 256, :]
    ksTd = ZT[768 + g * 64:768 + (g + 1) * 64, :]
    kwTd = ZT[1024 + g * 64:1024 + (g + 1) * 64, :]
    kcTd = ZT[512 + g * 64:512 + (g + 1) * 64, :]
    vcTd = ZT[640 + g * 64:640 + (g + 1) * 64, :]
    w1kd, w1vd, w2kd, w2vd, pekd, pevd = D["w1k"][l], D["w1v"][l], D["w2k"][l], D["w2v"][l], D["pek"][l], D["pev"][l]
    strd, ovd, vmvad, eseld, identd = D["strips"][g], D["ov"], D["vmva"], D["esel"], D["ident"]
    LK = [kb.banklock(f"lk{i}") for i in range(8)]
    bST = [kb.pst(f"bst{i}", [128, 512], F32) for i in range(2)]
    bC = [kb.pst(f"bC{i}", [128, 512], F32) for i in range(2)]
    bW = kb.pst("bW", [128, 512], F32)
    bS = [kb.pst(f"bS{i}", [128, 512], F32) for i in range(2)]
    bT = kb.pst("bT", [128, 1024], BF16)
    ST = [kb.sub(bST[i][:, :], f"ST{i}", LK[i]) for i in range(2)]
    hidp = [kb.sub(bST[i][:, :], f"hid{i}", LK[i]) for i in range(2)]
    accC = [kb.sub(bC[s_ // 2][:, (s_ % 2) * 256:(s_ % 2) * 256 + VW], f"accC{s_}", LK[2 + s_ // 2]) for s_ in range(4)]
    accW = [kb.sub(bW[:, s_ * 65:(s_ + 1) * 65], f"accW{s_}", LK[4]) for s_ in range(4)]
    accS = [[kb.sub(bS[p_][:, s_ * 65:(s_ + 1) * 65], f"accS{p_}{s_}", LK[5 + p_]) for s_ in range(4)] for p_ in range(2)]
    tpp = kb.sub(bT[:, 0:128], "tpp", LK[7])
    misc = kb.sub(bC[0][:, :], "misc", LK[2])
    misc2 = kb.sub(bC[1][:, 0:256], "misc2", LK[3])

    stg = [kb.sb([128, 2048], F32, f"stg{i}") for i in range(2)]
    sti = [0]

    def load_cast(dst, src, np_=128, w=None, eng="dve"):
        w = w or dst.shape[-1]
        s = stg[sti[0] % 2]; sti[0] += 1
        kb.dma(s[0:np_, 0:w], src)
        if eng == "act":
            kb.act("copy", out=dst, in_=s[0:np_, 0:w])
        else:
            kb.op(eng, "tensor_copy", out=dst, in_=s[0:np_, 0:w])

    identb = kb.sb([128, 128], BF16, "identb")
    identf = kb.sb([128, 128], F32, "identf")
    kb.dma(identf[:, :], identd[:, :])
    load_cast(identb[:, :], identd[:, :])
    strips = kb.sb([128, 4, NCLS * 128], BF16, "strips")
    for h in range(4 if cs >= 1 else 0):
        for c0 in range(0, NCLS * 128, 2048):
            w = min(2048, NCLS * 128 - c0)
            load_cast(strips[:, h, c0:c0 + w], strd[h][:, c0:c0 + w], eng=("dve" if (c0 // 2048) % 2 == 0 else "pool"))
    esel = kb.sb([NJ, NKC * 128], BF16, "esel")
    for c0 in range(0, NKC * 128 if cs >= 2 else 0, 2048):
        w = min(2048, NKC * 128 - c0)
        load_cast(esel[:, c0:c0 + w], eseld[:, c0:c0 + w], np_=NJ)
    vmva = kb.sb([128, 4 * NJ], F32, "vmva")
    b31 = kb.sb([128, 4], F32, "b31")
    kb.dma(b31[:, :], D["b31"][g])
    if cs >= 3: kb.dma(vmva[:, :], vmvad[:, :])
    ksT = kb.sb([64, T], BF16, "ksT"); kwT = kb.sb([64, T], BF16, "kwT")
    for c0 in range(0, T if cs >= 4 else 0, 2048):
        load_cast(ksT[:, c0:c0 + 2048], ksTd[:, c0:c0 + 2048], np_=64, eng="pool")
        load_cast(kwT[:, c0:c0 + 2048], kwTd[:, c0:c0 + 2048], np_=64, eng="dve")
    VsA = kb.sb([128, NKC, 65], BF16, "VsA"); VwA = kb.sb([128, NKC, 65], BF16, "VwA")
    if cs >= 5:
        kb.dve("memset", VsA[:, :, 64:65], 1.0)
        kb.dve("memset", VwA[:, :, 64:65], 1.0)
    for (dst, srcd) in ((VsA, ZV[:, vb:vb + 64]), (VwA, ZV[:, vb + 64:vb + 128])):
        src = srcd.rearrange("(c p) d -> p c d", p=128)
        for c0 in range(0, NKC, 32):
            cw = min(32, NKC - c0)
            s = stg[sti[0] % 2]; sti[0] += 1
            sv = s[:, 0:cw * 64].rearrange("p (c d) -> p c d", d=64)
            kb.dma(sv, src[:, c0:c0 + cw, :])
            kb.dve("tensor_copy", out=dst[:, c0:c0 + cw, 0:64], in_=sv)
    VcA = kb.sb([128, NCc, VW], BF16, "VcA")
    if cs >= 7: kb.dve("memset", VcA[:, :, 64:65], 1.0)
    for c in range(NCc if cs >= 8 else 0):
        load_cast(VcA[:, c, 65:VW], ovd[c], w=NJ)
    kcT = kb.sb([64, NCP], BF16, "kcT")

    es3 = ExitStack()
    kb.es = es3
    w1b = kb.sb([64, 32, 256], BF16, "w1b")
    w2b = kb.sb([128, 2, 64], BF16, "w2b")
    peb = kb.sb([64, 32], BF16, "peb")
    pbias = kb.sb([128, 2], F32, "pbias")
    cTf = kb.sb([64, T + 32], BF16, "cTf")
    hsb = kb.sb([128, 2, 512], F32, "hsb")
    tg = kb.sb([128, 512], F32, "tg")
    gT = kb.sb([128, 2, 512], BF16, "gT")
    kb.dve("memset", cTf[:, T:T + 32], 0.0)
    for which in range(2):
        srcT, w1d, w2d, ped = ((kcTd, w1kd, w2kd, pekd), (vcTd, w1vd, w2vd, pevd))[which]
        for c0_ in range(0, T, 2048):
            load_cast(cTf[:, c0_:c0_ + 2048], srcT[:, c0_:c0_ + 2048], np_=64)
        w1v_ = w1d.rearrange("(l d) h -> d l h", d=64)
        for l0 in range(0, 32, 8):
            s = stg[sti[0] % 2]; sti[0] += 1
            sv = s[0:64, 0:2048].rearrange("p (c h) -> p c h", h=256)
            kb.dma(sv, w1v_[:, l0:l0 + 8, :])
            kb.dve("tensor_copy", out=w1b[:, l0:l0 + 8, :], in_=sv)
        s = stg[sti[0] % 2]; sti[0] += 1
        sv = s[:, 0:128].rearrange("p (c d) -> p c d", d=64)
        kb.dma(sv, w2d.rearrange("(c p) d -> p c d", p=128))
        kb.dve("tensor_copy", out=w2b[:, :, :], in_=sv)
        s = stg[sti[0] % 2]; sti[0] += 1
        kb.dma(s[0:64, 0:32], ped.rearrange("(l d) -> d l", d=64), allow_slow_non_contiguous=True)
        kb.dve("tensor_copy", out=peb[:, :], in_=s[0:64, 0:32])
        for hc in range(2):
            for l_ in range(32):
                kb.pe("matmul", misc[:, 0:1], lhsT=w1b[:, l_, hc * 128:(hc + 1) * 128], rhs=peb[:, l_:l_ + 1], start=(l_ == 0), stop=(l_ == 31))
            kb.dve("tensor_copy", out=pbias[:, hc:hc + 1], in_=misc[:, 0:1])
        for nb in range(0, NCP, 512):
            nw = min(512, NCP - nb)
            for l_ in range(32):
                rhs = cTf[:, 16 * nb + l_:16 * nb + l_ + 16 * nw:16]
                for hc in range(2):
                    kb.pe("matmul", hidp[hc][:, 0:nw], lhsT=w1b[:, l_, hc * 128:(hc + 1) * 128], rhs=rhs, start=(l_ == 0), stop=(l_ == 31))
            for hc in range(2):
                hv = hsb[:, hc, 0:nw]
                kb.act("activation", out=hv, in_=hidp[hc][:, 0:nw], func=AF.Identity, bias=pbias[:, hc:hc + 1], scale=1.0)
                kb.dve("tensor_tensor", out=tg[:, 0:nw], in0=hv, in1=hv, op=ALU.mult)
                kb.dve("tensor_scalar", out=tg[:, 0:nw], in0=tg[:, 0:nw], scalar1=0.044715, scalar2=1.0, op0=ALU.mult, op1=ALU.add)
                kb.dve("tensor_tensor", out=tg[:, 0:nw], in0=tg[:, 0:nw], in1=hv, op=ALU.mult)
                kb.act("activation", out=tg[:, 0:nw], in_=tg[:, 0:nw], func=AF.Sigmoid, scale=1.5957691216057308)
                kb.dve("tensor_tensor", out=gT[:, hc, 0:nw], in0=tg[:, 0:nw], in1=hv, op=ALU.mult)
            if which == 0:
                for hc in range(2):
                    kb.pe("matmul", misc[0:64, 0:nw], lhsT=w2b[:, hc, :], rhs=gT[:, hc, 0:nw], start=(hc == 0), stop=(hc == 1))
                kb.act("copy", out=kcT[:, nb:nb + nw], in_=misc[0:64, 0:nw])
            else:
                for cc in range(nw // 128):
                    for hc in range(2):
                        kb.pe("matmul", misc[:, 0:64], lhsT=gT[:, hc, cc * 128:(cc + 1) * 128], rhs=w2b[:, hc, :], start=(hc == 0), stop=(hc == 1))
                    kb.act("copy", out=VcA[:, nb // 128 + cc, 0:64], in_=misc[:, 0:64])

    kb.barrier()
    kb.flush()
    es3.close()
    kb.es = es
    QB = 4 if NQ % 4 == 0 else 1
    QW = QB * 128
    qf = [kb.sb([64, 4, QW], F32, "qf0")] * 2
    qb = [kb.sb([64, 4, QW], BF16, f"qb{i}") for i in range(2)]
    glt = kb.sb([128, QB, 12], F32, "glt")
    gs = kb.sb([128, QB, 12], F32, "gs")
    PT = [kb.sb([128, QW], BF16, f"PT{i}") for i in range(4)]
    rd = kb.sb([128, QB, 12], F32, "rd")
    cf = kb.sb([128, QB, 12], F32, "cf")
    ocs = kb.sb([128, QB, 4, 64], F32, "ocs")
    imp = kb.sb([128, QB, NJ], F32, "imp")
    imp2 = kb.sb([128, NJ], F32, "imp2")
    impw = kb.sb([128, NJ], F32, "impw")
    mx = kb.sb([128, 16], F32, "mx")
    c1 = kb.sb([128, NJ], F32, "c1")
    negp = kb.sb([128, NJ], BF16, "negp")
    negT = kb.sb([NJ, QW], BF16, "negT")
    OSB = [kb.sb([128, 2, 128], F32, f"OSB{i}") for i in range(2)]
    qsrc = qTd_v.rearrange("(h d) t -> d h t", h=4)
    cnt = {"st": 0, "pt": 0, "o": 0}

    def scores(h, qbt, kT_chunk, cls0, extra=None, sat=False):
        st = ST[cnt["st"] % 2]; cnt["st"] += 1
        pt = PT[cnt["pt"] % 4]; cnt["pt"] += 1
        kb.pe("matmul", st[:, 0:QW], lhsT=kT_chunk, rhs=qbt[:, h, :], start=True, stop=(sat and extra is None))
        if extra is not None:
            kb.pe("matmul", st[:, 0:QW], lhsT=extra, rhs=negT[:, :], start=False, stop=sat)
        if sat:
            kb.act("activation", out=pt[:, :], in_=st[:, 0:QW], func=AF.Exp, bias=b31[:, h:h + 1], scale=1.0)
        else:
            kb.pe("matmul", st[:, 0:QW], lhsT=identb[:, :], rhs=strips[:, h, cls0 * 128:cls0 * 128 + QW], start=False, stop=True)
            kb.act("activation", out=pt[:, :], in_=st[:, 0:QW], func=AF.Exp)
        return pt

    for blk in range(NQ // QB):
        qt0 = blk * QB
        q0 = qt0 * 128
        i = blk % 2
        kb.dma(qf[i][:, :, :], qsrc[:, :, q0:q0 + QW])
        kb.act("mul", out=qb[i][:, :, :], in_=qf[i][:, :, :], mul=0.125)
        kb.dma(glt[:, :, :], ZV[q0:q0 + QW, vb + 128:vb + 140].rearrange("(s p) c -> p s c", p=128))
        kb.act("activation", out=gs[:, :, :], in_=glt[:, :, :], func=AF.Sigmoid)
        ncj_s = [min(NCc - 1, ((qt0 + s_) * 128 + 96) // 16 // 128) + 1 for s_ in range(QB)]
        for h in range(4):
            for cj in range(max(ncj_s)):
                m0 = min(qt0 - 16 * cj, CR_C[1] - 3)
                pt = scores(h, qb[i], kcT[:, cj * 128:(cj + 1) * 128], OFF_C + m0 - CR_C[0], sat=(qt0 - 16 * cj >= 23))
                for s_ in range(QB):
                    if cj < ncj_s[s_]:
                        kb.pe("matmul", accC[s_][:, :], lhsT=pt[:, s_ * 128:(s_ + 1) * 128], rhs=VcA[:, cj, :], start=(cj == 0 and s_ % 2 == 0), stop=(cj == ncj_s[s_] - 1))
            for s_ in range(QB):
                acc = accC[s_]
                rcol = rd[:, s_, 3 * h:3 * h + 1]
                kb.dve("tensor_scalar", out=rcol, in0=acc[:, 64:65], scalar1=1e-30, scalar2=None, op0=ALU.max)
                kb.dve("reciprocal", out=rcol, in_=rcol)
                kb.act("copy", out=ocs[:, s_, h, :], in_=acc[:, 0:64])
                if h == 0:
                    kb.dve("tensor_scalar", out=imp[:, s_, :], in0=acc[:, 65:VW], scalar1=rcol, scalar2=None, op0=ALU.mult)
                else:
                    kb.dve("scalar_tensor_tensor", out=imp[:, s_, :], in0=acc[:, 65:VW], scalar=rcol, in1=imp[:, s_, :], op0=ALU.mult, op1=ALU.add)
        for s_ in range(QB):
            qt = qt0 + s_
            j0 = NJ - 2 * qt
            kb.dve("tensor_tensor", out=imp2[:, :], in0=imp[:, s_, :], in1=vmva[:, j0:j0 + NJ], op=ALU.mult)
            kb.dve("tensor_tensor", out=imp2[:, :], in0=imp2[:, :], in1=vmva[:, 2 * NJ + j0:2 * NJ + j0 + NJ], op=ALU.add)
            kb.dve("memset", imp2[:, 0:1], 1e9)
            kb.dve("max", out=mx[:, 0:8], in_=imp2[:, :])
            kb.dve("match_replace", out=impw[:, :], in_to_replace=mx[:, 0:8], in_values=imp2[:, :], imm_value=-3e9)
            kb.dve("max", out=mx[:, 8:16], in_=impw[:, :])
            kb.dve("tensor_scalar", out=c1[:, :], in0=imp2[:, :], scalar1=mx[:, 15:16], scalar2=None, op0=ALU.is_ge)
            kb.dve("tensor_scalar", out=impw[:, :], in0=imp2[:, :], scalar1=0.0, scalar2=None, op0=ALU.is_ge)
            kb.dve("tensor_tensor", out=c1[:, :], in0=c1[:, :], in1=impw[:, :], op=ALU.mult)
            kb.dve("tensor_scalar", out=negp[:, :].rearrange("q (jj c) -> q c jj", jj=2), in0=c1[:, :].rearrange("q (c jj) -> q c jj", jj=2),
                   scalar1=-1.0, scalar2=-NEG, op0=ALU.add, op1=ALU.mult)
            kb.pe("transpose", out=tpp[0:NJ, :], in_=negp[:, :], identity=identb[:, :])
            kb.act("copy", out=negT[:, s_ * 128:(s_ + 1) * 128], in_=tpp[0:NJ, :])
        for h in range(4):
            aS = accS[h % 2]
            kcs = [kc for kc in range(qt0 - 4, qt0 + QB) if kc >= 0]
            for kc in kcs:
                pt = scores(h, qb[i], kwT[:, kc * 128:(kc + 1) * 128], OFF_W + (qt0 - kc) - CR_W[0])
                for s_ in range(QB):
                    d_ = qt0 + s_ - kc
                    if 0 <= d_ <= 4:
                        kb.pe("matmul", accW[s_][:, :], lhsT=pt[:, s_ * 128:(s_ + 1) * 128], rhs=VwA[:, kc, :], start=(kc == kcs[0] and s_ == 0), stop=(d_ == 0))
            for kc in range(qt0 + QB):
                ms0 = min(qt0 - kc, CR_S[1] - 3)
                pt = scores(h, qb[i], ksT[:, kc * 128:(kc + 1) * 128], OFF_S + ms0 - CR_S[0], extra=esel[:, kc * 128:(kc + 1) * 128], sat=(qt0 - kc >= 8))
                for s_ in range(QB):
                    if kc <= qt0 + s_:
                        kb.pe("matmul", aS[s_][:, :], lhsT=pt[:, s_ * 128:(s_ + 1) * 128], rhs=VsA[:, kc, :], start=(kc == 0 and s_ == 0), stop=(kc == qt0 + s_))
            for s_ in range(QB):
                kb.dve("reciprocal", out=rd[:, s_, 3 * h + 1:3 * h + 2], in_=aS[s_][:, 64:65])
                kb.dve("reciprocal", out=rd[:, s_, 3 * h + 2:3 * h + 3], in_=accW[s_][:, 64:65])
                kb.dve("tensor_tensor", out=cf[:, s_, 3 * h:3 * h + 3], in0=rd[:, s_, 3 * h:3 * h + 3], in1=gs[:, s_, 3 * h:3 * h + 3], op=ALU.mult)
                oc = ocs[:, s_, h, :]
                kb.dve("tensor_scalar", out=oc, in0=oc, scalar1=cf[:, s_, 3 * h:3 * h + 1], scalar2=None, op0=ALU.mult)
                kb.dve("scalar_tensor_tensor", out=oc, in0=aS[s_][:, 0:64], scalar=cf[:, s_, 3 * h + 1:3 * h + 2], in1=oc, op0=ALU.mult, op1=ALU.add)
                kb.dve("scalar_tensor_tensor", out=oc, in0=accW[s_][:, 0:64], scalar=cf[:, s_, 3 * h + 2:3 * h + 3], in1=oc, op0=ALU.mult, op1=ALU.add)
        for s_ in range(QB):
            osb = OSB[cnt["o"] % 2]; cnt["o"] += 1
            for j in range(2):
                kb.pe("transpose", out=misc2[:, j * 128:(j + 1) * 128], in_=ocs[:, s_, 2 * j:2 * j + 2, :].rearrange("p h c -> p (h c)"), identity=identf[:, :])
            kb.act("copy", out=osb[:, :, :].rearrange("p j q -> p (j q)"), in_=misc2[:, 0:256])
            kb.dma(onT[g * 256:(g + 1) * 256, q0 + s_ * 128:q0 + (s_ + 1) * 128].rearrange("(j p) q -> p j q", p=128), osb[:, :, :], q="pool")


def stage_k2b(kb, T, g, l, ZT, D, orT, vfirstT):
    nc, es = kb.nc, kb.es
    layer1 = l > 0
    ntile = T // TS
    base = 1304
    sl0, sl1 = g * 256, (g + 1) * 256
    zr = ZT[base + sl0:base + sl1, :]
    zk = ZT[base + 512 + sl0:base + 512 + sl1, :]
    zv = ZT[base + 1024 + sl0:base + 1024 + sl1, :]
    zvo = ZT[base + 1024 + (1 - g) * 256:base + 1024 + (2 - g) * 256, :]
    zw = ZT[base + 1536:base + 1600, :]
    za = ZT[base + 1600:base + 1664, :]
    zg = ZT[base + 1664:base + 1792, :]
    prd, mugd = D["pr"][l][g], D["mug"][l]
    w2d, a2d, g2d = D["w2"][l][:, sl0:sl1], D["a2"][l][:, sl0:sl1], D["g2"][l][:, sl0:sl1]
    gnd = D["gn"][l][:, sl0:sl1]
    cmd = D["cm"]
    vout = vfirstT[sl0:sl1, :]
    if layer1:
        vfd = vfirstT[sl0:sl1, :]
        v1d, v2d, pvd = D["v1"], D["v2"][:, sl0:sl1], D["pv"][g]
    cm = kb.sb([64, NCM], F32, "cm")
    kb.dma(cm[:, :], cmd[:, :])
    Mb = cm[:, 0:512].rearrange("p (h c) -> p h c", h=4)
    Mk = cm[:, 512:1024].rearrange("p (h c) -> p h c", h=4)
    Ml = cm[:, 1024:1280].rearrange("p (h c) -> p h c", h=4)
    I4 = cm[:, 1280:1536].rearrange("p (h c) -> p h c", h=4)
    rmask = cm[:, 1536:1536 + 4 * TS]
    ones = cm[:, 1536 + 4 * TS:1600 + 4 * TS]
    identb = kb.sb([64, 64], BF16, "identb")
    kb.dve("tensor_copy", out=identb[:, :], in_=cm[:, 1280:1344])
    pr = kb.sb([64, 34], F32, "pr")
    kb.dma(pr[:, :], prd[:, :])
    P = lambda i: pr[:, 4 * i:4 * i + 4]
    mu_r, mu_k, mu_v, w0, a0, k_k, k_a, r_k = [P(i) for i in range(8)]
    mug = kb.sb([128, 1], F32, "mug")
    kb.dma(mug[:, :], mugd[:, :])
    omka = kb.sb([64, 4], F32, "omka")
    kb.dve("tensor_scalar", out=omka[:, :], in0=k_a, scalar1=-1.0, scalar2=1.0, op0=ALU.mult, op1=ALU.add)
    stg = kb.sb([128, 256], F32, "wstg")
    w2b = kb.sb([64, 256], BF16, "w2b")
    a2b = kb.sb([64, 256], BF16, "a2b")
    g2b = kb.sb([128, 256], BF16, "g2b")
    kb.dma(stg[0:64, :], w2d[:, :]); kb.dve("tensor_copy", out=w2b[:, :], in_=stg[0:64, :])
    kb.dma(stg[0:64, :], a2d[:, :]); kb.dve("tensor_copy", out=a2b[:, :], in_=stg[0:64, :])
    kb.dma(stg[:, :], g2d[:, :]); kb.dve("tensor_copy", out=g2b[:, :], in_=stg[:, :])
    gng = kb.sb([64, 256], F32, "gng")
    gnb = kb.sb([64, 256], F32, "gnb")
    kb.dma(gng[:, :], gnd[0:1, :].partition_broadcast(64))
    kb.dma(gnb[:, :], gnd[1:2, :].partition_broadcast(64))

    if layer1:
        pv = kb.sb([64, 12], F32, "pv"); kb.dma(pv[:, :], pvd[:, :])
        v1b = kb.sb([64, 8, 32], BF16, "v1b")
        kb.dma(stg[0:64, 0:128].rearrange("p (h l) -> p h l", l=32), v1d[sl0:sl1, :].rearrange("(h j) l -> j h l", h=4))
        kb.dma(stg[0:64, 128:256].rearrange("p (h l) -> p h l", l=32), v1d[(1 - g) * 256:(2 - g) * 256, :].rearrange("(h j) l -> j h l", h=4))
        kb.dve("tensor_copy", out=v1b[:, :, :], in_=stg[0:64, :].rearrange("p (h l) -> p h l", l=32))
        v2b = kb.sb([32, 256], BF16, "v2b")
        kb.dma(stg[0:32, :], v2d[:, :]); kb.dve("tensor_copy", out=v2b[:, :], in_=stg[0:32, :])
        VA0 = kb.sb([64, 8, TS + 1], F32, "VA0"); vsa = kb.sb([64, 8, TS], F32, "vsa"); vsab = kb.sb([64, 8, TS], BF16, "vsab")
        vf = kb.sb([64, 4, TS], F32, "vf"); latb = kb.sb([32, TS], BF16, "latb"); sgv = kb.sb([64, 4, TS], F32, "sgv")
    bk = [kb.pst(f"bank{i}", [128, 512], F32) for i in (0, 2, 3, 4, 5, 6, 7)]
    bkT = kb.pst("bankT", [128, 1024], BF16)
    LK = [kb.banklock(f"lk{i}") for i in range(8)]
    sc = kb.sub(bk[0][0:64, 0:TS], "sc", LK[0])
    tpo = kb.sub(bk[0][:, 256:384].rearrange("p (j c) -> p j c", j=2), "tpo", LK[0])
    pB = kb.sub(bk[1][0:64, :].rearrange("p (h c) -> p h c", h=4), "pB", LK[1])
    pK = kb.sub(bk[2][0:64, :].rearrange("p (h c) -> p h c", h=4), "pK", LK[2])
    pL = kb.sub(bk[3][0:64, 0:256].rearrange("p (h c) -> p h c", h=4), "pL", LK[3])
    psG = kb.sub(bk[3][0:64, 256:512], "psG", LK[3])
    v4 = lambda ap_: ap_.rearrange("p (h c) -> p h c", h=4)
    XB = [(bk[1], LK[1]), (bk[2], LK[2]), (bk[4], LK[4]), (bk[5], LK[5])]
    psNLn = [kb.sub(v4(XB[n_][0][0:64, 0:256]), f"psNL{n_}", XB[n_][1]) for n_ in range(4)]
    psQn = [kb.sub(v4(XB[n_][0][0:64, 256:512]), f"psQ{n_}", XB[n_][1]) for n_ in range(4)]
    pB2 = kb.sub(bk[4][0:64, :].rearrange("p (h c) -> p h c", h=4), "pB2", LK[4])
    pK2 = kb.sub(bk[5][0:64, :].rearrange("p (h c) -> p h c", h=4), "pK2", LK[5])
    pL2 = kb.sub(bk[6][0:64, 0:256].rearrange("p (h c) -> p h c", h=4), "pL2", LK[6])
    pW = kb.sub(bk[5][0:64, 0:256].rearrange("p (h c) -> p h c", h=4), "pW", LK[5])
    pU = kb.sub(bk[5][0:64, 256:512].rearrange("p (h c) -> p h c", h=4), "pU", LK[5])
    pO = kb.sub(bk[6][0:64, 0:256].rearrange("p (h c) -> p h c", h=4), "pO", LK[6])
    pdS = kb.sub(bk[6][0:64, 256:512].rearrange("p (h c) -> p h c", h=4), "pdS", LK[6])
    tp = kb.sub(bkT[0:64, :].rearrange("p (b c) -> p b c", b=16), "tp", LK[7])

    S = kb.sb([64, 4, 64], F32, "S")
    Sb = kb.sb([64, 4, 64], BF16, "Sb")
    kb.dve("memset", S[:, :, :], 0.0)
    kb.dve("memset", Sb[:, :, :], 0.0)

    def two(shape, dt, name):
        return [kb.sb(shape, dt, f"{name}{i}") for i in range(2)]

    R0 = [kb.sb([64, 4, TS + 1], F32, "R0")] * 2; K0 = [kb.sb([64, 4, TS + 1], F32, "K0")] * 2
    V0 = ([kb.sb([64, 4, TS + 1], F32, "V0")] * 2) if not layer1 else None
    W0 = two([64, TS + 1], F32, "W0"); A0 = two([64, TS + 1], F32, "A0"); G0 = two([128, TS + 1], F32, "G0")
    d3 = kb.sb([64, 4, TS], F32, "d3"); t1 = d3
    rs = kb.sb([64, 4, TS], F32, "rs"); ks = kb.sb([64, 4, TS], F32, "ks"); vs = kb.sb([64, 4, TS], F32, "vs_")
    d1 = kb.sb([128, TS], F32, "d1")
    tzw = kb.sb([64, TS], BF16, "tzw"); zab = kb.sb([64, TS], BF16, "zab")
    sgb = two([128, TS], BF16, "sgb")
    sig = kb.sb([64, 4, TS], F32, "sig"); av = kb.sb([64, 4, TS], F32, "av")
    ell = sig; Gc = kb.sb([64, 4, TS], F32, "Gc")
    kk = kb.sb([64, 4, TS], F32, "kk"); sq = kb.sb([64, 4, TS], F32, "sq")
    lnss = kb.sb([64, TS], F32, "lnss")
    rn = kb.sb([64, 4, TS], F32, "rn")
    kap = kb.sb([64, 4, TS], F32, "kap")
    kp = kb.sb([64, 4, TS], F32, "kp"); bet = kb.sb([64, 4, TS], F32, "bet")
    pd = kk
    eG = kb.sb([64, 4, TS], F32, "eG"); eGm = eG
    enG = eG; edG = eG
    gC = two([64, HN], F32, "gC")
    KR = two([64, HN, 2, 64], BF16, "KR")
    KT = two([64, HN, 64], BF16, "KT"); BT = two([64, HN, 64], BF16, "BT")
    FM = two([64, 4, HN, 64], BF16, "FM")
    OUT = two([64, NCH, 256], F32, "OUT")
    OT = two([128, 2, TS], F32, "OT")

    TM = [kb.sb([64, 16, 64], BF16, f"TM{n_}") for n_ in range(NCH)]
    Nf = [two([64, 4, 64], BF16, f"Nf{n_}_") for n_ in range(NCH)]; Lf = [two([64, 4, 64], BF16, f"Lf{n_}_") for n_ in range(NCH)]
    Qm = [kb.sb([64, 4, 64], BF16, f"Qm{n_}") for n_ in range(NCH)]
    NArb = [kb.sb([64, 4, 64], BF16, f"NArb{n_}") for n_ in range(NCH)]; AK = [kb.sb([64, 4, 128], BF16, f"AK{n_}") for n_ in range(NCH)]
    TT = Qm
    Wb = kb.sb([64, 4, 64], BF16, "Wb"); Ub = kb.sb([64, 4, 64], BF16, "Ub")
    osbn = [kb.sb([64, 4, 64], F32, f"osb{n_}") for n_ in range(NCH)]; osq = kb.sb([64, 4, 64], F32, "osq")
    st = kb.sb([64, 24], F32, "st")
    on = kb.sb([64, 4, 64], F32, "on"); bon = kb.sb([64, 4, 64], F32, "bon")

    B3 = [64, 4, TS]
    f32v = lambda b: b[:, :, :].rearrange("p h (n c) -> p (h n) c", c=64)

    def shift3(X0, zd, mu, out, t0, i):
        src = zd.rearrange("(h j) t -> j h t", h=4)
        if t0 == 0:
            kb.pool("memset", X0[i][:, :, 0:1], 0.0)
            kb.dma(X0[i][:, :, 1:TS + 1], src[:, :, 0:TS])
        else:
            kb.dma(X0[i][:, :, :], src[:, :, t0 - 1:t0 + TS])
        kb.pool("tensor_tensor", out=d3[:, :, :], in0=X0[i][:, :, 0:TS], in1=X0[i][:, :, 1:TS + 1], op=ALU.subtract)
        kb.pool("tensor_tensor", out=d3[:, :, :], in0=d3[:, :, :], in1=mu.bc(B3), op=ALU.mult)
        kb.pool("tensor_tensor", out=out[:, :, :], in0=d3[:, :, :], in1=X0[i][:, :, 1:TS + 1], op=ALU.add)

    def shift1(X0, zd, mucol, np_, t0, i):
        if t0 == 0:
            kb.pool("memset", X0[i][:, 0:1], 0.0)
            kb.dma(X0[i][:, 1:TS + 1], zd[:, 0:TS])
        else:
            kb.dma(X0[i][:, :], zd[:, t0 - 1:t0 + TS])
        kb.dve("tensor_tensor", out=d1[0:np_, :], in0=X0[i][:, 0:TS], in1=X0[i][:, 1:TS + 1], op=ALU.subtract)
        kb.dve("scalar_tensor_tensor", out=d1[0:np_, :], in0=d1[0:np_, :], scalar=mucol, in1=X0[i][:, 1:TS + 1], op0=ALU.mult, op1=ALU.add)

    for tt in range(ntile):
        t0 = tt * TS
        i = tt % 2
        shift3(R0, zr, mu_r, rs, t0, i)
        shift3(K0, zk, mu_k, ks, t0, i)
        if not layer1:
            shift3(V0, zv, mu_v, vs, t0, i)
        else:
            srcm = zv.rearrange("(h j) t -> j h t", h=4)
            srco = zvo.rearrange("(h j) t -> j h t", h=4)
            if t0 == 0:
                kb.pool("memset", VA0[:, :, 0:1], 0.0)
                kb.dma(VA0[:, 0:4, 1:TS + 1], srcm[:, :, 0:TS])
                kb.dma(VA0[:, 4:8, 1:TS + 1], srco[:, :, 0:TS])
            else:
                kb.dma(VA0[:, 0:4, :], srcm[:, :, t0 - 1:t0 + TS])
                kb.dma(VA0[:, 4:8, :], srco[:, :, t0 - 1:t0 + TS])
            B8 = [64, 8, TS]
            kb.pool("tensor_tensor", out=vsa[:, :, :], in0=VA0[:, :, 0:TS], in1=VA0[:, :, 1:TS + 1], op=ALU.subtract)
            kb.pool("tensor_tensor", out=vsa[:, :, :], in0=vsa[:, :, :], in1=pv[:, 0:8].bc(B8), op=ALU.mult)
            kb.pool("tensor_tensor", out=vsa[:, :, :], in0=vsa[:, :, :], in1=VA0[:, :, 1:TS + 1], op=ALU.add)
            kb.dve("tensor_copy", out=vsab[:, :, :], in_=vsa[:, :, :])
            kb.dma(vf[:, :, :], vfd.rearrange("(h j) t -> j h t", h=4)[:, :, t0:t0 + TS])
            for h8 in range(8):
                kb.pe("matmul", sc[0:32, :], lhsT=v1b[:, h8, :], rhs=vsab[:, h8, :], start=(h8 == 0), stop=(h8 == 7))
            kb.act("copy", out=latb[:, :], in_=sc[0:32, :])
            for h in range(4):
                kb.pe("matmul", sc[:, :], lhsT=v2b[:, h * 64:(h + 1) * 64], rhs=latb[:, :], start=True, stop=True)
                kb.act("activation", out=sgv[:, h, :], in_=sc[:, :], func=AF.Sigmoid, bias=pv[:, 8 + h:9 + h], scale=1.0)
            kb.dve("tensor_tensor", out=vf[:, :, :], in0=vf[:, :, :], in1=vsa[:, 0:4, :], op=ALU.subtract)
            kb.dve("tensor_tensor", out=vf[:, :, :], in0=vf[:, :, :], in1=sgv[:, :, :], op=ALU.mult)
            kb.dve("tensor_tensor", out=vs[:, :, :], in0=vsa[:, 0:4, :], in1=vf[:, :, :], op=ALU.add)
        if not layer1:
            kb.dma(vout.rearrange("(h j) t -> j h t", h=4)[:, :, t0:t0 + TS], vs[:, :, :], q="pool")
        shift1(W0, zw, pr[:, 32:33], 64, t0, i)
        kb.act("activation", out=tzw[:, :], in_=d1[0:64, :], func=AF.Tanh)
        shift1(A0, za, pr[:, 33:34], 64, t0, i)
        kb.act("copy", out=zab[:, :], in_=d1[0:64, :])
        shift1(G0, zg, mug[:, 0:1], 128, t0, i)
        kb.act("activation", out=sgb[i][:, :], in_=d1[:, :], func=AF.Sigmoid)
        for h in range(4):
            kb.pe("matmul", sc[:, :], lhsT=w2b[:, h * 64:(h + 1) * 64], rhs=tzw[:, :], start=True, stop=True)
            kb.act("activation", out=sig[:, h, :], in_=sc[:, :], func=AF.Sigmoid, bias=w0[:, h:h + 1], scale=1.0)
        for h in range(4):
            kb.pe("matmul", sc[:, :], lhsT=a2b[:, h * 64:(h + 1) * 64], rhs=zab[:, :], start=True, stop=True)
            kb.act("activation", out=av[:, h, :], in_=sc[:, :], func=AF.Sigmoid, bias=a0[:, h:h + 1], scale=1.0)
        kb.dve("tensor_scalar", out=ell[:, :, :], in0=sig[:, :, :], scalar1=-0.6065306597126334, scalar2=None, op0=ALU.mult)
        kb.dve("tensor_tensor_scan", out=Gc[:, :, :].rearrange("p h t -> p (h t)"), data0=rmask,
               data1=ell[:, :, :].rearrange("p h t -> p (h t)"), initial=0.0, op0=ALU.mult, op1=ALU.add)
        kb.dve("tensor_tensor", out=kk[:, :, :], in0=ks[:, :, :], in1=k_k.bc(B3), op=ALU.mult)
        kb.dve("tensor_tensor", out=sq[:, :, :], in0=kk[:, :, :], in1=kk[:, :, :], op=ALU.mult)
        for h in range(4):
            kb.pe("matmul", sc[:, :], lhsT=ones, rhs=sq[:, h, :], start=True, stop=True)
            kb.act("activation", out=lnss[:, :], in_=sc[:, :], func=AF.Ln)
            kb.act("activation", out=rn[:, h, :], in_=lnss[:, :], func=AF.Exp, scale=-0.5)
        kb.dve("tensor_tensor", out=kap[:, :, :], in0=kk[:, :, :], in1=rn[:, :, :], op=ALU.mult)
        kb.dve("tensor_tensor", out=t1[:, :, :], in0=av[:, :, :], in1=k_a.bc(B3), op=ALU.mult)
        kb.dve("tensor_tensor", out=t1[:, :, :], in0=t1[:, :, :], in1=omka[:, :].bc(B3), op=ALU.add)
        kb.dve("tensor_tensor", out=kp[:, :, :], in0=ks[:, :, :], in1=t1[:, :, :], op=ALU.mult)
        kb.dve("tensor_tensor", out=bet[:, :, :], in0=kap[:, :, :], in1=av[:, :, :], op=ALU.mult)
        kb.pool("tensor_tensor", out=pd[:, :, :], in0=rs[:, :, :], in1=kp[:, :, :], op=ALU.mult)
        kb.pool("tensor_tensor", out=pd[:, :, :], in0=pd[:, :, :], in1=r_k.bc(B3), op=ALU.mult)
        fm = FM[i]
        kb.pool("tensor_copy", out=fm[:, 3, :, :], in_=f32v(pd))
        G3 = f32v(Gc)
        kr = KR[i]
        kb.act("activation", out=eG[:, :, :], in_=Gc[:, :, :], func=AF.Exp)
        kb.dve("tensor_tensor", out=kr[:, :, 1, :], in0=f32v(rs), in1=f32v(eG), op=ALU.mult)
        kb.act("activation", out=enG[:, :, :], in_=Gc[:, :, :], func=AF.Exp, scale=-1.0)
        kb.dve("tensor_tensor", out=KT[i][:, :, :], in0=f32v(kp), in1=f32v(enG), op=ALU.mult)
        kb.dve("tensor_tensor", out=BT[i][:, :, :], in0=f32v(bet), in1=f32v(enG), op=ALU.mult)
        kb.dve("tensor_tensor", out=t1[:, :, :], in0=Gc[:, :, :], in1=ell[:, :, :], op=ALU.subtract)
        kb.act("activation", out=eGm[:, :, :], in_=t1[:, :, :], func=AF.Exp)
        kb.dve("tensor_tensor", out=kr[:, :, 0, :], in0=f32v(kap), in1=f32v(eGm), op=ALU.mult)
        kb.dve("tensor_tensor", out=f32v(sq), in0=G3[:, :, 63:64].to_broadcast([64, HN, 64]), in1=G3, op=ALU.subtract)
        kb.act("activation", out=edG[:, :, :], in_=sq[:, :, :], func=AF.Exp)
        kb.act("activation", out=gC[i][:, :], in_=G3[:, :, 63:64].rearrange("p a c -> p (a c)"), func=AF.Exp)
        kb.pool("tensor_copy", out=fm[:, 0, :, :], in_=f32v(vs))
        kb.pool("tensor_tensor", out=fm[:, 1, :, :], in0=f32v(kp), in1=f32v(edG), op=ALU.mult)
        kb.dve("scalar_tensor_tensor", out=fm[:, 2, :, :], in0=f32v(bet), scalar=-1.0, in1=f32v(edG), op0=ALU.mult, op1=ALU.mult)

        for n in range(NCH):
            for blk in range(4):
                for h in range(4):
                    kb.pe("transpose", out=tp[:, blk * 4 + h, :], in_=fm[:, blk, h * NCH + n, :], identity=identb[:, :])
            kb.act("copy", out=TM[n][:, :, :], in_=tp[:, :, :])
            pB_, pK_, pL_ = (pB, pK, pL) if n % 2 == 0 else (pB2, pK2, pL2)
            for h in range(4):
                hn = h * NCH + n
                krhs = kr[:, hn, :, :].rearrange("p a c -> p (a c)")
                kb.pe("matmul", pB_[:, h, :], lhsT=BT[i][:, hn, :], rhs=krhs, start=True, stop=True)
                kb.pe("matmul", pK_[:, h, :], lhsT=KT[i][:, hn, :], rhs=krhs, start=True, stop=True)
                kb.pe("matmul", pL_[:, h, :], lhsT=kr[:, hn, 0, :], rhs=BT[i][:, hn, :], start=True, stop=True)
            kb.dve("tensor_tensor", out=Nf[n][0][:, :, :], in0=pB_[:, :, 0:64], in1=Mb[:, :, 0:64], op=ALU.mult)
            kb.dve("tensor_tensor", out=NArb[n][:, :, :], in0=pB_[:, :, 64:128], in1=Mb[:, :, 64:128], op=ALU.mult)
            kb.dve("tensor_tensor", out=AK[n][:, :, :], in0=pK_[:, :, :], in1=Mk, op=ALU.mult)
            kb.dve("tensor_tensor", out=Lf[n][0][:, :, :], in0=pL_[:, :, :], in1=Ml, op=ALU.mult)
            kb.pool("tensor_tensor", out=Qm[n][:, :, :], in0=I4, in1=Nf[n][0][:, :, :], op=ALU.subtract)
        for j in range(5):
            for n in range(NCH):
                Nc, Lc = Nf[n][j % 2], Lf[n][j % 2]
                Ln, Nn = Lf[n][(j + 1) % 2], Nf[n][(j + 1) % 2]
                for h in range(4):
                    kb.pe("matmul", psNLn[n][:, h, :], lhsT=Nc[:, h, :], rhs=Lc[:, h, :], start=True, stop=True)
                kb.act("copy", out=Ln[:, :, :], in_=psNLn[n][:, :, :])
                if j < 4:
                    for h in range(4):
                        kb.pe("matmul", psNLn[n][:, h, :], lhsT=Lc[:, h, :], rhs=Nc[:, h, :], start=True, stop=True)
                    kb.dve("tensor_copy", out=Nn[:, :, :], in_=psNLn[n][:, :, :])
            for n in range(NCH):
                Ln = Lf[n][(j + 1) % 2]
                for h in range(4):
                    kb.pe("matmul", psQn[n][:, h, :], lhsT=Ln[:, h, :], rhs=Qm[n][:, h, :], start=True, stop=True)
                kb.dve("tensor_tensor", out=Qm[n][:, :, :], in0=Qm[n][:, :, :], in1=psQn[n][:, :, :], op=ALU.add)
        for n in range(NCH):
            ci = n
            tm = TM[n]
            for h in range(4):
                hn = h * NCH + n
                kb.pe("matmul", pW[:, h, :], lhsT=kr[:, hn, 0, :], rhs=Sb[:, h, :], start=True, stop=False)
                kb.pe("matmul", pW[:, h, :], lhsT=AK[ci][:, h, 0:64], rhs=tm[:, h, :], start=False, stop=True)
            kb.act("copy", out=Wb[:, :, :], in_=pW[:, :, :])
            for h in range(4):
                kb.pe("matmul", pU[:, h, :], lhsT=TT[ci][:, h, :], rhs=Wb[:, h, :], start=True, stop=True)
            kb.dve("tensor_copy", out=Ub[:, :, :], in_=pU[:, :, :])
            for h in range(4):
                kb.pe("matmul", pdS[:, h, :], lhsT=tm[:, 4 + h, :], rhs=tm[:, h, :], start=True, stop=False)
                kb.pe("matmul", pdS[:, h, :], lhsT=tm[:, 8 + h, :], rhs=Ub[:, h, :], start=False, stop=True)
            for h in range(4):
                hn = h * NCH + n
                kb.pe("matmul", pO[:, h, :], lhsT=kr[:, hn, 1, :], rhs=Sb[:, h, :], start=True, stop=False)
                kb.pe("matmul", pO[:, h, :], lhsT=AK[ci][:, h, 64:128], rhs=tm[:, h, :], start=False, stop=False)
                kb.pe("matmul", pO[:, h, :], lhsT=NArb[ci][:, h, :], rhs=Ub[:, h, :], start=False, stop=True)
            gcv = gC[i][:, :].rearrange("p (h n) -> p h n", h=4)[:, :, n:n + 1].to_broadcast([64, 4, 64])
            kb.dve("tensor_tensor", out=S[:, :, :], in0=S[:, :, :], in1=gcv, op=ALU.mult)
            kb.dve("tensor_tensor", out=S[:, :, :], in0=S[:, :, :], in1=pdS[:, :, :], op=ALU.add)
            kb.act("copy", out=Sb[:, :, :], in_=S[:, :, :])
            kb.act("copy", out=osbn[n][:, :, :], in_=pO[:, :, :])
        for n in range(NCH):
            tm = TM[n]
            kb.pe("matmul", psG[:, :], lhsT=sgb[i][:, n * 64:(n + 1) * 64], rhs=g2b[:, :], start=True, stop=True)
            kb.pool("tensor_tensor", out=osq[:, :, :], in0=osbn[n][:, :, :], in1=osbn[n][:, :, :], op=ALU.mult)
            kb.dve("tensor_reduce", out=st[:, 0:4], in_=osbn[n][:, :, :], axis=AX.X, op=ALU.add)
            kb.dve("tensor_reduce", out=st[:, 4:8], in_=osq[:, :, :], axis=AX.X, op=ALU.add)
            kb.dve("tensor_reduce", out=st[:, 20:24], in_=tm[:, 12:16, :], axis=AX.X, op=ALU.add)
            kb.dve("tensor_scalar", out=st[:, 0:4], in0=st[:, 0:4], scalar1=1.0 / 64, scalar2=None, op0=ALU.mult)
            kb.dve("tensor_tensor", out=st[:, 8:12], in0=st[:, 0:4], in1=st[:, 0:4], op=ALU.mult)
            kb.dve("scalar_tensor_tensor", out=st[:, 12:16], in0=st[:, 4:8], scalar=1.0 / 64, in1=st[:, 8:12], op0=ALU.mult, op1=ALU.subtract)
            kb.dve("tensor_scalar", out=st[:, 12:16], in0=st[:, 12:16], scalar1=GN_EPS, scalar2=None, op0=ALU.add)
            kb.act("activation", out=st[:, 16:20], in_=st[:, 12:16], func=AF.Sqrt)
            kb.dve("reciprocal", out=st[:, 16:20], in_=st[:, 16:20])
            S3 = [64, 4, 64]
            kb.dve("tensor_tensor", out=on[:, :, :], in0=osbn[n][:, :, :], in1=st[:, 0:4].bc(S3), op=ALU.subtract)
            kb.dve("tensor_tensor", out=on[:, :, :], in0=on[:, :, :], in1=st[:, 16:20].bc(S3), op=ALU.mult)
            kb.pool("tensor_tensor", out=on[:, :, :], in0=on[:, :, :], in1=gng[:, :].rearrange("p (h c) -> p h c", h=4), op=ALU.mult)
            kb.pool("tensor_tensor", out=on[:, :, :], in0=on[:, :, :], in1=gnb[:, :].rearrange("p (h c) -> p h c", h=4), op=ALU.add)
            kb.pool("tensor_tensor", out=bon[:, :, :], in0=tm[:, 0:4, :], in1=st[:, 20:24].bc(S3), op=ALU.mult)
            kb.pool("tensor_tensor", out=on[:, :, :], in0=on[:, :, :], in1=bon[:, :, :], op=ALU.add)
            kb.dve("tensor_tensor", out=OUT[i][:, n, :], in0=on[:, :, :].rearrange("p h c -> p (h c)"), in1=psG[:, :], op=ALU.mult)
        for n in range(NCH):
            for j in range(2):
                kb.pe("transpose", out=tpo[:, j, :], in_=OUT[i][:, n, j * 128:(j + 1) * 128], identity=cm[:, 1280:1344])
            kb.act("copy", out=OT[i][:, :, n * 64:(n + 1) * 64], in_=tpo[:, :, :])
        kb.dma(orT[sl0:sl1, t0:t0 + TS].rearrange("(j p) t -> p j t", p=128), OT[i][:, :, :], q="pool")


def stage_k3a(kb, T, l, xTd, onTd, orTd, ZT, D, x2Td):
    nc, es = kb.nc, kb.es
    stage = 9
    NT = T
    W = 512
    ntile = NT // W
    zgTd = ZT[3096:5144, :]
    pnd, prd, wod, xqd, xkd, xvd, xod = D["p_nsa"][l], D["p_rwkv"][l], D["w_out"][l], D["xq"][l], D["xk"][l], D["xv"][l], D["xo"][l]
    memTd, lnd = D["memT"], D["ln12"][l]
    stg = Stager(kb, 2048, 2)
    pn = kb.sb([128, 4, 1024], BF16, "pn"); prw = kb.sb([128, 4, 1024], BF16, "prw")
    wo = kb.sb([128, 8, 1024], BF16, "wo"); xq = kb.sb([128, 8, 512], BF16, "xq_")
    xk = kb.sb([128, 8, 512], BF16, "xk_"); xv = kb.sb([128, 8, 512], BF16, "xv_")
    xo = kb.sb([128, 4, 1024], BF16, "xo_"); memb = kb.sb([128, 8, 256], BF16, "memb")
    for (dst, srcd, nk, ncol) in ((pn, pnd, 4, 1024), (prw, prd, 4, 1024), (wo, wod, 8, 1024), (xq, xqd, 8, 512), (xk, xkd, 8, 512),
                                  (xv, xvd, 8, 512), (xo, xod, 4, 1024), (memb, memTd, 8, 256)):
        src = srcd.rearrange("(k p) c -> p k c", p=128)
        kstep = 2048 // ncol
        for k0 in range(0, nk, kstep):
            kw = min(kstep, nk - k0)
            stg.load_cast(dst[:, k0:k0 + kw, :], src[:, k0:k0 + kw, :])
    ln = kb.sb([128, 32], F32, "ln_"); kb.dma(ln[:, :], lnd[:, :])
    onesf = kb.sb([128, 128], F32, "onesf"); kb.dve("memset", onesf[:, :], 1.0 / 1024)
    onesb = kb.sb([128, 128], BF16, "onesb"); kb.dve("memset", onesb[:, :], 1.0)

    P = [kb.ps([128, 512], F32, f"bank{i}") for i in range(8)]
    pA, pB_, pY, pM, pV, pQ, pO, pD = P
    KT = kb.sb([128, 4, 256], BF16, "KT"); Vm = kb.sb([128, 2, 512], BF16, "Vm")
    for h in range(4):
        for k in range(8):
            kb.pe("matmul", pQ[:, 0:256], lhsT=xk[:, k, h * 128:(h + 1) * 128], rhs=memb[:, k, :], start=(k == 0), stop=(k == 7))
        kb.act("copy", out=KT[:, h, :], in_=pQ[:, 0:256])
    for mc in range(2):
        for k in range(8):
            kb.pe("matmul", pO[:, :], lhsT=memb[:, k, mc * 128:(mc + 1) * 128], rhs=xv[:, k, :], start=(k == 0), stop=(k == 7))
        kb.act("copy", out=Vm[:, mc, :], in_=pO[:, :])

    xt = kb.sb([128, 8, W], F32, "xt"); vv = kb.sb([128, 8, W], F32, "vv")
    onb = kb.sb([128, 4, W], BF16, "onb"); orb = kb.sb([128, 4, W], BF16, "orb")
    zgn = [kb.sb([128, W], F32, f"zgn{i}") for i in range(2)]; zgr = [kb.sb([128, W], F32, f"zgr{i}") for i in range(2)]
    m1 = kb.sb([128, W], F32, "m1"); m2 = kb.sb([128, W], F32, "m2")
    mT = kb.sb([128, 8, W], BF16, "mT"); x1b = kb.sb([128, 8, W], BF16, "x1b")
    qTb = kb.sb([128, 4, W], BF16, "qTb"); PTm = kb.sb([128, 2, W], BF16, "PTm")
    rden = kb.sb([128, W], F32, "rden"); oTn = kb.sb([128, 4, W], BF16, "oTn")
    tmp = {"sq": [kb.sb([128, W], F32, f"sq{i}") for i in range(2)], "mean": kb.sb([128, W], F32, "mean"),
           "msq": kb.sb([128, W], F32, "msq"), "rstd": kb.sb([128, W], F32, "rstd")}

    for tt in range(ntile if stage >= 1 else 0):
        c0, c1 = tt * W, (tt + 1) * W
        for k in range(8):
            kb.dma(xt[:, k, :], xTd[k * 128:(k + 1) * 128, c0:c1])
        stg.load_cast(onb[:, :, :], onTd.rearrange("(k p) t -> p k t", p=128)[:, :, c0:c1])
        stg.load_cast(orb[:, :, :], orTd.rearrange("(k p) t -> p k t", p=128)[:, :, c0:c1])
        for dc in range(8):
            gn, gr = zgn[dc % 2], zgr[dc % 2]
            kb.dma(gn[:, :], zgTd[dc * 128:(dc + 1) * 128, c0:c1])
            kb.dma(gr[:, :], zgTd[1024 + dc * 128:1024 + (dc + 1) * 128, c0:c1])
            kb.act("activation", out=gn[:, :], in_=gn[:, :], func=AF.Sigmoid)
            kb.act("activation", out=gr[:, :], in_=gr[:, :], func=AF.Sigmoid)
            for kc in range(4):
                kb.pe("matmul", pA[:, :], lhsT=pn[:, kc, dc * 128:(dc + 1) * 128], rhs=onb[:, kc, :], start=(kc == 0), stop=(kc == 3))
            for kc in range(4):
                kb.pe("matmul", pB_[:, :], lhsT=prw[:, kc, dc * 128:(dc + 1) * 128], rhs=orb[:, kc, :], start=(kc == 0), stop=(kc == 3))
            kb.dve("tensor_tensor", out=m1[:, :], in0=pA[:, :], in1=gn[:, :], op=ALU.mult)
            kb.dve("tensor_tensor", out=m2[:, :], in0=pB_[:, :], in1=gr[:, :], op=ALU.mult)
            kb.pool("tensor_tensor", out=mT[:, dc, :], in0=m1[:, :], in1=m2[:, :], op=ALU.add)
        for dc in range(8):
            for k in range(8):
                kb.pe("matmul", pY[:, :], lhsT=wo[:, k, dc * 128:(dc + 1) * 128], rhs=mT[:, k, :], start=(k == 0), stop=(k == 7))
            kb.dve("scalar_tensor_tensor", out=vv[:, dc, :], in0=xt[:, dc, :], scalar=DN_ALPHA, in1=pY[:, :], op0=ALU.mult, op1=ALU.add)
        if stage < 2: continue
        layer_norm_fm(kb, vv, ln[:, 0:16], None, W, pM, pV, tmp, onesf, x1b=x1b)
        if stage < 3: continue
        for h in range(4):
            for k in range(8):
                kb.pe("matmul", pQ[:, :], lhsT=xq[:, k, h * 128:(h + 1) * 128], rhs=x1b[:, k, :], start=(k == 0), stop=(k == 7))
            kb.act("mul", out=qTb[:, h, :], in_=pQ[:, :], mul=128 ** -0.5)
        if stage < 4: continue
        for h in range(4):
            for mc in range(2 if stage >= 4 else 0):
                kb.pe("matmul", pA[:, :] if mc == 0 else pB_[:, :], lhsT=KT[:, h, mc * 128:(mc + 1) * 128], rhs=qTb[:, h, :], start=True, stop=True)
                kb.act("activation", out=PTm[:, mc, :], in_=(pA if mc == 0 else pB_)[:, :], func=AF.Exp)
            if stage < 5: continue
            for mc in range(2):
                kb.pe("matmul", pO[:, :], lhsT=Vm[:, mc, h * 128:(h + 1) * 128], rhs=PTm[:, mc, :], start=(mc == 0), stop=(mc == 1))
            for mc in range(2):
                kb.pe("matmul", pD[:, :], lhsT=onesb[:, :], rhs=PTm[:, mc, :], start=(mc == 0), stop=(mc == 1))
            kb.dve("reciprocal", out=rden[:, :], in_=pD[:, :])
            kb.dve("tensor_tensor", out=oTn[:, h, :], in0=pO[:, :], in1=rden[:, :], op=ALU.mult)
        if stage < 6: continue
        for dc in range(8):
            for h in range(4):
                kb.pe("matmul", pY[:, :], lhsT=xo[:, h, dc * 128:(dc + 1) * 128], rhs=oTn[:, h, :], start=(h == 0), stop=(h == 3))
            kb.dve("scalar_tensor_tensor", out=vv[:, dc, :], in0=vv[:, dc, :], scalar=DN_ALPHA, in1=pY[:, :], op0=ALU.mult, op1=ALU.add)
        layer_norm_fm(kb, vv, ln[:, 16:32], (c0, c1), W, pM, pV, tmp, onesf, out=x2Td)


def stage_k3b(kb, T, l, x2Td, D, x3Td):
    nc, es = kb.nc, kb.es
    NT = T
    HT = min(2048, NT)
    nhalf = NT // HT
    W = 512
    WL = 256
    rwd, rbd, wgd, wud, wdd, lnd, ohd, idd = D["rw"], D["rb"], D["wg"][l], D["wu"][l], D["wd"][l], D["ln3"][l], D["oh16"], D["identf"]
    stg = Stager(kb, 1024, 2)
    rw = kb.sb([128, 8, 16], F32, "rw_"); kb.dma(rw[:, :, :], rwd.rearrange("(k p) e -> p k e", p=128))
    rb = kb.sb([128, 16], F32, "rb_"); kb.dma(rb[:, :], rbd[0:1, :].partition_broadcast(128))
    ln = kb.sb([128, 16], F32, "ln_"); kb.dma(ln[:, :], lnd[:, :])
    oh16 = kb.sb([16, 16, 128], F32, "oh16_"); kb.dma(oh16[:, :, :], ohd.rearrange("r (e c) -> r e c", c=128))
    identf = kb.sb([128, 128], F32, "identf_"); kb.dma(identf[:, :], idd[:, :])
    onesf = kb.sb([128, 128], F32, "onesf"); kb.dve("memset", onesf[:, :], 1.0 / 1024)

    P = [kb.ps([128, 512], F32, f"bank{i}") for i in range(8)]
    pR, pGb, pHg0, pHu0, pHg1, pHu1, pY0, pY1 = P

    x2b = kb.sb([128, 8, HT], BF16, "x2b"); yacc = kb.sb([128, 8, HT], F32, "yacc")
    gateT = kb.sb([16, HT], F32, "gateT")
    wgb = [kb.sb([128, 8, 512], BF16, f"wgb{i}") for i in range(2)]
    wub = [kb.sb([128, 8, 512], BF16, f"wub{i}") for i in range(2)]
    wdb = [kb.sb([128, 4, 1024], BF16, f"wdb{i}") for i in range(2)]
    hT = kb.sb([128, 4, W], BF16, "hT")
    sg = [kb.sb([128, W], F32, f"sg{i}") for i in range(2)]; hu = [kb.sb([128, W], F32, f"hu{i}") for i in range(2)]
    gbs = kb.sb([128, W], F32, "gbs")
    xr = [kb.sb([128, 8, 128], F32, f"xr{i}") for i in range(2)]
    r = {n: kb.sb([128, 16], F32, "r_" + n) for n in ("s", "ssel", "eq", "s2", "msel", "oh1", "oh2", "st", "gate", "t16")}
    q = {n: kb.sb([128, 4], F32, "q_" + n) for n in ("m1", "m2", "gsc", "ing", "t")}
    c = {n: kb.sb([128, 1], F32, "c_" + n) for n in ("gmax", "e1", "e2", "ssum")}
    xt = kb.sb([128, 8, WL], F32, "xt")
    tmp = {"sq": [kb.sb([128, WL], F32, f"sq{i}") for i in range(2)], "mean": kb.sb([128, WL], F32, "mean"),
           "msq": kb.sb([128, WL], F32, "msq"), "rstd": kb.sb([128, WL], F32, "rstd")}
    G44 = [128, 4, 4]
    v44 = lambda b: b[:, :].rearrange("p (g e) -> p g e", e=4)

    for hf in range(nhalf):
        h0 = hf * HT
        for k in range(8):
            cs_ = min(1024, HT)
            for c0 in range(0, HT, cs_):
                stg.load_cast(x2b[:, k, c0:c0 + cs_], x2Td[k * 128:(k + 1) * 128, h0 + c0:h0 + c0 + cs_])
        for rt in range(HT // 128):
            xrt = xr[rt % 2]
            kb.dma(xrt[:, :, :], x2Td.rearrange("(k p) t -> p k t", p=128)[:, :, h0 + rt * 128:h0 + (rt + 1) * 128])
            for k in range(8):
                kb.pe("matmul", pR[:, 0:16], lhsT=xrt[:, k, :], rhs=rw[:, k, :], start=(k == 0), stop=(k == 7))
            kb.act("activation", out=r["s"][:, :], in_=pR[:, 0:16], func=AF.Sigmoid)
            kb.dve("tensor_tensor", out=r["ssel"][:, :], in0=r["s"][:, :], in1=rb[:, :], op=ALU.add)
            kb.dve("tensor_reduce", out=q["m1"][:, :], in_=v44(r["ssel"]), axis=AX.X, op=ALU.max)
            kb.dve("tensor_tensor", out=v44(r["eq"]), in0=v44(r["ssel"]), in1=q["m1"][:, :].bc(G44), op=ALU.is_equal)
            kb.dve("scalar_tensor_tensor", out=r["s2"][:, :], in0=r["eq"][:, :], scalar=-1e9, in1=r["ssel"][:, :], op0=ALU.mult, op1=ALU.add)
            kb.dve("tensor_reduce", out=q["m2"][:, :], in_=v44(r["s2"]), axis=AX.X, op=ALU.max)
            kb.dve("tensor_tensor", out=q["gsc"][:, :], in0=q["m1"][:, :], in1=q["m2"][:, :], op=ALU.add)
            kb.dve("tensor_reduce", out=c["gmax"][:, :], in_=q["gsc"][:, :], axis=AX.X, op=ALU.max)
            kb.dve("tensor_scalar", out=q["ing"][:, :], in0=q["gsc"][:, :], scalar1=c["gmax"][:, 0:1], scalar2=None, op0=ALU.is_equal)
            kb.dve("tensor_scalar", out=q["t"][:, :], in0=q["ing"][:, :], scalar1=1e9, scalar2=-1e9, op0=ALU.mult, op1=ALU.add)
            kb.dve("tensor_tensor", out=v44(r["msel"]), in0=v44(r["ssel"]), in1=q["ing"][:, :].bc(G44), op=ALU.mult)
            kb.dve("tensor_tensor", out=v44(r["msel"]), in0=v44(r["msel"]), in1=q["t"][:, :].bc(G44), op=ALU.add)
            kb.dve("tensor_reduce", out=c["e1"][:, :], in_=r["msel"][:, :], axis=AX.X, op=ALU.max)
            kb.dve("tensor_scalar", out=r["oh1"][:, :], in0=r["msel"][:, :], scalar1=c["e1"][:, 0:1], scalar2=None, op0=ALU.is_equal)
            kb.dve("scalar_tensor_tensor", out=r["s2"][:, :], in0=r["oh1"][:, :], scalar=-2e9, in1=r["msel"][:, :], op0=ALU.mult, op1=ALU.add)
            kb.dve("tensor_reduce", out=c["e2"][:, :], in_=r["s2"][:, :], axis=AX.X, op=ALU.max)
            kb.dve("tensor_scalar", out=r["oh2"][:, :], in0=r["s2"][:, :], scalar1=c["e2"][:, 0:1], scalar2=None, op0=ALU.is_equal)
            kb.dve("tensor_tensor", out=r["oh1"][:, :], in0=r["oh1"][:, :], in1=r["oh2"][:, :], op=ALU.add)
            kb.dve("tensor_tensor", out=r["st"][:, :], in0=r["s"][:, :], in1=r["oh1"][:, :], op=ALU.mult)
            kb.dve("tensor_reduce", out=c["ssum"][:, :], in_=r["st"][:, :], axis=AX.X, op=ALU.add)
            kb.dve("reciprocal", out=c["ssum"][:, :], in_=c["ssum"][:, :])
            kb.dve("tensor_scalar", out=r["gate"][:, :], in0=r["st"][:, :], scalar1=c["ssum"][:, 0:1], scalar2=None, op0=ALU.mult)
            kb.pe("transpose", out=pGb[0:16, 0:128], in_=r["gate"][:, :], identity=identf[:, :])
            kb.act("copy", out=gateT[:, rt * 128:(rt + 1) * 128], in_=pGb[0:16, 0:128])
        it = 0
        def load_expert(e_):
            wi_ = e_ % 2
            for k0 in range(0, 8, 2):
                stg.load_cast(wgb[wi_][:, k0:k0 + 2, :], wgd[e_].rearrange("(k p) f -> p k f", p=128)[:, k0:k0 + 2, :])
                stg.load_cast(wub[wi_][:, k0:k0 + 2, :], wud[e_].rearrange("(k p) f -> p k f", p=128)[:, k0:k0 + 2, :])
            for k0 in range(4):
                stg.load_cast(wdb[wi_][:, k0:k0 + 1, :], wdd[e_].rearrange("(k p) d -> p k d", p=128)[:, k0:k0 + 1, :])

        load_expert(0)
        for e in range(16):
            wi = e % 2
            if e + 1 < 16:
                load_expert(e + 1)
            for tt in range(HT // W):
                c0, c1 = tt * W, (tt + 1) * W
                kb.pe("matmul", pGb[:, :], lhsT=oh16[:, e, :], rhs=gateT[:, c0:c1], start=True, stop=True)
                kb.act("copy", out=gbs[:, :], in_=pGb[:, :])
                for fc in range(4):
                    pHg, pHu = (pHg0, pHu0) if it % 2 == 0 else (pHg1, pHu1)
                    sgi, hui = sg[it % 2], hu[it % 2]
                    it += 1
                    for k in range(8):
                        kb.pe("matmul", pHg[:, :], lhsT=wgb[wi][:, k, fc * 128:(fc + 1) * 128], rhs=x2b[:, k, c0:c1], start=(k == 0), stop=(k == 7))
                    for k in range(8):
                        kb.pe("matmul", pHu[:, :], lhsT=wub[wi][:, k, fc * 128:(fc + 1) * 128], rhs=x2b[:, k, c0:c1], start=(k == 0), stop=(k == 7))
                    kb.act("activation", out=sgi[:, :], in_=pHg[:, :], func=AF.Silu)
                    kb.dve("tensor_tensor", out=hui[:, :], in0=pHu[:, :], in1=sgi[:, :], op=ALU.mult)
                    kb.pool("tensor_tensor", out=hT[:, fc, :], in0=hui[:, :], in1=gbs[:, :], op=ALU.mult)
                for dc in range(8):
                    pY = pY0 if dc % 2 == 0 else pY1
                    for fc in range(4):
                        kb.pe("matmul", pY[:, :], lhsT=wdb[wi][:, fc, dc * 128:(dc + 1) * 128], rhs=hT[:, fc, :], start=(fc == 0), stop=(fc == 3))
                    if e == 0:
                        kb.dve("tensor_copy", out=yacc[:, dc, c0:c1], in_=pY[:, :])
                    else:
                        kb.dve("tensor_tensor", out=yacc[:, dc, c0:c1], in0=yacc[:, dc, c0:c1], in1=pY[:, :], op=ALU.add)
        for tt in range(HT // WL):
            c0, c1 = tt * WL, (tt + 1) * WL
            for k in range(8):
                kb.dma(xt[:, k, :], x2Td[k * 128:(k + 1) * 128, h0 + c0:h0 + c1])
            for dc in range(8):
                kb.dve("scalar_tensor_tensor", out=xt[:, dc, :], in0=xt[:, dc, :], scalar=DN_ALPHA, in1=yacc[:, dc, c0:c1], op0=ALU.mult, op1=ALU.add)
            layer_norm_fm(kb, xt, ln[:, 0:16], (h0 + c0, h0 + c1), WL, pHg0, pHu0, tmp, onesf, out=x3Td)


WSPEC = None


def fused_specs(T):
    NJ, NKC = T // 64, T // 128
    NCc = (T // 16 + 127) // 128
    return {
        "xT0": [1024, T], "memT": [1024, 256], "w_in": [2, 1024, 5144], "w_v": [2, 1024, 280],
        "w1k": [2, 2048, 256], "w1v": [2, 2048, 256], "w2k": [2, 256, 64], "w2v": [2, 256, 64], "pek": [2, 2048], "pev": [2, 2048],
        "strips": [2, 4, 128, NCLS * 128], "b31": [2, 128, 4], "ov": [NCc, 128, NJ], "vmva": [128, 4 * NJ], "esel": [NJ, NKC * 128], "ident": [128, 128],
        "pr": [2, 2, 64, 34], "mug": [2, 128, 1], "w2": [2, 64, 512], "a2": [2, 64, 512], "g2": [2, 128, 512], "gn": [2, 2, 512], "cm": [64, NCM],
        "v1": [512, 32], "v2": [32, 512], "pv": [2, 64, 12],
        "p_nsa": [2, 512, 1024], "p_rwkv": [2, 512, 1024], "w_out": [2, 1024, 1024], "xq": [2, 1024, 512], "xk": [2, 1024, 512], "xv": [2, 1024, 512],
        "xo": [2, 512, 1024], "ln12": [2, 128, 32],
        "rw": [1024, 16], "rb": [1, 16], "wg": [2, 16, 1024, 512], "wu": [2, 16, 1024, 512], "wd": [2, 16, 512, 1024], "ln3": [2, 128, 16],
        "oh16": [16, 16 * 128], "identf": [128, 128],
    }


def build_fused(T, nlayers=2, upto=99):
    nc = bass.Bass("TRN2", target_bir_lowering=False)
    es0 = ExitStack()
    with es0:
        kb = KB(nc, es0)
        kb.pfx = ""
        D = {k: kb.dram(k, shp, F32, kind="ExternalInput") for k, shp in fused_specs(T).items()}
        outT = kb.dram("outT", [1024, T], F32, kind="ExternalOutput")
        ZT = kb.dram("ZT", [41 * 128, T], F32)
        ZV = kb.dram("ZV", [T, 280], F32)
        onT = kb.dram("onT", [512, T], F32)
        orT = kb.dram("orT", [512, T], F32)
        vfT = kb.dram("vfT", [512, T], F32)
        x2T = kb.dram("x2T", [1024, T], F32)
        xT1 = kb.dram("xT1", [1024, T], F32)
        sid = [0]

        def run_stage(fn, *a):
            sid[0] += 1
            if sid[0] > upto:
                return
            with ExitStack() as es2:
                kb.es = es2
                kb.pfx = f"s{sid[0]}_"
                kb.barrier()
                fn(kb, T, *a)
                kb.barrier()
                kb.flush()

        for l in range(nlayers):
            xsrc = D["xT0"] if l == 0 else xT1
            xdst = outT if l == nlayers - 1 else xT1
            run_stage(stage_k1, l, xsrc, D, ZT, ZV)
            for g in range(2):
                run_stage(stage_k2a, g, l, ZT, ZV, D, onT)
            for g in range(2):
                run_stage(stage_k2b, g, l, ZT, D, orT, vfT)
            run_stage(stage_k3a, l, xsrc, onT, orT, ZT, D, x2T)
            run_stage(stage_k3b, l, x2T, D, xdst)
    return nc


def _c(a):
    return np.ascontiguousarray(a, dtype=np.float32)


def _lnpack(g, b):
    return np.concatenate([g.reshape(8, 128).T, b.reshape(8, 128).T], 1)


def fused_inputs(inp, b, T):
    L = 2
    d = {"xT0": _c(inp["x"][b, :T].T), "memT": _c(inp["mem"][b].T), "w_in": _c(inp["w_in"])}
    cols = []
    for g in range(2):
        cols += list(range(896 + g * 64, 896 + (g + 1) * 64)) + list(range(1152 + g * 64, 1152 + (g + 1) * 64)) + list(range(1280 + g * 12, 1280 + (g + 1) * 12))
    d["w_v"] = _c(inp["w_in"][:, :, cols])
    d.update({"w1k": _c(inp["cmp_w1k"]), "w1v": _c(inp["cmp_w1v"]), "w2k": _c(inp["cmp_w2k"]), "w2v": _c(inp["cmp_w2v"]),
              "pek": _c(inp["cmp_pe_k"].reshape(L, -1)), "pev": _c(inp["cmp_pe_v"].reshape(L, -1))})
    n0, n1 = nsa_consts(T, inp["rel_table"], 0), nsa_consts(T, inp["rel_table"], 1)
    d["strips"] = _c(np.stack([n0["strips"], n1["strips"]]))
    tb = inp["rel_table"].reshape(32, 2, 4)[31]
    d["b31"] = _c(np.broadcast_to(tb[:, None, :], (2, 128, 4)))
    for k in ("ov", "vmva", "esel", "ident"):
        d[k] = n0[k]
    pr = np.zeros((L, 2, 64, 34), np.float32)
    for l in range(L):
        mu = inp["rwkv_mu"][l]
        plist = [mu[0:512], mu[512:1024], mu[1024:1536], inp["rwkv_w0"][l], inp["rwkv_a0"][l], inp["rwkv_kk"][l], inp["rwkv_ka"][l], inp["rwkv_rk"][l].reshape(-1)]
        for g in range(2):
            for p_, v in enumerate(plist):
                pr[l, g, :, 4 * p_:4 * p_ + 4] = v[g * 256:(g + 1) * 256].reshape(4, 64).T
            pr[l, g, :, 32] = mu[1536:1600]
            pr[l, g, :, 33] = mu[1600:1664]
    d["pr"] = pr
    d["mug"] = _c(inp["rwkv_mu"][:, 1664:1792].reshape(L, 128, 1))
    d["w2"], d["a2"], d["g2"] = _c(inp["rwkv_w2"]), _c(inp["rwkv_a2"]), _c(inp["rwkv_g2"])
    d["gn"] = _c(np.stack([inp["rwkv_gn_g"], inp["rwkv_gn_b"]], axis=1))
    d["cm"] = rwkv_consts()
    d["v1"], d["v2"] = _c(inp["rwkv_v1"][0]), _c(inp["rwkv_v2"][0])
    pv = np.zeros((2, 64, 12), np.float32)
    muv = inp["rwkv_mu"][1][1024:1536]
    for g in range(2):
        perm = np.concatenate([np.arange(g * 256, (g + 1) * 256), np.arange((1 - g) * 256, (2 - g) * 256)])
        pv[g, :, 0:8] = muv[perm].reshape(8, 64).T
        pv[g, :, 8:12] = inp["rwkv_v0"][0][g * 256:(g + 1) * 256].reshape(4, 64).T
    d["pv"] = pv
    d.update({"p_nsa": _c(inp["p_nsa"]), "p_rwkv": _c(inp["p_rwkv"]), "w_out": _c(inp["w_out"]), "xq": _c(inp["xq_w"]), "xk": _c(inp["xk_w"]),
              "xv": _c(inp["xv_w"]), "xo": _c(inp["xo_w"])})
    d["ln12"] = _c(np.stack([np.concatenate([_lnpack(inp["ln1_g"][l], inp["ln1_b"][l]), _lnpack(inp["ln2_g"][l], inp["ln2_b"][l])], 1) for l in range(L)]))
    d.update({"rw": _c(inp["router_w"]), "rb": _c(inp["router_bias"].reshape(1, 16)), "wg": _c(inp["moe_w_gate"]), "wu": _c(inp["moe_w_up"]),
              "wd": _c(inp["moe_w_down"])})
    d["ln3"] = _c(np.stack([_lnpack(inp["ln3_g"][l], inp["ln3_b"][l]) for l in range(L)]))
    d.update(moe_consts())
    return d


_PROG = {}


def kernel(**inp):
    inp = {k: np.asarray(v) for k, v in inp.items()}
    B, T, Dm = inp["x"].shape
    if T not in _PROG:
        _PROG[T] = build_fused(T)
    per_b = [fused_inputs(inp, b, T) for b in range(B)]
    maps = [per_b[c // 2] for c in range(8)]
    res = run_bass_kernel_spmd(_PROG[T], maps, core_ids=list(range(8))).results
    out = np.zeros((B, T, Dm), np.float32)
    half = T // 2
    for c in range(8):
        b, h = c // 2, c % 2
        out[b, h * half:(h + 1) * half] = res[c]["outT"][:, h * half:(h + 1) * half].T
    return out
```

```python
import numpy as np
from contextlib import ExitStack
import concourse.bass as bass
import concourse.mybir as mybir
from concourse.bass_utils import run_bass_kernel_spmd

F32 = mybir.dt.float32
BF16 = mybir.dt.bfloat16
AF = mybir.ActivationFunctionType
ALU = mybir.AluOpType
AX = mybir.AxisListType

ENGS = ("pe", "act", "dve", "pool", "sp")
NDS = 20


class Buf:
    def __init__(self, h, name):
        self.h = h
        self.name = name
        self.w = None
        self.r = {}

    def __getitem__(self, idx):
        return V(self, self.h[idx])

    def ap(self):
        return V(self, self.h[:])

    def rearrange(self, *a, **k):
        return self.ap().rearrange(*a, **k)


class V:
    def __init__(self, buf, ap):
        self.buf = buf
        self.ap = ap

    def __getitem__(self, idx):
        return V(self.buf, self.ap[idx])

    def rearrange(self, *a, **k):
        return V(self.buf, self.ap.rearrange(*a, **k))

    def bitcast(self, dt):
        return V(self.buf, self.ap.bitcast(dt))

    def to_broadcast(self, shape):
        return V(self.buf, self.ap.to_broadcast(list(shape)))

    def unsqueeze(self, ax):
        return V(self.buf, self.ap.unsqueeze(ax))

    def partition_broadcast(self, n):
        return V(self.buf, self.ap.partition_broadcast(n))

    def bc(self, shape):
        ap = self.ap
        while len(ap.shape) < len(shape):
            ap = ap.unsqueeze(len(ap.shape))
        return V(self.buf, ap.to_broadcast(list(shape)))

    @property
    def shape(self):
        return self.ap.shape


WRITE_KW = ("out", "accum_out", "out_max", "out_indices")


class KB:
    def __init__(self, nc, es):
        self.nc = nc
        self.es = es
        self.q = {e: [] for e in ENGS}
        self.cnt = {e: 0 for e in ENGS}
        self.sem = {}
        for e in ENGS:
            if e == "sp":
                continue
            self.sem[e] = es.enter_context(nc.semaphore("s_" + e))
        self.dsem = {}
        self.dval = {}
        self.dn = {}
        for e in ("sp", "pool", "act"):
            self.dsem[e] = [es.enter_context(nc.semaphore(f"d_{e}{i}")) for i in range(NDS)]
            self.dval[e] = [0] * NDS
            self.dn[e] = 0
        self.seen = {e: {} for e in ENGS}
        self.semobj = {}
        self.nbuf = 0

    def sb(self, shape, dt=F32, name=None):
        self.nbuf += 1
        name = name or f"t{self.nbuf}"
        h = self.es.enter_context(self.nc.sbuf_tensor("sb_" + getattr(self, "pfx", "") + name, list(shape), dt))
        return Buf(h, name)

    def ps(self, shape, dt=F32, name=None):
        self.nbuf += 1
        name = name or f"p{self.nbuf}"
        h = self.es.enter_context(self.nc.psum_tensor(getattr(self, "pfx", "") + name, list(shape), dt))
        return Buf(h, name)

    def pst(self, name, shape, dt=F32):
        return self.es.enter_context(self.nc.psum_tensor(getattr(self, "pfx", "") + name, list(shape), dt))

    def sub(self, view, name, bank=None):
        b = Buf(view.ap if isinstance(view, V) else view, name)
        b.bank = bank
        return b

    def banklock(self, name):
        return Buf(None, name)

    def dram(self, name, shape, dt=F32, kind="Internal"):
        h = self.nc.dram_tensor(name, list(shape), dt, kind=kind)
        return Buf(h.ap(), name)

    def _need(self, eng, tok, waits):
        if tok is None:
            return
        key, val, teng = tok
        if teng == eng and eng in ("pe", "sp"):
            return
        if self.seen[eng].get(key, 0) >= val:
            return
        waits[key] = max(waits.get(key, 0), val)

    def _deps(self, eng, reads, writes, same_ok=False):
        waits = {}
        for b in reads:
            self._need(eng, b.w, waits)
        for b in writes:
            self._need(eng, b.w, waits)
            for key, (val, teng) in b.r.items():
                if teng == eng:
                    continue
                self._need(eng, (key, val, teng), waits)
        for key, val in waits.items():
            self.seen[eng][key] = val
        return [(self.semobj[key], val) for key, val in waits.items()]

    def _mark(self, tok, reads, writes):
        key, val, eng = tok
        for b in reads:
            b.r[key] = (val, eng)
        for b in writes:
            b.w = tok
            b.r = {}

    def op(self, eng, meth, *args, **kw):
        reads, writes = [], []
        extra_r = kw.pop("_reads", [])
        extra_w = kw.pop("_writes", [])
        a2 = []
        for a in args:
            if isinstance(a, V):
                (writes if not a2 and not writes else reads).append(a.buf)
                a2.append(a.ap)
            else:
                a2.append(a)
        k2 = {}
        for k, v in kw.items():
            if isinstance(v, V):
                (writes if k in WRITE_KW else reads).append(v.buf)
                k2[k] = v.ap
            else:
                k2[k] = v
        reads += [b.buf if isinstance(b, V) else b for b in extra_r]
        writes += [b.buf if isinstance(b, V) else b for b in extra_w]
        waits = self._deps(eng, reads, writes)
        banks = []
        for b in reads + writes:
            bk = getattr(b, "bank", None)
            if bk is not None and bk not in banks:
                banks.append(bk)
        for bk in banks:
            if bk.w is not None and bk.w[2] != eng and self.seen[eng].get(bk.w[0], 0) < bk.w[1]:
                self.seen[eng][bk.w[0]] = bk.w[1]
                waits.append((self.semobj[bk.w[0]], bk.w[1]))
        self.cnt[eng] += 1
        n = self.cnt[eng]
        sem = self.sem[eng]
        key = "c_" + eng
        self.semobj[key] = sem
        self._mark((key, n, eng), reads, writes)
        for bk in banks:
            bk.w = (key, n, eng)

        def emit(e, meth=meth, a2=a2, k2=k2, waits=waits, sem=sem):
            for s, v in waits:
                e.wait_ge(s, v)
            getattr(e, meth)(*a2, **k2).then_inc(sem, 1)

        self.q[eng].append(emit)

    def pe(self, meth, *a, **k):
        self.op("pe", meth, *a, **k)

    def act(self, meth, *a, **k):
        self.op("act", meth, *a, **k)

    def dve(self, meth, *a, **k):
        self.op("dve", meth, *a, **k)

    def pool(self, meth, *a, **k):
        self.op("pool", meth, *a, **k)

    def dma(self, out, in_, q="sp", **kw):
        reads, writes = [in_.buf], [out.buf]
        i = self.dn[q] % NDS
        self.dn[q] += 1
        sem = self.dsem[q][i]
        key = f"d_{q}{i}"
        self.semobj[key] = sem
        waits = self._deps(q, reads, writes)
        prev = self.dval[q][i]
        if prev > 0 and self.seen[q].get(key, 0) < prev:
            waits.append((sem, prev))
            self.seen[q][key] = prev
        self.dval[q][i] = prev + 16
        val = prev + 16
        self._mark((key, val, "dma_" + q), reads, writes)
        oap, iap = out.ap, in_.ap

        def emit(e, waits=waits, sem=sem, oap=oap, iap=iap, kw=kw):
            for s, v in waits:
                e.wait_ge(s, v)
            e.dma_start(out=oap, in_=iap, **kw).then_inc(sem, 16)

        self.q[q].append(emit)

    def wait_all(self, eng, bufs):
        waits = {}
        for b in bufs:
            self._need(eng, b.w, waits)
        ws = [(self.semobj[k], v) for k, v in waits.items()]

        def emit(e, ws=ws):
            for s, v in ws:
                e.wait_ge(s, v)

        self.q[eng].append(emit)

    def barrier(self):
        for x in ENGS:
            ws = []
            for y in ENGS:
                if y == "sp" or y == x:
                    continue
                key = "c_" + y
                if self.cnt[y] > 0 and self.seen[x].get(key, 0) < self.cnt[y]:
                    self.seen[x][key] = self.cnt[y]
                    ws.append((self.sem[y], self.cnt[y]))
            for qn in ("sp", "pool", "act"):
                for i in range(NDS):
                    v = self.dval[qn][i]
                    key = f"d_{qn}{i}"
                    if v > 0 and self.seen[x].get(key, 0) < v:
                        self.seen[x][key] = v
                        ws.append((self.dsem[qn][i], v))

            def emit(e, ws=ws):
                for s_, v in ws:
                    e.wait_ge(s_, v)

            self.q[x].append(emit)

    def flush(self):
        self.finish()
        self.q = {e: [] for e in ENGS}

    def finish(self):
        nc = self.nc
        with nc.Block() as block:
            @block.tensor
            def _(e):
                for f in self.q["pe"]:
                    f(e)

            @block.scalar
            def _(e):
                for f in self.q["act"]:
                    f(e)

            @block.vector
            def _(e):
                for f in self.q["dve"]:
                    f(e)

            @block.gpsimd
            def _(e):
                for f in self.q["pool"]:
                    f(e)

            @block.sync
            def _(e):
                for f in self.q["sp"]:
                    f(e)


NEG = -30000.0
CR_C, CR_W, CR_S = (-3, 26), (-3, 7), (-3, 11)
NCLS_C, NCLS_W, NCLS_S = CR_C[1] - CR_C[0] + 1, CR_W[1] - CR_W[0] + 1, CR_S[1] - CR_S[0] + 1
OFF_C, OFF_W, OFF_S = 0, NCLS_C, NCLS_C + NCLS_W
NCLS = NCLS_C + NCLS_W + NCLS_S


def t5_bucket_np(dist):
    n = np.maximum(dist, 0)
    max_exact = 16
    lr = np.log(np.maximum(n, 1).astype(np.float32) / np.float32(max_exact)) / np.float32(np.log(1024 / 16))
    large = np.minimum(max_exact + (lr.astype(np.float32) * np.float32(16)).astype(np.int32), 31)
    return np.where(n < max_exact, n, large)


def nsa_consts(T, rel_table, g):
    NJ = T // 64
    NKC = T // 128
    NCc = (T // 16 + 127) // 128
    tbl = rel_table.reshape(32, 2, 4)[:, g, :]
    ni = np.arange(128)[:, None]
    qi = np.arange(128)[None, :]
    strips = np.zeros((4, 128, NCLS, 128), np.float32)
    for ci, m in enumerate(range(CR_C[0], CR_C[1] + 1)):
        dist = 128 * m + qi - 16 * ni - 31
        b = t5_bucket_np(dist)
        for h in range(4):
            strips[h, :, OFF_C + ci, :] = np.where(dist >= 0, tbl[b, h], np.float32(NEG))
    for ci, m in enumerate(range(CR_W[0], CR_W[1] + 1)):
        dist = 128 * m + qi - ni
        b = t5_bucket_np(dist)
        for h in range(4):
            strips[h, :, OFF_W + ci, :] = np.where((dist >= 0) & (dist < 512), tbl[b, h], np.float32(NEG))
    for ci, m in enumerate(range(CR_S[0], CR_S[1] + 1)):
        dist = 128 * m + qi - ni
        b = t5_bucket_np(dist)
        for h in range(4):
            strips[h, :, OFF_S + ci, :] = np.where(dist >= 0, tbl[b, h], np.float32(NEG))
    n = np.arange(NCc * 128)
    cs = n * 16
    ce = cs + 31
    ss = np.arange(NJ) * 64
    ov = ((cs[:, None] <= ss[None, :] + 63) & (ce[:, None] >= ss[None, :])).astype(np.float32)
    ov[n >= T // 16 - 1] = 0.0
    jr = np.arange(2 * NJ)[None, :] - NJ
    hi = (np.arange(128)[:, None] >= 64).astype(np.int64)
    valid = jr <= hi
    f1 = jr == hi
    f2 = jr == hi - 1
    vm = (valid & ~f1 & ~f2).astype(np.float32)
    va = np.where(f1, 2e9, np.where(f2, 3e9, np.where(valid, 0.0, -1e9))).astype(np.float32)
    es = np.zeros((NJ, NKC, 128), np.float32)
    for kc in range(NKC):
        es[kc, kc, 0:64] = 1.0
        es[NJ // 2 + kc, kc, 64:128] = 1.0
    return {
        "strips": np.ascontiguousarray(strips.reshape(4, 128, NCLS * 128)),
        "ov": np.ascontiguousarray(ov.reshape(NCc, 128, NJ)),
        "vmva": np.ascontiguousarray(np.concatenate([vm, va], 1)),
        "esel": np.ascontiguousarray(es.reshape(NJ, NKC * 128)),
        "ident": np.eye(128, dtype=np.float32),
    }


GN_EPS = 64e-5
TS = 256
NCH = TS // 64
HN = 4 * NCH
NCM = 512 + 512 + 256 + 256 + 4 * TS + 64


def rwkv_consts():
    su = np.triu(np.ones((64, 64), np.float32), 1)
    ui = np.triu(np.ones((64, 64), np.float32), 0)
    sl = np.tril(np.ones((64, 64), np.float32), -1)
    ident = np.eye(64, dtype=np.float32)
    mb = np.tile(np.concatenate([su, -ui], 1), (1, 4))
    mk = np.tile(np.concatenate([su, ui], 1), (1, 4))
    ml = np.tile(sl, (1, 4))
    i4 = np.tile(ident, (1, 4))
    rm = np.ones((64, 4 * TS), np.float32)
    rm[:, ::64] = 0.0
    ones = np.ones((64, 64), np.float32)
    return np.ascontiguousarray(np.concatenate([mb, mk, ml, i4, rm, ones], 1))


DN_ALPHA = 4 ** 0.25
LN_EPS = 1e-5


def layer_norm_fm(kb, v, gb, out_dram_cols, W, psM, psV, tmp, onesf, x1b=None, out=None):
    sq, mean_sb, msq, rstd = tmp["sq"], tmp["mean"], tmp["msq"], tmp["rstd"]
    for dc in range(8):
        kb.pe("matmul", psM[:, 0:W], lhsT=onesf[:, :], rhs=v[:, dc, 0:W], start=(dc == 0), stop=(dc == 7))
    for dc in range(8):
        s = sq[dc % 2]
        kb.act("activation", out=s[:, 0:W], in_=v[:, dc, 0:W], func=AF.Square)
        kb.pe("matmul", psV[:, 0:W], lhsT=onesf[:, :], rhs=s[:, 0:W], start=(dc == 0), stop=(dc == 7))
    kb.act("copy", out=mean_sb[:, 0:W], in_=psM[:, 0:W])
    kb.dve("tensor_tensor", out=msq[:, 0:W], in0=mean_sb[:, 0:W], in1=mean_sb[:, 0:W], op=ALU.mult)
    kb.dve("tensor_tensor", out=msq[:, 0:W], in0=psV[:, 0:W], in1=msq[:, 0:W], op=ALU.subtract)
    kb.dve("tensor_scalar", out=msq[:, 0:W], in0=msq[:, 0:W], scalar1=LN_EPS, scalar2=None, op0=ALU.add)
    kb.act("activation", out=rstd[:, 0:W], in_=msq[:, 0:W], func=AF.Sqrt)
    kb.dve("reciprocal", out=rstd[:, 0:W], in_=rstd[:, 0:W])
    for dc in range(8):
        kb.dve("tensor_tensor", out=v[:, dc, 0:W], in0=v[:, dc, 0:W], in1=mean_sb[:, 0:W], op=ALU.subtract)
        kb.pool("tensor_tensor", out=v[:, dc, 0:W], in0=v[:, dc, 0:W], in1=rstd[:, 0:W], op=ALU.mult)
        kb.act("activation", out=v[:, dc, 0:W], in_=v[:, dc, 0:W], func=AF.Identity, scale=gb[:, dc:dc + 1], bias=gb[:, 8 + dc:9 + dc])
        if x1b is not None:
            kb.pool("tensor_copy", out=x1b[:, dc, 0:W], in_=v[:, dc, 0:W])
        if out is not None:
            kb.dma(out[dc * 128:(dc + 1) * 128, out_dram_cols[0]:out_dram_cols[1]], v[:, dc, 0:W], q="pool")


class Stager:
    def __init__(self, kb, w=2048, n=2):
        self.kb = kb
        self.b = [kb.sb([128, w], F32, f"stg{i}") for i in range(n)]
        self.i = 0
        self.w = w

    def load_cast(self, dst, src, np_=128, eng=None):
        kb = self.kb
        s = self.b[self.i % len(self.b)]
        eng = eng or ("dve", "pool")[self.i % 2]
        self.i += 1
        shp = list(dst.shape)
        n = int(np.prod(shp[1:]))
        sv = s[0:np_, 0:n]
        if len(shp) == 3:
            sv = sv.rearrange("p (a b) -> p a b", b=shp[2])
        kb.dma(sv, src)
        if eng == "act":
            kb.act("copy", out=dst, in_=sv)
        else:
            kb.op(eng, "tensor_copy", out=dst, in_=sv)


def moe_consts():
    oh = np.zeros((16, 16, 128), np.float32)
    for e in range(16):
        oh[e, e, :] = 1.0
    return {"oh16": np.ascontiguousarray(oh.reshape(16, 16 * 128)), "identf": np.eye(128, dtype=np.float32)}


def stage_k1(kb, T, l, xsrc, D, ZT, ZV):
    HALF = min(4096, T)
    NCOL = 5144
    NCHK = 41
    wd = D["w_in"][l]
    wvd = D["w_v"][l]
    wb = kb.sb([128, 8, NCHK * 128], BF16, "wb")
    wvb = kb.sb([128, 8, 280], BF16, "wvb")
    xb = kb.sb([128, 8, HALF], BF16, "xb")
    stg = [kb.sb([128, 2048], F32, f"stg{i}") for i in range(3)]
    ceng = ["dve", "pool", "act"]
    si = [0]

    def load_cast(dst, src, w):
        s = stg[si[0] % 3]
        e = ceng[si[0] % 3]
        si[0] += 1
        kb.dma(s[:, :w], src)
        if e == "act":
            kb.act("copy", out=dst, in_=s[:, :w])
        else:
            kb.op(e, "tensor_copy", out=dst, in_=s[:, :w])

    kb.dve("memset", wb[:, :, NCOL:NCHK * 128], 0.0)
    for k in range(8):
        for c0 in range(0, NCOL, 2048):
            cw = min(2048, NCOL - c0)
            load_cast(wb[:, k, c0:c0 + cw], wd[k * 128:(k + 1) * 128, c0:c0 + cw], cw)
        load_cast(wvb[:, k, :], wvd[k * 128:(k + 1) * 128, :], 280)
    pss = [kb.ps([128, 512], F32, f"ps{i}") for i in range(4)]
    psv = [kb.ps([128, 512], F32, f"psv{i}") for i in range(2)]
    outs = [kb.sb([128, 512], F32, f"ob{i}") for i in range(4)]
    ovs = [kb.sb([128, 280], F32, f"ov{i}") for i in range(2)]
    it = 0
    for th in range(T // HALF):
        h0 = th * HALF
        for k in range(8):
            for c0 in range(0, HALF, 2048):
                load_cast(xb[:, k, c0:c0 + 2048], xsrc[k * 128:(k + 1) * 128, h0 + c0:h0 + c0 + 2048], 2048)
        for ct in range(NCHK):
            for tt in range(HALF // 512):
                ps = pss[it % 4]; ob = outs[it % 4]
                for k in range(8):
                    kb.pe("matmul", ps[:, :], lhsT=wb[:, k, ct * 128:(ct + 1) * 128], rhs=xb[:, k, tt * 512:(tt + 1) * 512], start=(k == 0), stop=(k == 7))
                if it % 2 == 0:
                    kb.act("copy", out=ob[:, :], in_=ps[:, :])
                else:
                    kb.dve("tensor_copy", out=ob[:, :], in_=ps[:, :])
                kb.dma(ZT[ct * 128:(ct + 1) * 128, h0 + tt * 512:h0 + (tt + 1) * 512], ob[:, :], q="sp")
                it += 1
        for tk in range(HALF // 128):
            ps = psv[tk % 2]; ov = ovs[tk % 2]
            for k in range(8):
                kb.pe("matmul", ps[:, 0:280], lhsT=xb[:, k, tk * 128:(tk + 1) * 128], rhs=wvb[:, k, :], start=(k == 0), stop=(k == 7))
            kb.act("copy", out=ov[:, :], in_=ps[:, 0:280])
            kb.dma(ZV[h0 + tk * 128:h0 + (tk + 1) * 128, :], ov[:, :], q="sp")


def stage_k2a(kb, T, g, l, ZT, ZV, D, onT):
    nc, es = kb.nc, kb.es
    stage, nq, cs = 9, None, 99
    NJ = T // 64
    NKC = T // 128
    NQ = T // 128
    NCc = (T // 16 + 127) // 128
    NCP = NCc * 128
    VW = 65 + NJ
    vb = g * 140
    qTd_v = ZT[g * 256:(g + 1) * 256, :]
    ksTd = ZT[768 + g * 64:768 + (g + 1) * 64, :]
    kwTd = ZT[1024 + g * 64:1024 + (g + 1) * 64, :]
    kcTd = ZT[512 + g * 64:512 + (g + 1) * 64, :]
    vcTd = ZT[640 + g * 64:640 + (g + 1) * 64, :]
    w1kd, w1vd, w2kd, w2vd, pekd, pevd = D["w1k"][l], D["w1v"][l], D["w2k"][l], D["w2v"][l], D["pek"][l], D["pev"][l]
    strd, ovd, vmvad, eseld, identd = D["strips"][g], D["ov"], D["vmva"], D["esel"], D["ident"]
    LK = [kb.banklock(f"lk{i}") for i in range(8)]
    bST = [kb.pst(f"bst{i}", [128, 512], F32) for i in range(2)]
    bC = [kb.pst(f"bC{i}", [128, 512], F32) for i in range(2)]
    bW = kb.pst("bW", [128, 512], F32)
    bS = [kb.pst(f"bS{i}", [128, 512], F32) for i in range(2)]
    bT = kb.pst("bT", [128, 1024], BF16)
    ST = [kb.sub(bST[i][:, :], f"ST{i}", LK[i]) for i in range(2)]
    hidp = [kb.sub(bST[i][:, :], f"hid{i}", LK[i]) for i in range(2)]
    accC = [kb.sub(bC[s_ // 2][:, (s_ % 2) * 256:(s_ % 2) * 256 + VW], f"accC{s_}", LK[2 + s_ // 2]) for s_ in range(4)]
    accW = [kb.sub(bW[:, s_ * 65:(s_ + 1) * 65], f"accW{s_}", LK[4]) for s_ in range(4)]
    accS = [[kb.sub(bS[p_][:, s_ * 65:(s_ + 1) * 65], f"accS{p_}{s_}", LK[5 + p_]) for s_ in range(4)] for p_ in range(2)]
    tpp = kb.sub(bT[:, 0:128], "tpp", LK[7])
    misc = kb.sub(bC[0][:, :], "misc", LK[2])
    misc2 = kb.sub(bC[1][:, 0:256], "misc2", LK[3])

    stg = [kb.sb([128, 2048], F32, f"stg{i}") for i in range(2)]
    sti = [0]

    def load_cast(dst, src, np_=128, w=None, eng="dve"):
        w = w or dst.shape[-1]
        s = stg[sti[0] % 2]; sti[0] += 1
        kb.dma(s[0:np_, 0:w], src)
        if eng == "act":
            kb.act("copy", out=dst, in_=s[0:np_, 0:w])
        else:
            kb.op(eng, "tensor_copy", out=dst, in_=s[0:np_, 0:w])

    identb = kb.sb([128, 128], BF16, "identb")
    identf = kb.sb([128, 128], F32, "identf")
    kb.dma(identf[:, :], identd[:, :])
    load_cast(identb[:, :], identd[:, :])
    strips = kb.sb([128, 4, NCLS * 128], BF16, "strips")
    for h in range(4 if cs >= 1 else 0):
        for c0 in range(0, NCLS * 128, 2048):
            w = min(2048, NCLS * 128 - c0)
            load_cast(strips[:, h, c0:c0 + w], strd[h][:, c0:c0 + w], eng=("dve" if (c0 // 2048) % 2 == 0 else "pool"))
    esel = kb.sb([NJ, NKC * 128], BF16, "esel")
    for c0 in range(0, NKC * 128 if cs >= 2 else 0, 2048):
        w = min(2048, NKC * 128 - c0)
        load_cast(esel[:, c0:c0 + w], eseld[:, c0:c0 + w], np_=NJ)
    vmva = kb.sb([128, 4 * NJ], F32, "vmva")
    b31 = kb.sb([128, 4], F32, "b31")
    kb.dma(b31[:, :], D["b31"][g])
    if cs >= 3: kb.dma(vmva[:, :], vmvad[:, :])
    ksT = kb.sb([64, T], BF16, "ksT"); kwT = kb.sb([64, T], BF16, "kwT")
    for c0 in range(0, T if cs >= 4 else 0, 2048):
        load_cast(ksT[:, c0:c0 + 2048], ksTd[:, c0:c0 + 2048], np_=64, eng="pool")
        load_cast(kwT[:, c0:c0 + 2048], kwTd[:, c0:c0 + 2048], np_=64, eng="dve")
    VsA = kb.sb([128, NKC, 65], BF16, "VsA"); VwA = kb.sb([128, NKC, 65], BF16, "VwA")
    if cs >= 5:
        kb.dve("memset", VsA[:, :, 64:65], 1.0)
        kb.dve("memset", VwA[:, :, 64:65], 1.0)
    for (dst, srcd) in ((VsA, ZV[:, vb:vb + 64]), (VwA, ZV[:, vb + 64:vb + 128])):
        src = srcd.rearrange("(c p) d -> p c d", p=128)
        for c0 in range(0, NKC, 32):
            cw = min(32, NKC - c0)
            s = stg[sti[0] % 2]; sti[0] += 1
            sv = s[:, 0:cw * 64].rearrange("p (c d) -> p c d", d=64)
            kb.dma(sv, src[:, c0:c0 + cw, :])
            kb.dve("tensor_copy", out=dst[:, c0:c0 + cw, 0:64], in_=sv)
    VcA = kb.sb([128, NCc, VW], BF16, "VcA")
    if cs >= 7: kb.dve("memset", VcA[:, :, 64:65], 1.0)
    for c in range(NCc if cs >= 8 else 0):
        load_cast(VcA[:, c, 65:VW], ovd[c], w=NJ)
    kcT = kb.sb([64, NCP], BF16, "kcT")

    es3 = ExitStack()
    kb.es = es3
    w1b = kb.sb([64, 32, 256], BF16, "w1b")
    w2b = kb.sb([128, 2, 64], BF16, "w2b")
    peb = kb.sb([64, 32], BF16, "peb")
    pbias = kb.sb([128, 2], F32, "pbias")
    cTf = kb.sb([64, T + 32], BF16, "cTf")
    hsb = kb.sb([128, 2, 512], F32, "hsb")
    tg = kb.sb([128, 512], F32, "tg")
    gT = kb.sb([128, 2, 512], BF16, "gT")
    kb.dve("memset", cTf[:, T:T + 32], 0.0)
    for which in range(2):
        srcT, w1d, w2d, ped = ((kcTd, w1kd, w2kd, pekd), (vcTd, w1vd, w2vd, pevd))[which]
        for c0_ in range(0, T, 2048):
            load_cast(cTf[:, c0_:c0_ + 2048], srcT[:, c0_:c0_ + 2048], np_=64)
        w1v_ = w1d.rearrange("(l d) h -> d l h", d=64)
        for l0 in range(0, 32, 8):
            s = stg[sti[0] % 2]; sti[0] += 1
            sv = s[0:64, 0:2048].rearrange("p (c h) -> p c h", h=256)
            kb.dma(sv, w1v_[:, l0:l0 + 8, :])
            kb.dve("tensor_copy", out=w1b[:, l0:l0 + 8, :], in_=sv)
        s = stg[sti[0] % 2]; sti[0] += 1
        sv = s[:, 0:128].rearrange("p (c d) -> p c d", d=64)
        kb.dma(sv, w2d.rearrange("(c p) d -> p c d", p=128))
        kb.dve("tensor_copy", out=w2b[:, :, :], in_=sv)
        s = stg[sti[0] % 2]; sti[0] += 1
        kb.dma(s[0:64, 0:32], ped.rearrange("(l d) -> d l", d=64), allow_slow_non_contiguous=True)
        kb.dve("tensor_copy", out=peb[:, :], in_=s[0:64, 0:32])
        for hc in range(2):
            for l_ in range(32):
                kb.pe("matmul", misc[:, 0:1], lhsT=w1b[:, l_, hc * 128:(hc + 1) * 128], rhs=peb[:, l_:l_ + 1], start=(l_ == 0), stop=(l_ == 31))
            kb.dve("tensor_copy", out=pbias[:, hc:hc + 1], in_=misc[:, 0:1])
        for nb in range(0, NCP, 512):
            nw = min(512, NCP - nb)
            for l_ in range(32):
                rhs = cTf[:, 16 * nb + l_:16 * nb + l_ + 16 * nw:16]
                for hc in range(2):
                    kb.pe("matmul", hidp[hc][:, 0:nw], lhsT=w1b[:, l_, hc * 128:(hc + 1) * 128], rhs=rhs, start=(l_ == 0), stop=(l_ == 31))
            for hc in range(2):
                hv = hsb[:, hc, 0:nw]
                kb.act("activation", out=hv, in_=hidp[hc][:, 0:nw], func=AF.Identity, bias=pbias[:, hc:hc + 1], scale=1.0)
                kb.dve("tensor_tensor", out=tg[:, 0:nw], in0=hv, in1=hv, op=ALU.mult)
                kb.dve("tensor_scalar", out=tg[:, 0:nw], in0=tg[:, 0:nw], scalar1=0.044715, scalar2=1.0, op0=ALU.mult, op1=ALU.add)
                kb.dve("tensor_tensor", out=tg[:, 0:nw], in0=tg[:, 0:nw], in1=hv, op=ALU.mult)
                kb.act("activation", out=tg[:, 0:nw], in_=tg[:, 0:nw], func=AF.Sigmoid, scale=1.5957691216057308)
                kb.dve("tensor_tensor", out=gT[:, hc, 0:nw], in0=tg[:, 0:nw], in1=hv, op=ALU.mult)
            if which == 0:
                for hc in range(2):
                    kb.pe("matmul", misc[0:64, 0:nw], lhsT=w2b[:, hc, :], rhs=gT[:, hc, 0:nw], start=(hc == 0), stop=(hc == 1))
                kb.act("copy", out=kcT[:, nb:nb + nw], in_=misc[0:64, 0:nw])
            else:
                for cc in range(nw // 128):
                    for hc in range(2):
                        kb.pe("matmul", misc[:, 0:64], lhsT=gT[:, hc, cc * 128:(cc + 1) * 128], rhs=w2b[:, hc, :], start=(hc == 0), stop=(hc == 1))
                    kb.act("copy", out=VcA[:, nb // 128 + cc, 0:64], in_=misc[:, 0:64])

    kb.barrier()
    kb.flush()
    es3.close()
    kb.es = es
    QB = 4 if NQ % 4 == 0 else 1
    QW = QB * 128
    qf = [kb.sb([64, 4, QW], F32, "qf0")] * 2
    qb = [kb.sb([64, 4, QW], BF16, f"qb{i}") for i in range(2)]
    glt = kb.sb([128, QB, 12], F32, "glt")
    gs = kb.sb([128, QB, 12], F32, "gs")
    PT = [kb.sb([128, QW], BF16, f"PT{i}") for i in range(4)]
    rd = kb.sb([128, QB, 12], F32, "rd")
    cf = kb.sb([128, QB, 12], F32, "cf")
    ocs = kb.sb([128, QB, 4, 64], F32, "ocs")
    imp = kb.sb([128, QB, NJ], F32, "imp")
    imp2 = kb.sb([128, NJ], F32, "imp2")
    impw = kb.sb([128, NJ], F32, "impw")
    mx = kb.sb([128, 16], F32, "mx")
    c1 = kb.sb([128, NJ], F32, "c1")
    negp = kb.sb([128, NJ], BF16, "negp")
    negT = kb.sb([NJ, QW], BF16, "negT")
    OSB = [kb.sb([128, 2, 128], F32, f"OSB{i}") for i in range(2)]
    qsrc = qTd_v.rearrange("(h d) t -> d h t", h=4)
    cnt = {"st": 0, "pt": 0, "o": 0}

    def scores(h, qbt, kT_chunk, cls0, extra=None, sat=False):
        st = ST[cnt["st"] % 2]; cnt["st"] += 1
        pt = PT[cnt["pt"] % 4]; cnt["pt"] += 1
        kb.pe("matmul", st[:, 0:QW], lhsT=kT_chunk, rhs=qbt[:, h, :], start=True, stop=(sat and extra is None))
        if extra is not None:
            kb.pe("matmul", st[:, 0:QW], lhsT=extra, rhs=negT[:, :], start=False, stop=sat)
        if sat:
            kb.act("activation", out=pt[:, :], in_=st[:, 0:QW], func=AF.Exp, bias=b31[:, h:h + 1], scale=1.0)
        else:
            kb.pe("matmul", st[:, 0:QW], lhsT=identb[:, :], rhs=strips[:, h, cls0 * 128:cls0 * 128 + QW], start=False, stop=True)
            kb.act("activation", out=pt[:, :], in_=st[:, 0:QW], func=AF.Exp)
        return pt

    for blk in range(NQ // QB):
        qt0 = blk * QB
        q0 = qt0 * 128
        i = blk % 2
        kb.dma(qf[i][:, :, :], qsrc[:, :, q0:q0 + QW])
        kb.act("mul", out=qb[i][:, :, :], in_=qf[i][:, :, :], mul=0.125)
        kb.dma(glt[:, :, :], ZV[q0:q0 + QW, vb + 128:vb + 140].rearrange("(s p) c -> p s c", p=128))
        kb.act("activation", out=gs[:, :, :], in_=glt[:, :, :], func=AF.Sigmoid)
        ncj_s = [min(NCc - 1, ((qt0 + s_) * 128 + 96) // 16 // 128) + 1 for s_ in range(QB)]
        for h in range(4):
            for cj in range(max(ncj_s)):
                m0 = min(qt0 - 16 * cj, CR_C[1] - 3)
                pt = scores(h, qb[i], kcT[:, cj * 128:(cj + 1) * 128], OFF_C + m0 - CR_C[0], sat=(qt0 - 16 * cj >= 23))
                for s_ in range(QB):
                    if cj < ncj_s[s_]:
                        kb.pe("matmul", accC[s_][:, :], lhsT=pt[:, s_ * 128:(s_ + 1) * 128], rhs=VcA[:, cj, :], start=(cj == 0 and s_ % 2 == 0), stop=(cj == ncj_s[s_] - 1))
            for s_ in range(QB):
                acc = accC[s_]
                rcol = rd[:, s_, 3 * h:3 * h + 1]
                kb.dve("tensor_scalar", out=rcol, in0=acc[:, 64:65], scalar1=1e-30, scalar2=None, op0=ALU.max)
                kb.dve("reciprocal", out=rcol, in_=rcol)
                kb.act("copy", out=ocs[:, s_, h, :], in_=acc[:, 0:64])
                if h == 0:
                    kb.dve("tensor_scalar", out=imp[:, s_, :], in0=acc[:, 65:VW], scalar1=rcol, scalar2=None, op0=ALU.mult)
                else:
                    kb.dve("scalar_tensor_tensor", out=imp[:, s_, :], in0=acc[:, 65:VW], scalar=rcol, in1=imp[:, s_, :], op0=ALU.mult, op1=ALU.add)
        for s_ in range(QB):
            qt = qt0 + s_
            j0 = NJ - 2 * qt
            kb.dve("tensor_tensor", out=imp2[:, :], in0=imp[:, s_, :], in1=vmva[:, j0:j0 + NJ], op=ALU.mult)
            kb.dve("tensor_tensor", out=imp2[:, :], in0=imp2[:, :], in1=vmva[:, 2 * NJ + j0:2 * NJ + j0 + NJ], op=ALU.add)
            kb.dve("memset", imp2[:, 0:1], 1e9)
            kb.dve("max", out=mx[:, 0:8], in_=imp2[:, :])
            kb.dve("match_replace", out=impw[:, :], in_to_replace=mx[:, 0:8], in_values=imp2[:, :], imm_value=-3e9)
            kb.dve("max", out=mx[:, 8:16], in_=impw[:, :])
            kb.dve("tensor_scalar", out=c1[:, :], in0=imp2[:, :], scalar1=mx[:, 15:16], scalar2=None, op0=ALU.is_ge)
            kb.dve("tensor_scalar", out=impw[:, :], in0=imp2[:, :], scalar1=0.0, scalar2=None, op0=ALU.is_ge)
            kb.dve("tensor_tensor", out=c1[:, :], in0=c1[:, :], in1=impw[:, :], op=ALU.mult)
            kb.dve("tensor_scalar", out=negp[:, :].rearrange("q (jj c) -> q c jj", jj=2), in0=c1[:, :].rearrange("q (c jj) -> q c jj", jj=2),
                   scalar1=-1.0, scalar2=-NEG, op0=ALU.add, op1=ALU.mult)
            kb.pe("transpose", out=tpp[0:NJ, :], in_=negp[:, :], identity=identb[:, :])
            kb.act("copy", out=negT[:, s_ * 128:(s_ + 1) * 128], in_=tpp[0:NJ, :])
        for h in range(4):
            aS = accS[h % 2]
            kcs = [kc for kc in range(qt0 - 4, qt0 + QB) if kc >= 0]
            for kc in kcs:
                pt = scores(h, qb[i], kwT[:, kc * 128:(kc + 1) * 128], OFF_W + (qt0 - kc) - CR_W[0])
                for s_ in range(QB):
                    d_ = qt0 + s_ - kc
                    if 0 <= d_ <= 4:
                        kb.pe("matmul", accW[s_][:, :], lhsT=pt[:, s_ * 128:(s_ + 1) * 128], rhs=VwA[:, kc, :], start=(kc == kcs[0] and s_ == 0), stop=(d_ == 0))
            for kc in range(qt0 + QB):
                ms0 = min(qt0 - kc, CR_S[1] - 3)
                pt = scores(h, qb[i], ksT[:, kc * 128:(kc + 1) * 128], OFF_S + ms0 - CR_S[0], extra=esel[:, kc * 128:(kc + 1) * 128], sat=(qt0 - kc >= 8))
                for s_ in range(QB):
                    if kc <= qt0 + s_:
                        kb.pe("matmul", aS[s_][:, :], lhsT=pt[:, s_ * 128:(s_ + 1) * 128], rhs=VsA[:, kc, :], start=(kc == 0 and s_ == 0), stop=(kc == qt0 + s_))
            for s_ in range(QB):
                kb.dve("reciprocal", out=rd[:, s_, 3 * h + 1:3 * h + 2], in_=aS[s_][:, 64:65])
                kb.dve("reciprocal", out=rd[:, s_, 3 * h + 2:3 * h + 3], in_=accW[s_][:, 64:65])
                kb.dve("tensor_tensor", out=cf[:, s_, 3 * h:3 * h + 3], in0=rd[:, s_, 3 * h:3 * h + 3], in1=gs[:, s_, 3 * h:3 * h + 3], op=ALU.mult)
                oc = ocs[:, s_, h, :]
                kb.dve("tensor_scalar", out=oc, in0=oc, scalar1=cf[:, s_, 3 * h:3 * h + 1], scalar2=None, op0=ALU.mult)
                kb.dve("scalar_tensor_tensor", out=oc, in0=aS[s_][:, 0:64], scalar=cf[:, s_, 3 * h + 1:3 * h + 2], in1=oc, op0=ALU.mult, op1=ALU.add)
                kb.dve("scalar_tensor_tensor", out=oc, in0=accW[s_][:, 0:64], scalar=cf[:, s_, 3 * h + 2:3 * h + 3], in1=oc, op0=ALU.mult, op1=ALU.add)
        for s_ in range(QB):
            osb = OSB[cnt["o"] % 2]; cnt["o"] += 1
            for j in range(2):
                kb.pe("transpose", out=misc2[:, j * 128:(j + 1) * 128], in_=ocs[:, s_, 2 * j:2 * j + 2, :].rearrange("p h c -> p (h c)"), identity=identf[:, :])
            kb.act("copy", out=osb[:, :, :].rearrange("p j q -> p (j q)"), in_=misc2[:, 0:256])
            kb.dma(onT[g * 256:(g + 1) * 256, q0 + s_ * 128:q0 + (s_ + 1) * 128].rearrange("(j p) q -> p j q", p=128), osb[:, :, :], q="pool")


def stage_k2b(kb, T, g, l, ZT, D, orT, vfirstT):
    nc, es = kb.nc, kb.es
    layer1 = l > 0
    ntile = T // TS
    base = 1304
    sl0, sl1 = g * 256, (g + 1) * 256
    zr = ZT[base + sl0:base + sl1, :]
    zk = ZT[base + 512 + sl0:base + 512 + sl1, :]
    zv = ZT[base + 1024 + sl0:base + 1024 + sl1, :]
    zvo = ZT[base + 1024 + (1 - g) * 256:base + 1024 + (2 - g) * 256, :]
    zw = ZT[base + 1536:base + 1600, :]
    za = ZT[base + 1600:base + 1664, :]
    zg = ZT[base + 1664:base + 1792, :]
    prd, mugd = D["pr"][l][g], D["mug"][l]
    w2d, a2d, g2d = D["w2"][l][:, sl0:sl1], D["a2"][l][:, sl0:sl1], D["g2"][l][:, sl0:sl1]
    gnd = D["gn"][l][:, sl0:sl1]
    cmd = D["cm"]
    vout = vfirstT[sl0:sl1, :]
    if layer1:
        vfd = vfirstT[sl0:sl1, :]
        v1d, v2d, pvd = D["v1"], D["v2"][:, sl0:sl1], D["pv"][g]
    cm = kb.sb([64, NCM], F32, "cm")
    kb.dma(cm[:, :], cmd[:, :])
    Mb = cm[:, 0:512].rearrange("p (h c) -> p h c", h=4)
    Mk = cm[:, 512:1024].rearrange("p (h c) -> p h c", h=4)
    Ml = cm[:, 1024:1280].rearrange("p (h c) -> p h c", h=4)
    I4 = cm[:, 1280:1536].rearrange("p (h c) -> p h c", h=4)
    rmask = cm[:, 1536:1536 + 4 * TS]
    ones = cm[:, 1536 + 4 * TS:1600 + 4 * TS]
    identb = kb.sb([64, 64], BF16, "identb")
    kb.dve("tensor_copy", out=identb[:, :], in_=cm[:, 1280:1344])
    pr = kb.sb([64, 34], F32, "pr")
    kb.dma(pr[:, :], prd[:, :])
    P = lambda i: pr[:, 4 * i:4 * i + 4]
    mu_r, mu_k, mu_v, w0, a0, k_k, k_a, r_k = [P(i) for i in range(8)]
    mug = kb.sb([128, 1], F32, "mug")
    kb.dma(mug[:, :], mugd[:, :])
    omka = kb.sb([64, 4], F32, "omka")
    kb.dve("tensor_scalar", out=omka[:, :], in0=k_a, scalar1=-1.0, scalar2=1.0, op0=ALU.mult, op1=ALU.add)
    stg = kb.sb([128, 256], F32, "wstg")
    w2b = kb.sb([64, 256], BF16, "w2b")
    a2b = kb.sb([64, 256], BF16, "a2b")
    g2b = kb.sb([128, 256], BF16, "g2b")
    kb.dma(stg[0:64, :], w2d[:, :]); kb.dve("tensor_copy", out=w2b[:, :], in_=stg[0:64, :])
    kb.dma(stg[0:64, :], a2d[:, :]); kb.dve("tensor_copy", out=a2b[:, :], in_=stg[0:64, :])
    kb.dma(stg[:, :], g2d[:, :]); kb.dve("tensor_copy", out=g2b[:, :], in_=stg[:, :])
    gng = kb.sb([64, 256], F32, "gng")
    gnb = kb.sb([64, 256], F32, "gnb")
    kb.dma(gng[:, :], gnd[0:1, :].partition_broadcast(64))
    kb.dma(gnb[:, :], gnd[1:2, :].partition_broadcast(64))

    if layer1:
        pv = kb.sb([64, 12], F32, "pv"); kb.dma(pv[:, :], pvd[:, :])
        v1b = kb.sb([64, 8, 32], BF16, "v1b")
        kb.dma(stg[0:64, 0:128].rearrange("p (h l) -> p h l", l=32), v1d[sl0:sl1, :].rearrange("(h j) l -> j h l", h=4))
        kb.dma(stg[0:64, 128:256].rearrange("p (h l) -> p h l", l=32), v1d[(1 - g) * 256:(2 - g) * 256, :].rearrange("(h j) l -> j h l", h=4))
        kb.dve("tensor_copy", out=v1b[:, :, :], in_=stg[0:64, :].rearrange("p (h l) -> p h l", l=32))
        v2b = kb.sb([32, 256], BF16, "v2b")
        kb.dma(stg[0:32, :], v2d[:, :]); kb.dve("tensor_copy", out=v2b[:, :], in_=stg[0:32, :])
        VA0 = kb.sb([64, 8, TS + 1], F32, "VA0"); vsa = kb.sb([64, 8, TS], F32, "vsa"); vsab = kb.sb([64, 8, TS], BF16, "vsab")
        vf = kb.sb([64, 4, TS], F32, "vf"); latb = kb.sb([32, TS], BF16, "latb"); sgv = kb.sb([64, 4, TS], F32, "sgv")
    bk = [kb.pst(f"bank{i}", [128, 512], F32) for i in (0, 2, 3, 4, 5, 6, 7)]
    bkT = kb.pst("bankT", [128, 1024], BF16)
    LK = [kb.banklock(f"lk{i}") for i in range(8)]
    sc = kb.sub(bk[0][0:64, 0:TS], "sc", LK[0])
    tpo = kb.sub(bk[0][:, 256:384].rearrange("p (j c) -> p j c", j=2), "tpo", LK[0])
    pB = kb.sub(bk[1][0:64, :].rearrange("p (h c) -> p h c", h=4), "pB", LK[1])
    pK = kb.sub(bk[2][0:64, :].rearrange("p (h c) -> p h c", h=4), "pK", LK[2])
    pL = kb.sub(bk[3][0:64, 0:256].rearrange("p (h c) -> p h c", h=4), "pL", LK[3])
    psG = kb.sub(bk[3][0:64, 256:512], "psG", LK[3])
    v4 = lambda ap_: ap_.rearrange("p (h c) -> p h c", h=4)
    XB = [(bk[1], LK[1]), (bk[2], LK[2]), (bk[4], LK[4]), (bk[5], LK[5])]
    psNLn = [kb.sub(v4(XB[n_][0][0:64, 0:256]), f"psNL{n_}", XB[n_][1]) for n_ in range(4)]
    psQn = [kb.sub(v4(XB[n_][0][0:64, 256:512]), f"psQ{n_}", XB[n_][1]) for n_ in range(4)]
    pW = kb.sub(bk[5][0:64, 0:256].rearrange("p (h c) -> p h c", h=4), "pW", LK[5])
    pU = kb.sub(bk[5][0:64, 256:512].rearrange("p (h c) -> p h c", h=4), "pU", LK[5])
    pO = kb.sub(bk[6][0:64, 0:256].rearrange("p (h c) -> p h c", h=4), "pO", LK[6])
    pdS = kb.sub(bk[6][0:64, 256:512].rearrange("p (h c) -> p h c", h=4), "pdS", LK[6])
    tp = kb.sub(bkT[0:64, :].rearrange("p (b c) -> p b c", b=16), "tp", LK[7])

    S = kb.sb([64, 4, 64], F32, "S")
    Sb = kb.sb([64, 4, 64], BF16, "Sb")
    kb.dve("memset", S[:, :, :], 0.0)
    kb.dve("memset", Sb[:, :, :], 0.0)

    def two(shape, dt, name):
        return [kb.sb(shape, dt, f"{name}{i}") for i in range(2)]

    R0 = [kb.sb([64, 4, TS + 1], F32, "R0")] * 2; K0 = [kb.sb([64, 4, TS + 1], F32, "K0")] * 2
    V0 = ([kb.sb([64, 4, TS + 1], F32, "V0")] * 2) if not layer1 else None
    W0 = two([64, TS + 1], F32, "W0"); A0 = two([64, TS + 1], F32, "A0"); G0 = two([128, TS + 1], F32, "G0")
    d3 = kb.sb([64, 4, TS], F32, "d3"); t1 = d3
    rs = kb.sb([64, 4, TS], F32, "rs"); ks = kb.sb([64, 4, TS], F32, "ks"); vs = kb.sb([64, 4, TS], F32, "vs_")
    d1 = kb.sb([128, TS], F32, "d1")
    tzw = kb.sb([64, TS], BF16, "tzw"); zab = kb.sb([64, TS], BF16, "zab")
    sgb = two([128, TS], BF16, "sgb")
    sig = kb.sb([64, 4, TS], F32, "sig"); av = kb.sb([64, 4, TS], F32, "av")
    ell = sig; Gc = kb.sb([64, 4, TS], F32, "Gc")
    kk = kb.sb([64, 4, TS], F32, "kk"); sq = kb.sb([64, 4, TS], F32, "sq")
    lnss = kb.sb([64, TS], F32, "lnss")
    rn = kb.sb([64, 4, TS], F32, "rn")
    kap = kb.sb([64, 4, TS], F32, "kap")
    kp = kb.sb([64, 4, TS], F32, "kp"); bet = kb.sb([64, 4, TS], F32, "bet")
    pd = kk
    eG = kb.sb([64, 4, TS], F32, "eG"); eGm = eG
    enG = eG; edG = eG
    gC = two([64, HN], F32, "gC")
    KR = two([64, HN, 2, 64], BF16, "KR")
    KT = two([64, HN, 64], BF16, "KT"); BT = two([64, HN, 64], BF16, "BT")
    FM = two([64, 4, HN, 64], BF16, "FM")
    OUT = two([64, NCH, 256], F32, "OUT")
    OT = two([128, 2, TS], F32, "OT")

    TM = [kb.sb([64, 16, 64], BF16, f"TM{n_}") for n_ in range(NCH)]
    Nf = [two([64, 4, 64], BF16, f"Nf{n_}_") for n_ in range(NCH)]; Lf = [two([64, 4, 64], BF16, f"Lf{n_}_") for n_ in range(NCH)]
    Qm = [kb.sb([64, 4, 64], BF16, f"Qm{n_}") for n_ in range(NCH)]
    NArb = [kb.sb([64, 4, 64], BF16, f"NArb{n_}") for n_ in range(NCH)]; AK = [kb.sb([64, 4, 128], BF16, f"AK{n_}") for n_ in range(NCH)]
    TT = Qm
    Wb = kb.sb([64, 4, 64], BF16, "Wb"); Ub = kb.sb([64, 4, 64], BF16, "Ub")
    osb = kb.sb([64, 4, 64], F32, "osb"); osq = kb.sb([64, 4, 64], F32, "osq")
    st = kb.sb([64, 24], F32, "st")
    on = kb.sb([64, 4, 64], F32, "on"); bon = kb.sb([64, 4, 64], F32, "bon")

    B3 = [64, 4, TS]
    f32v = lambda b: b[:, :, :].rearrange("p h (n c) -> p (h n) c", c=64)

    def shift3(X0, zd, mu, out, t0, i):
        src = zd.rearrange("(h j) t -> j h t", h=4)
        if t0 == 0:
            kb.pool("memset", X0[i][:, :, 0:1], 0.0)
            kb.dma(X0[i][:, :, 1:TS + 1], src[:, :, 0:TS])
        else:
            kb.dma(X0[i][:, :, :], src[:, :, t0 - 1:t0 + TS])
        kb.pool("tensor_tensor", out=d3[:, :, :], in0=X0[i][:, :, 0:TS], in1=X0[i][:, :, 1:TS + 1], op=ALU.subtract)
        kb.pool("tensor_tensor", out=d3[:, :, :], in0=d3[:, :, :], in1=mu.bc(B3), op=ALU.mult)
        kb.pool("tensor_tensor", out=out[:, :, :], in0=d3[:, :, :], in1=X0[i][:, :, 1:TS + 1], op=ALU.add)

    def shift1(X0, zd, mucol, np_, t0, i):
        if t0 == 0:
            kb.pool("memset", X0[i][:, 0:1], 0.0)
            kb.dma(X0[i][:, 1:TS + 1], zd[:, 0:TS])
        else:
            kb.dma(X0[i][:, :], zd[:, t0 - 1:t0 + TS])
        kb.dve("tensor_tensor", out=d1[0:np_, :], in0=X0[i][:, 0:TS], in1=X0[i][:, 1:TS + 1], op=ALU.subtract)
        kb.dve("scalar_tensor_tensor", out=d1[0:np_, :], in0=d1[0:np_, :], scalar=mucol, in1=X0[i][:, 1:TS + 1], op0=ALU.mult, op1=ALU.add)

    for tt in range(ntile):
        t0 = tt * TS
        i = tt % 2
        shift3(R0, zr, mu_r, rs, t0, i)
        shift3(K0, zk, mu_k, ks, t0, i)
        if not layer1:
            shift3(V0, zv, mu_v, vs, t0, i)
        else:
            srcm = zv.rearrange("(h j) t -> j h t", h=4)
            srco = zvo.rearrange("(h j) t -> j h t", h=4)
            if t0 == 0:
                kb.pool("memset", VA0[:, :, 0:1], 0.0)
                kb.dma(VA0[:, 0:4, 1:TS + 1], srcm[:, :, 0:TS])
                kb.dma(VA0[:, 4:8, 1:TS + 1], srco[:, :, 0:TS])
            else:
                kb.dma(VA0[:, 0:4, :], srcm[:, :, t0 - 1:t0 + TS])
                kb.dma(VA0[:, 4:8, :], srco[:, :, t0 - 1:t0 + TS])
            B8 = [64, 8, TS]
            kb.pool("tensor_tensor", out=vsa[:, :, :], in0=VA0[:, :, 0:TS], in1=VA0[:, :, 1:TS + 1], op=ALU.subtract)
            kb.pool("tensor_tensor", out=vsa[:, :, :], in0=vsa[:, :, :], in1=pv[:, 0:8].bc(B8), op=ALU.mult)
            kb.pool("tensor_tensor", out=vsa[:, :, :], in0=vsa[:, :, :], in1=VA0[:, :, 1:TS + 1], op=ALU.add)
            kb.dve("tensor_copy", out=vsab[:, :, :], in_=vsa[:, :, :])
            kb.dma(vf[:, :, :], vfd.rearrange("(h j) t -> j h t", h=4)[:, :, t0:t0 + TS])
            for h8 in range(8):
                kb.pe("matmul", sc[0:32, :], lhsT=v1b[:, h8, :], rhs=vsab[:, h8, :], start=(h8 == 0), stop=(h8 == 7))
            kb.act("copy", out=latb[:, :], in_=sc[0:32, :])
            for h in range(4):
                kb.pe("matmul", sc[:, :], lhsT=v2b[:, h * 64:(h + 1) * 64], rhs=latb[:, :], start=True, stop=True)
                kb.act("activation", out=sgv[:, h, :], in_=sc[:, :], func=AF.Sigmoid, bias=pv[:, 8 + h:9 + h], scale=1.0)
            kb.dve("tensor_tensor", out=vf[:, :, :], in0=vf[:, :, :], in1=vsa[:, 0:4, :], op=ALU.subtract)
            kb.dve("tensor_tensor", out=vf[:, :, :], in0=vf[:, :, :], in1=sgv[:, :, :], op=ALU.mult)
            kb.dve("tensor_tensor", out=vs[:, :, :], in0=vsa[:, 0:4, :], in1=vf[:, :, :], op=ALU.add)
        if not layer1:
            kb.dma(vout.rearrange("(h j) t -> j h t", h=4)[:, :, t0:t0 + TS], vs[:, :, :], q="pool")
        shift1(W0, zw, pr[:, 32:33], 64, t0, i)
        kb.act("activation", out=tzw[:, :], in_=d1[0:64, :], func=AF.Tanh)
        shift1(A0, za, pr[:, 33:34], 64, t0, i)
        kb.act("copy", out=zab[:, :], in_=d1[0:64, :])
        shift1(G0, zg, mug[:, 0:1], 128, t0, i)
        kb.act("activation", out=sgb[i][:, :], in_=d1[:, :], func=AF.Sigmoid)
        for h in range(4):
            kb.pe("matmul", sc[:, :], lhsT=w2b[:, h * 64:(h + 1) * 64], rhs=tzw[:, :], start=True, stop=True)
            kb.act("activation", out=sig[:, h, :], in_=sc[:, :], func=AF.Sigmoid, bias=w0[:, h:h + 1], scale=1.0)
        for h in range(4):
            kb.pe("matmul", sc[:, :], lhsT=a2b[:, h * 64:(h + 1) * 64], rhs=zab[:, :], start=True, stop=True)
            kb.act("activation", out=av[:, h, :], in_=sc[:, :], func=AF.Sigmoid, bias=a0[:, h:h + 1], scale=1.0)
        kb.dve("tensor_scalar", out=ell[:, :, :], in0=sig[:, :, :], scalar1=-0.6065306597126334, scalar2=None, op0=ALU.mult)
        kb.dve("tensor_tensor_scan", out=Gc[:, :, :].rearrange("p h t -> p (h t)"), data0=rmask,
               data1=ell[:, :, :].rearrange("p h t -> p (h t)"), initial=0.0, op0=ALU.mult, op1=ALU.add)
        kb.dve("tensor_tensor", out=kk[:, :, :], in0=ks[:, :, :], in1=k_k.bc(B3), op=ALU.mult)
        kb.dve("tensor_tensor", out=sq[:, :, :], in0=kk[:, :, :], in1=kk[:, :, :], op=ALU.mult)
        for h in range(4):
            kb.pe("matmul", sc[:, :], lhsT=ones, rhs=sq[:, h, :], start=True, stop=True)
            kb.act("activation", out=lnss[:, :], in_=sc[:, :], func=AF.Ln)
            kb.act("activation", out=rn[:, h, :], in_=lnss[:, :], func=AF.Exp, scale=-0.5)
        kb.dve("tensor_tensor", out=kap[:, :, :], in0=kk[:, :, :], in1=rn[:, :, :], op=ALU.mult)
        kb.dve("tensor_tensor", out=t1[:, :, :], in0=av[:, :, :], in1=k_a.bc(B3), op=ALU.mult)
        kb.dve("tensor_tensor", out=t1[:, :, :], in0=t1[:, :, :], in1=omka[:, :].bc(B3), op=ALU.add)
        kb.dve("tensor_tensor", out=kp[:, :, :], in0=ks[:, :, :], in1=t1[:, :, :], op=ALU.mult)
        kb.dve("tensor_tensor", out=bet[:, :, :], in0=kap[:, :, :], in1=av[:, :, :], op=ALU.mult)
        kb.pool("tensor_tensor", out=pd[:, :, :], in0=rs[:, :, :], in1=kp[:, :, :], op=ALU.mult)
        kb.pool("tensor_tensor", out=pd[:, :, :], in0=pd[:, :, :], in1=r_k.bc(B3), op=ALU.mult)
        fm = FM[i]
        kb.pool("tensor_copy", out=fm[:, 3, :, :], in_=f32v(pd))
        G3 = f32v(Gc)
        kr = KR[i]
        kb.act("activation", out=eG[:, :, :], in_=Gc[:, :, :], func=AF.Exp)
        kb.dve("tensor_tensor", out=kr[:, :, 1, :], in0=f32v(rs), in1=f32v(eG), op=ALU.mult)
        kb.act("activation", out=enG[:, :, :], in_=Gc[:, :, :], func=AF.Exp, scale=-1.0)
        kb.dve("tensor_tensor", out=KT[i][:, :, :], in0=f32v(kp), in1=f32v(enG), op=ALU.mult)
        kb.dve("tensor_tensor", out=BT[i][:, :, :], in0=f32v(bet), in1=f32v(enG), op=ALU.mult)
        kb.dve("tensor_tensor", out=t1[:, :, :], in0=Gc[:, :, :], in1=ell[:, :, :], op=ALU.subtract)
        kb.act("activation", out=eGm[:, :, :], in_=t1[:, :, :], func=AF.Exp)
        kb.dve("tensor_tensor", out=kr[:, :, 0, :], in0=f32v(kap), in1=f32v(eGm), op=ALU.mult)
        kb.dve("tensor_tensor", out=f32v(sq), in0=G3[:, :, 63:64].to_broadcast([64, HN, 64]), in1=G3, op=ALU.subtract)
        kb.act("activation", out=edG[:, :, :], in_=sq[:, :, :], func=AF.Exp)
        kb.act("activation", out=gC[i][:, :], in_=G3[:, :, 63:64].rearrange("p a c -> p (a c)"), func=AF.Exp)
        kb.pool("tensor_copy", out=fm[:, 0, :, :], in_=f32v(vs))
        kb.pool("tensor_tensor", out=fm[:, 1, :, :], in0=f32v(kp), in1=f32v(edG), op=ALU.mult)
        kb.dve("scalar_tensor_tensor", out=fm[:, 2, :, :], in0=f32v(bet), scalar=-1.0, in1=f32v(edG), op0=ALU.mult, op1=ALU.mult)

        for n in range(NCH):
            for blk in range(4):
                for h in range(4):
                    kb.pe("transpose", out=tp[:, blk * 4 + h, :], in_=fm[:, blk, h * NCH + n, :], identity=identb[:, :])
            kb.act("copy", out=TM[n][:, :, :], in_=tp[:, :, :])
            for h in range(4):
                hn = h * NCH + n
                krhs = kr[:, hn, :, :].rearrange("p a c -> p (a c)")
                kb.pe("matmul", pB[:, h, :], lhsT=BT[i][:, hn, :], rhs=krhs, start=True, stop=True)
                kb.pe("matmul", pK[:, h, :], lhsT=KT[i][:, hn, :], rhs=krhs, start=True, stop=True)
                kb.pe("matmul", pL[:, h, :], lhsT=kr[:, hn, 0, :], rhs=BT[i][:, hn, :], start=True, stop=True)
            kb.dve("tensor_tensor", out=Nf[n][0][:, :, :], in0=pB[:, :, 0:64], in1=Mb[:, :, 0:64], op=ALU.mult)
            kb.dve("tensor_tensor", out=NArb[n][:, :, :], in0=pB[:, :, 64:128], in1=Mb[:, :, 64:128], op=ALU.mult)
            kb.dve("tensor_tensor", out=AK[n][:, :, :], in0=pK[:, :, :], in1=Mk, op=ALU.mult)
            kb.dve("tensor_tensor", out=Lf[n][0][:, :, :], in0=pL[:, :, :], in1=Ml, op=ALU.mult)
            kb.pool("tensor_tensor", out=Qm[n][:, :, :], in0=I4, in1=Nf[n][0][:, :, :], op=ALU.subtract)
        for j in range(5):
            for n in range(NCH):
                Nc, Lc = Nf[n][j % 2], Lf[n][j % 2]
                Ln, Nn = Lf[n][(j + 1) % 2], Nf[n][(j + 1) % 2]
                for h in range(4):
                    kb.pe("matmul", psNLn[n][:, h, :], lhsT=Nc[:, h, :], rhs=Lc[:, h, :], start=True, stop=True)
                kb.act("copy", out=Ln[:, :, :], in_=psNLn[n][:, :, :])
                if j < 4:
                    for h in range(4):
                        kb.pe("matmul", psNLn[n][:, h, :], lhsT=Lc[:, h, :], rhs=Nc[:, h, :], start=True, stop=True)
                    kb.dve("tensor_copy", out=Nn[:, :, :], in_=psNLn[n][:, :, :])
            for n in range(NCH):
                Ln = Lf[n][(j + 1) % 2]
                for h in range(4):
                    kb.pe("matmul", psQn[n][:, h, :], lhsT=Ln[:, h, :], rhs=Qm[n][:, h, :], start=True, stop=True)
                kb.dve("tensor_tensor", out=Qm[n][:, :, :], in0=Qm[n][:, :, :], in1=psQn[n][:, :, :], op=ALU.add)
        for n in range(NCH):
            ci = n
            tm = TM[n]
            for h in range(4):
                hn = h * NCH + n
                kb.pe("matmul", pW[:, h, :], lhsT=kr[:, hn, 0, :], rhs=Sb[:, h, :], start=True, stop=False)
                kb.pe("matmul", pW[:, h, :], lhsT=AK[ci][:, h, 0:64], rhs=tm[:, h, :], start=False, stop=True)
            kb.act("copy", out=Wb[:, :, :], in_=pW[:, :, :])
            for h in range(4):
                kb.pe("matmul", pU[:, h, :], lhsT=TT[ci][:, h, :], rhs=Wb[:, h, :], start=True, stop=True)
            kb.dve("tensor_copy", out=Ub[:, :, :], in_=pU[:, :, :])
            for h in range(4):
                kb.pe("matmul", pdS[:, h, :], lhsT=tm[:, 4 + h, :], rhs=tm[:, h, :], start=True, stop=False)
                kb.pe("matmul", pdS[:, h, :], lhsT=tm[:, 8 + h, :], rhs=Ub[:, h, :], start=False, stop=True)
            for h in range(4):
                hn = h * NCH + n
                kb.pe("matmul", pO[:, h, :], lhsT=kr[:, hn, 1, :], rhs=Sb[:, h, :], start=True, stop=False)
                kb.pe("matmul", pO[:, h, :], lhsT=AK[ci][:, h, 64:128], rhs=tm[:, h, :], start=False, stop=False)
                kb.pe("matmul", pO[:, h, :], lhsT=NArb[ci][:, h, :], rhs=Ub[:, h, :], start=False, stop=True)
            gcv = gC[i][:, :].rearrange("p (h n) -> p h n", h=4)[:, :, n:n + 1].to_broadcast([64, 4, 64])
            kb.dve("tensor_tensor", out=S[:, :, :], in0=S[:, :, :], in1=gcv, op=ALU.mult)
            kb.dve("tensor_tensor", out=S[:, :, :], in0=S[:, :, :], in1=pdS[:, :, :], op=ALU.add)
            kb.act("copy", out=Sb[:, :, :], in_=S[:, :, :])
            kb.pe("matmul", psG[:, :], lhsT=sgb[i][:, n * 64:(n + 1) * 64], rhs=g2b[:, :], start=True, stop=True)
            kb.act("copy", out=osb[:, :, :], in_=pO[:, :, :])
            kb.pool("tensor_tensor", out=osq[:, :, :], in0=osb[:, :, :], in1=osb[:, :, :], op=ALU.mult)
            kb.dve("tensor_reduce", out=st[:, 0:4], in_=osb[:, :, :], axis=AX.X, op=ALU.add)
            kb.dve("tensor_reduce", out=st[:, 4:8], in_=osq[:, :, :], axis=AX.X, op=ALU.add)
            kb.dve("tensor_reduce", out=st[:, 20:24], in_=tm[:, 12:16, :], axis=AX.X, op=ALU.add)
            kb.dve("tensor_scalar", out=st[:, 0:4], in0=st[:, 0:4], scalar1=1.0 / 64, scalar2=None, op0=ALU.mult)
            kb.dve("tensor_tensor", out=st[:, 8:12], in0=st[:, 0:4], in1=st[:, 0:4], op=ALU.mult)
            kb.dve("scalar_tensor_tensor", out=st[:, 12:16], in0=st[:, 4:8], scalar=1.0 / 64, in1=st[:, 8:12], op0=ALU.mult, op1=ALU.subtract)
            kb.dve("tensor_scalar", out=st[:, 12:16], in0=st[:, 12:16], scalar1=GN_EPS, scalar2=None, op0=ALU.add)
            kb.act("activation", out=st[:, 16:20], in_=st[:, 12:16], func=AF.Sqrt)
            kb.dve("reciprocal", out=st[:, 16:20], in_=st[:, 16:20])
            S3 = [64, 4, 64]
            kb.dve("tensor_tensor", out=on[:, :, :], in0=osb[:, :, :], in1=st[:, 0:4].bc(S3), op=ALU.subtract)
            kb.dve("tensor_tensor", out=on[:, :, :], in0=on[:, :, :], in1=st[:, 16:20].bc(S3), op=ALU.mult)
            kb.pool("tensor_tensor", out=on[:, :, :], in0=on[:, :, :], in1=gng[:, :].rearrange("p (h c) -> p h c", h=4), op=ALU.mult)
            kb.pool("tensor_tensor", out=on[:, :, :], in0=on[:, :, :], in1=gnb[:, :].rearrange("p (h c) -> p h c", h=4), op=ALU.add)
            kb.pool("tensor_tensor", out=bon[:, :, :], in0=tm[:, 0:4, :], in1=st[:, 20:24].bc(S3), op=ALU.mult)
            kb.pool("tensor_tensor", out=on[:, :, :], in0=on[:, :, :], in1=bon[:, :, :], op=ALU.add)
            kb.dve("tensor_tensor", out=OUT[i][:, n, :], in0=on[:, :, :].rearrange("p h c -> p (h c)"), in1=psG[:, :], op=ALU.mult)
        for n in range(NCH):
            for j in range(2):
                kb.pe("transpose", out=tpo[:, j, :], in_=OUT[i][:, n, j * 128:(j + 1) * 128], identity=cm[:, 1280:1344])
            kb.act("copy", out=OT[i][:, :, n * 64:(n + 1) * 64], in_=tpo[:, :, :])
        kb.dma(orT[sl0:sl1, t0:t0 + TS].rearrange("(j p) t -> p j t", p=128), OT[i][:, :, :], q="pool")


def stage_k3a(kb, T, l, xTd, onTd, orTd, ZT, D, x2Td):
    nc, es = kb.nc, kb.es
    stage = 9
    NT = T
    W = 512
    ntile = NT // W
    zgTd = ZT[3096:5144, :]
    pnd, prd, wod, xqd, xkd, xvd, xod = D["p_nsa"][l], D["p_rwkv"][l], D["w_out"][l], D["xq"][l], D["xk"][l], D["xv"][l], D["xo"][l]
    memTd, lnd = D["memT"], D["ln12"][l]
    stg = Stager(kb, 2048, 2)
    pn = kb.sb([128, 4, 1024], BF16, "pn"); prw = kb.sb([128, 4, 1024], BF16, "prw")
    wo = kb.sb([128, 8, 1024], BF16, "wo"); xq = kb.sb([128, 8, 512], BF16, "xq_")
    xk = kb.sb([128, 8, 512], BF16, "xk_"); xv = kb.sb([128, 8, 512], BF16, "xv_")
    xo = kb.sb([128, 4, 1024], BF16, "xo_"); memb = kb.sb([128, 8, 256], BF16, "memb")
    for (dst, srcd, nk, ncol) in ((pn, pnd, 4, 1024), (prw, prd, 4, 1024), (wo, wod, 8, 1024), (xq, xqd, 8, 512), (xk, xkd, 8, 512),
                                  (xv, xvd, 8, 512), (xo, xod, 4, 1024), (memb, memTd, 8, 256)):
        src = srcd.rearrange("(k p) c -> p k c", p=128)
        kstep = 2048 // ncol
        for k0 in range(0, nk, kstep):
            kw = min(kstep, nk - k0)
            stg.load_cast(dst[:, k0:k0 + kw, :], src[:, k0:k0 + kw, :])
    ln = kb.sb([128, 32], F32, "ln_"); kb.dma(ln[:, :], lnd[:, :])
    onesf = kb.sb([128, 128], F32, "onesf"); kb.dve("memset", onesf[:, :], 1.0 / 1024)
    onesb = kb.sb([128, 128], BF16, "onesb"); kb.dve("memset", onesb[:, :], 1.0)

    P = [kb.ps([128, 512], F32, f"bank{i}") for i in range(8)]
    pA, pB_, pY, pM, pV, pQ, pO, pD = P
    KT = kb.sb([128, 4, 256], BF16, "KT"); Vm = kb.sb([128, 2, 512], BF16, "Vm")
    for h in range(4):
        for k in range(8):
            kb.pe("matmul", pQ[:, 0:256], lhsT=xk[:, k, h * 128:(h + 1) * 128], rhs=memb[:, k, :], start=(k == 0), stop=(k == 7))
        kb.act("copy", out=KT[:, h, :], in_=pQ[:, 0:256])
    for mc in range(2):
        for k in range(8):
            kb.pe("matmul", pO[:, :], lhsT=memb[:, k, mc * 128:(mc + 1) * 128], rhs=xv[:, k, :], start=(k == 0), stop=(k == 7))
        kb.act("copy", out=Vm[:, mc, :], in_=pO[:, :])

    xt = kb.sb([128, 8, W], F32, "xt"); vv = kb.sb([128, 8, W], F32, "vv")
    onb = kb.sb([128, 4, W], BF16, "onb"); orb = kb.sb([128, 4, W], BF16, "orb")
    zgn = [kb.sb([128, W], F32, f"zgn{i}") for i in range(2)]; zgr = [kb.sb([128, W], F32, f"zgr{i}") for i in range(2)]
    m1 = kb.sb([128, W], F32, "m1"); m2 = kb.sb([128, W], F32, "m2")
    mT = kb.sb([128, 8, W], BF16, "mT"); x1b = kb.sb([128, 8, W], BF16, "x1b")
    qTb = kb.sb([128, 4, W], BF16, "qTb"); PTm = kb.sb([128, 2, W], BF16, "PTm")
    rden = kb.sb([128, W], F32, "rden"); oTn = kb.sb([128, 4, W], BF16, "oTn")
    tmp = {"sq": [kb.sb([128, W], F32, f"sq{i}") for i in range(2)], "mean": kb.sb([128, W], F32, "mean"),
           "msq": kb.sb([128, W], F32, "msq"), "rstd": kb.sb([128, W], F32, "rstd")}

    for tt in range(ntile if stage >= 1 else 0):
        c0, c1 = tt * W, (tt + 1) * W
        for k in range(8):
            kb.dma(xt[:, k, :], xTd[k * 128:(k + 1) * 128, c0:c1])
        stg.load_cast(onb[:, :, :], onTd.rearrange("(k p) t -> p k t", p=128)[:, :, c0:c1])
        stg.load_cast(orb[:, :, :], orTd.rearrange("(k p) t -> p k t", p=128)[:, :, c0:c1])
        for dc in range(8):
            gn, gr = zgn[dc % 2], zgr[dc % 2]
            kb.dma(gn[:, :], zgTd[dc * 128:(dc + 1) * 128, c0:c1])
            kb.dma(gr[:, :], zgTd[1024 + dc * 128:1024 + (dc + 1) * 128, c0:c1])
            kb.act("activation", out=gn[:, :], in_=gn[:, :], func=AF.Sigmoid)
            kb.act("activation", out=gr[:, :], in_=gr[:, :], func=AF.Sigmoid)
            for kc in range(4):
                kb.pe("matmul", pA[:, :], lhsT=pn[:, kc, dc * 128:(dc + 1) * 128], rhs=onb[:, kc, :], start=(kc == 0), stop=(kc == 3))
            for kc in range(4):
                kb.pe("matmul", pB_[:, :], lhsT=prw[:, kc, dc * 128:(dc + 1) * 128], rhs=orb[:, kc, :], start=(kc == 0), stop=(kc == 3))
            kb.dve("tensor_tensor", out=m1[:, :], in0=pA[:, :], in1=gn[:, :], op=ALU.mult)
            kb.dve("tensor_tensor", out=m2[:, :], in0=pB_[:, :], in1=gr[:, :], op=ALU.mult)
            kb.pool("tensor_tensor", out=mT[:, dc, :], in0=m1[:, :], in1=m2[:, :], op=ALU.add)
        for dc in range(8):
            for k in range(8):
                kb.pe("matmul", pY[:, :], lhsT=wo[:, k, dc * 128:(dc + 1) * 128], rhs=mT[:, k, :], start=(k == 0), stop=(k == 7))
            kb.dve("scalar_tensor_tensor", out=vv[:, dc, :], in0=xt[:, dc, :], scalar=DN_ALPHA, in1=pY[:, :], op0=ALU.mult, op1=ALU.add)
        if stage < 2: continue
        layer_norm_fm(kb, vv, ln[:, 0:16], None, W, pM, pV, tmp, onesf, x1b=x1b)
        if stage < 3: continue
        for h in range(4):
            for k in range(8):
                kb.pe("matmul", pQ[:, :], lhsT=xq[:, k, h * 128:(h + 1) * 128], rhs=x1b[:, k, :], start=(k == 0), stop=(k == 7))
            kb.act("mul", out=qTb[:, h, :], in_=pQ[:, :], mul=128 ** -0.5)
        if stage < 4: continue
        for h in range(4):
            for mc in range(2 if stage >= 4 else 0):
                kb.pe("matmul", pA[:, :] if mc == 0 else pB_[:, :], lhsT=KT[:, h, mc * 128:(mc + 1) * 128], rhs=qTb[:, h, :], start=True, stop=True)
                kb.act("activation", out=PTm[:, mc, :], in_=(pA if mc == 0 else pB_)[:, :], func=AF.Exp)
            if stage < 5: continue
            for mc in range(2):
                kb.pe("matmul", pO[:, :], lhsT=Vm[:, mc, h * 128:(h + 1) * 128], rhs=PTm[:, mc, :], start=(mc == 0), stop=(mc == 1))
            for mc in range(2):
                kb.pe("matmul", pD[:, :], lhsT=onesb[:, :], rhs=PTm[:, mc, :], start=(mc == 0), stop=(mc == 1))
            kb.dve("reciprocal", out=rden[:, :], in_=pD[:, :])
            kb.dve("tensor_tensor", out=oTn[:, h, :], in0=pO[:, :], in1=rden[:, :], op=ALU.mult)
        if stage < 6: continue
        for dc in range(8):
            for h in range(4):
                kb.pe("matmul", pY[:, :], lhsT=xo[:, h, dc * 128:(dc + 1) * 128], rhs=oTn[:, h, :], start=(h == 0), stop=(h == 3))
            kb.dve("scalar_tensor_tensor", out=vv[:, dc, :], in0=vv[:, dc, :], scalar=DN_ALPHA, in1=pY[:, :], op0=ALU.mult, op1=ALU.add)
        layer_norm_fm(kb, vv, ln[:, 16:32], (c0, c1), W, pM, pV, tmp, onesf, out=x2Td)


def stage_k3b(kb, T, l, x2Td, D, x3Td):
    nc, es = kb.nc, kb.es
    NT = T
    HT = min(2048, NT)
    nhalf = NT // HT
    W = 512
    WL = 256
    rwd, rbd, wgd, wud, wdd, lnd, ohd, idd = D["rw"], D["rb"], D["wg"][l], D["wu"][l], D["wd"][l], D["ln3"][l], D["oh16"], D["identf"]
    stg = Stager(kb, 1024, 2)
    rw = kb.sb([128, 8, 16], F32, "rw_"); kb.dma(rw[:, :, :], rwd.rearrange("(k p) e -> p k e", p=128))
    rb = kb.sb([128, 16], F32, "rb_"); kb.dma(rb[:, :], rbd[0:1, :].partition_broadcast(128))
    ln = kb.sb([128, 16], F32, "ln_"); kb.dma(ln[:, :], lnd[:, :])
    oh16 = kb.sb([16, 16, 128], F32, "oh16_"); kb.dma(oh16[:, :, :], ohd.rearrange("r (e c) -> r e c", c=128))
    identf = kb.sb([128, 128], F32, "identf_"); kb.dma(identf[:, :], idd[:, :])
    onesf = kb.sb([128, 128], F32, "onesf"); kb.dve("memset", onesf[:, :], 1.0 / 1024)

    P = [kb.ps([128, 512], F32, f"bank{i}") for i in range(8)]
    pR, pGb, pHg0, pHu0, pHg1, pHu1, pY0, pY1 = P

    x2b = kb.sb([128, 8, HT], BF16, "x2b"); yacc = kb.sb([128, 8, HT], F32, "yacc")
    gateT = kb.sb([16, HT], F32, "gateT")
    wgb = [kb.sb([128, 8, 512], BF16, f"wgb{i}") for i in range(2)]
    wub = [kb.sb([128, 8, 512], BF16, f"wub{i}") for i in range(2)]
    wdb = [kb.sb([128, 4, 1024], BF16, f"wdb{i}") for i in range(2)]
    hT = kb.sb([128, 4, W], BF16, "hT")
    sg = [kb.sb([128, W], F32, f"sg{i}") for i in range(2)]; hu = [kb.sb([128, W], F32, f"hu{i}") for i in range(2)]
    gbs = kb.sb([128, W], F32, "gbs")
    xr = [kb.sb([128, 8, 128], F32, f"xr{i}") for i in range(2)]
    r = {n: kb.sb([128, 16], F32, "r_" + n) for n in ("s", "ssel", "eq", "s2", "msel", "oh1", "oh2", "st", "gate", "t16")}
    q = {n: kb.sb([128, 4], F32, "q_" + n) for n in ("m1", "m2", "gsc", "ing", "t")}
    c = {n: kb.sb([128, 1], F32, "c_" + n) for n in ("gmax", "e1", "e2", "ssum")}
    xt = kb.sb([128, 8, WL], F32, "xt")
    tmp = {"sq": [kb.sb([128, WL], F32, f"sq{i}") for i in range(2)], "mean": kb.sb([128, WL], F32, "mean"),
           "msq": kb.sb([128, WL], F32, "msq"), "rstd": kb.sb([128, WL], F32, "rstd")}
    G44 = [128, 4, 4]
    v44 = lambda b: b[:, :].rearrange("p (g e) -> p g e", e=4)

    for hf in range(nhalf):
        h0 = hf * HT
        for k in range(8):
            cs_ = min(1024, HT)
            for c0 in range(0, HT, cs_):
                stg.load_cast(x2b[:, k, c0:c0 + cs_], x2Td[k * 128:(k + 1) * 128, h0 + c0:h0 + c0 + cs_])
        for rt in range(HT // 128):
            xrt = xr[rt % 2]
            kb.dma(xrt[:, :, :], x2Td.rearrange("(k p) t -> p k t", p=128)[:, :, h0 + rt * 128:h0 + (rt + 1) * 128])
            for k in range(8):
                kb.pe("matmul", pR[:, 0:16], lhsT=xrt[:, k, :], rhs=rw[:, k, :], start=(k == 0), stop=(k == 7))
            kb.act("activation", out=r["s"][:, :], in_=pR[:, 0:16], func=AF.Sigmoid)
            kb.dve("tensor_tensor", out=r["ssel"][:, :], in0=r["s"][:, :], in1=rb[:, :], op=ALU.add)
            kb.dve("tensor_reduce", out=q["m1"][:, :], in_=v44(r["ssel"]), axis=AX.X, op=ALU.max)
            kb.dve("tensor_tensor", out=v44(r["eq"]), in0=v44(r["ssel"]), in1=q["m1"][:, :].bc(G44), op=ALU.is_equal)
            kb.dve("scalar_tensor_tensor", out=r["s2"][:, :], in0=r["eq"][:, :], scalar=-1e9, in1=r["ssel"][:, :], op0=ALU.mult, op1=ALU.add)
            kb.dve("tensor_reduce", out=q["m2"][:, :], in_=v44(r["s2"]), axis=AX.X, op=ALU.max)
            kb.dve("tensor_tensor", out=q["gsc"][:, :], in0=q["m1"][:, :], in1=q["m2"][:, :], op=ALU.add)
            kb.dve("tensor_reduce", out=c["gmax"][:, :], in_=q["gsc"][:, :], axis=AX.X, op=ALU.max)
            kb.dve("tensor_scalar", out=q["ing"][:, :], in0=q["gsc"][:, :], scalar1=c["gmax"][:, 0:1], scalar2=None, op0=ALU.is_equal)
            kb.dve("tensor_scalar", out=q["t"][:, :], in0=q["ing"][:, :], scalar1=1e9, scalar2=-1e9, op0=ALU.mult, op1=ALU.add)
            kb.dve("tensor_tensor", out=v44(r["msel"]), in0=v44(r["ssel"]), in1=q["ing"][:, :].bc(G44), op=ALU.mult)
            kb.dve("tensor_tensor", out=v44(r["msel"]), in0=v44(r["msel"]), in1=q["t"][:, :].bc(G44), op=ALU.add)
            kb.dve("tensor_reduce", out=c["e1"][:, :], in_=r["msel"][:, :], axis=AX.X, op=ALU.max)
            kb.dve("tensor_scalar", out=r["oh1"][:, :], in0=r["msel"][:, :], scalar1=c["e1"][:, 0:1], scalar2=None, op0=ALU.is_equal)
            kb.dve("scalar_tensor_tensor", out=r["s2"][:, :], in0=r["oh1"][:, :], scalar=-2e9, in1=r["msel"][:, :], op0=ALU.mult, op1=ALU.add)
            kb.dve("tensor_reduce", out=c["e2"][:, :], in_=r["s2"][:, :], axis=AX.X, op=ALU.max)
            kb.dve("tensor_scalar", out=r["oh2"][:, :], in0=r["s2"][:, :], scalar1=c["e2"][:, 0:1], scalar2=None, op0=ALU.is_equal)
            kb.dve("tensor_tensor", out=r["oh1"][:, :], in0=r["oh1"][:, :], in1=r["oh2"][:, :], op=ALU.add)
            kb.dve("tensor_tensor", out=r["st"][:, :], in0=r["s"][:, :], in1=r["oh1"][:, :], op=ALU.mult)
            kb.dve("tensor_reduce", out=c["ssum"][:, :], in_=r["st"][:, :], axis=AX.X, op=ALU.add)
            kb.dve("reciprocal", out=c["ssum"][:, :], in_=c["ssum"][:, :])
            kb.dve("tensor_scalar", out=r["gate"][:, :], in0=r["st"][:, :], scalar1=c["ssum"][:, 0:1], scalar2=None, op0=ALU.mult)
            kb.pe("transpose", out=pGb[0:16, 0:128], in_=r["gate"][:, :], identity=identf[:, :])
            kb.act("copy", out=gateT[:, rt * 128:(rt + 1) * 128], in_=pGb[0:16, 0:128])
        it = 0
        def load_expert(e_):
            wi_ = e_ % 2
            for k0 in range(0, 8, 2):
                stg.load_cast(wgb[wi_][:, k0:k0 + 2, :], wgd[e_].rearrange("(k p) f -> p k f", p=128)[:, k0:k0 + 2, :])
                stg.load_cast(wub[wi_][:, k0:k0 + 2, :], wud[e_].rearrange("(k p) f -> p k f", p=128)[:, k0:k0 + 2, :])
            for k0 in range(4):
                stg.load_cast(wdb[wi_][:, k0:k0 + 1, :], wdd[e_].rearrange("(k p) d -> p k d", p=128)[:, k0:k0 + 1, :])

        load_expert(0)
        for e in range(16):
            wi = e % 2
            if e + 1 < 16:
                load_expert(e + 1)
            for tt in range(HT // W):
                c0, c1 = tt * W, (tt + 1) * W
                kb.pe("matmul", pGb[:, :], lhsT=oh16[:, e, :], rhs=gateT[:, c0:c1], start=True, stop=True)
                kb.act("copy", out=gbs[:, :], in_=pGb[:, :])
                for fc in range(4):
                    pHg, pHu = (pHg0, pHu0) if it % 2 == 0 else (pHg1, pHu1)
                    sgi, hui = sg[it % 2], hu[it % 2]
                    it += 1
                    for k in range(8):
                        kb.pe("matmul", pHg[:, :], lhsT=wgb[wi][:, k, fc * 128:(fc + 1) * 128], rhs=x2b[:, k, c0:c1], start=(k == 0), stop=(k == 7))
                    for k in range(8):
                        kb.pe("matmul", pHu[:, :], lhsT=wub[wi][:, k, fc * 128:(fc + 1) * 128], rhs=x2b[:, k, c0:c1], start=(k == 0), stop=(k == 7))
                    kb.act("activation", out=sgi[:, :], in_=pHg[:, :], func=AF.Silu)
                    kb.dve("tensor_tensor", out=hui[:, :], in0=pHu[:, :], in1=sgi[:, :], op=ALU.mult)
                    kb.pool("tensor_tensor", out=hT[:, fc, :], in0=hui[:, :], in1=gbs[:, :], op=ALU.mult)
                for dc in range(8):
                    pY = pY0 if dc % 2 == 0 else pY1
                    for fc in range(4):
                        kb.pe("matmul", pY[:, :], lhsT=wdb[wi][:, fc, dc * 128:(dc + 1) * 128], rhs=hT[:, fc, :], start=(fc == 0), stop=(fc == 3))
                    if e == 0:
                        kb.dve("tensor_copy", out=yacc[:, dc, c0:c1], in_=pY[:, :])
                    else:
                        kb.dve("tensor_tensor", out=yacc[:, dc, c0:c1], in0=yacc[:, dc, c0:c1], in1=pY[:, :], op=ALU.add)
        for tt in range(HT // WL):
            c0, c1 = tt * WL, (tt + 1) * WL
            for k in range(8):
                kb.dma(xt[:, k, :], x2Td[k * 128:(k + 1) * 128, h0 + c0:h0 + c1])
            for dc in range(8):
                kb.dve("scalar_tensor_tensor", out=xt[:, dc, :], in0=xt[:, dc, :], scalar=DN_ALPHA, in1=yacc[:, dc, c0:c1], op0=ALU.mult, op1=ALU.add)
            layer_norm_fm(kb, xt, ln[:, 0:16], (h0 + c0, h0 + c1), WL, pHg0, pHu0, tmp, onesf, out=x3Td)


WSPEC = None


def fused_specs(T):
    NJ, NKC = T // 64, T // 128
    NCc = (T // 16 + 127) // 128
    return {
        "xT0": [1024, T], "memT": [1024, 256], "w_in": [2, 1024, 5144], "w_v": [2, 1024, 280],
        "w1k": [2, 2048, 256], "w1v": [2, 2048, 256], "w2k": [2, 256, 64], "w2v": [2, 256, 64], "pek": [2, 2048], "pev": [2, 2048],
        "strips": [2, 4, 128, NCLS * 128], "b31": [2, 128, 4], "ov": [NCc, 128, NJ], "vmva": [128, 4 * NJ], "esel": [NJ, NKC * 128], "ident": [128, 128],
        "pr": [2, 2, 64, 34], "mug": [2, 128, 1], "w2": [2, 64, 512], "a2": [2, 64, 512], "g2": [2, 128, 512], "gn": [2, 2, 512], "cm": [64, NCM],
        "v1": [512, 32], "v2": [32, 512], "pv": [2, 64, 12],
        "p_nsa": [2, 512, 1024], "p_rwkv": [2, 512, 1024], "w_out": [2, 1024, 1024], "xq": [2, 1024, 512], "xk": [2, 1024, 512], "xv": [2, 1024, 512],
        "xo": [2, 512, 1024], "ln12": [2, 128, 32],
        "rw": [1024, 16], "rb": [1, 16], "wg": [2, 16, 1024, 512], "wu": [2, 16, 1024, 512], "wd": [2, 16, 512, 1024], "ln3": [2, 128, 16],
        "oh16": [16, 16 * 128], "identf": [128, 128],
    }


def build_fused(T, nlayers=2, upto=99):
    nc = bass.Bass("TRN2", target_bir_lowering=False)
    es0 = ExitStack()
    with es0:
        kb = KB(nc, es0)
        kb.pfx = ""
        D = {k: kb.dram(k, shp, F32, kind="ExternalInput") for k, shp in fused_specs(T).items()}
        outT = kb.dram("outT", [1024, T], F32, kind="ExternalOutput")
        ZT = kb.dram("ZT", [41 * 128, T], F32)
        ZV = kb.dram("ZV", [T, 280], F32)
        onT = kb.dram("onT", [512, T], F32)
        orT = kb.dram("orT", [512, T], F32)
        vfT = kb.dram("vfT", [512, T], F32)
        x2T = kb.dram("x2T", [1024, T], F32)
        xT1 = kb.dram("xT1", [1024, T], F32)
        sid = [0]

        def run_stage(fn, *a):
            sid[0] += 1
            if sid[0] > upto:
                return
            with ExitStack() as es2:
                kb.es = es2
                kb.pfx = f"s{sid[0]}_"
                kb.barrier()
                fn(kb, T, *a)
                kb.barrier()
                kb.flush()

        for l in range(nlayers):
            xsrc = D["xT0"] if l == 0 else xT1
            xdst = outT if l == nlayers - 1 else xT1
            run_stage(stage_k1, l, xsrc, D, ZT, ZV)
            for g in range(2):
                run_stage(stage_k2a, g, l, ZT, ZV, D, onT)
            for g in range(2):
                run_stage(stage_k2b, g, l, ZT, D, orT, vfT)
            run_stage(stage_k3a, l, xsrc, onT, orT, ZT, D, x2T)
            run_stage(stage_k3b, l, x2T, D, xdst)
    return nc


def _c(a):
    return np.ascontiguousarray(a, dtype=np.float32)


def _lnpack(g, b):
    return np.concatenate([g.reshape(8, 128).T, b.reshape(8, 128).T], 1)


def fused_inputs(inp, b, T):
    L = 2
    d = {"xT0": _c(inp["x"][b, :T].T), "memT": _c(inp["mem"][b].T), "w_in": _c(inp["w_in"])}
    cols = []
    for g in range(2):
        cols += list(range(896 + g * 64, 896 + (g + 1) * 64)) + list(range(1152 + g * 64, 1152 + (g + 1) * 64)) + list(range(1280 + g * 12, 1280 + (g + 1) * 12))
    d["w_v"] = _c(inp["w_in"][:, :, cols])
    d.update({"w1k": _c(inp["cmp_w1k"]), "w1v": _c(inp["cmp_w1v"]), "w2k": _c(inp["cmp_w2k"]), "w2v": _c(inp["cmp_w2v"]),
              "pek": _c(inp["cmp_pe_k"].reshape(L, -1)), "pev": _c(inp["cmp_pe_v"].reshape(L, -1))})
    n0, n1 = nsa_consts(T, inp["rel_table"], 0), nsa_consts(T, inp["rel_table"], 1)
    d["strips"] = _c(np.stack([n0["strips"], n1["strips"]]))
    tb = inp["rel_table"].reshape(32, 2, 4)[31]
    d["b31"] = _c(np.broadcast_to(tb[:, None, :], (2, 128, 4)))
    for k in ("ov", "vmva", "esel", "ident"):
        d[k] = n0[k]
    pr = np.zeros((L, 2, 64, 34), np.float32)
    for l in range(L):
        mu = inp["rwkv_mu"][l]
        plist = [mu[0:512], mu[512:1024], mu[1024:1536], inp["rwkv_w0"][l], inp["rwkv_a0"][l], inp["rwkv_kk"][l], inp["rwkv_ka"][l], inp["rwkv_rk"][l].reshape(-1)]
        for g in range(2):
            for p_, v in enumerate(plist):
                pr[l, g, :, 4 * p_:4 * p_ + 4] = v[g * 256:(g + 1) * 256].reshape(4, 64).T
            pr[l, g, :, 32] = mu[1536:1600]
            pr[l, g, :, 33] = mu[1600:1664]
    d["pr"] = pr
    d["mug"] = _c(inp["rwkv_mu"][:, 1664:1792].reshape(L, 128, 1))
    d["w2"], d["a2"], d["g2"] = _c(inp["rwkv_w2"]), _c(inp["rwkv_a2"]), _c(inp["rwkv_g2"])
    d["gn"] = _c(np.stack([inp["rwkv_gn_g"], inp["rwkv_gn_b"]], axis=1))
    d["cm"] = rwkv_consts()
    d["v1"], d["v2"] = _c(inp["rwkv_v1"][0]), _c(inp["rwkv_v2"][0])
    pv = np.zeros((2, 64, 12), np.float32)
    muv = inp["rwkv_mu"][1][1024:1536]
    for g in range(2):
        perm = np.concatenate([np.arange(g * 256, (g + 1) * 256), np.arange((1 - g) * 256, (2 - g) * 256)])
        pv[g, :, 0:8] = muv[perm].reshape(8, 64).T
        pv[g, :, 8:12] = inp["rwkv_v0"][0][g * 256:(g + 1) * 256].reshape(4, 64).T
    d["pv"] = pv
    d.update({"p_nsa": _c(inp["p_nsa"]), "p_rwkv": _c(inp["p_rwkv"]), "w_out": _c(inp["w_out"]), "xq": _c(inp["xq_w"]), "xk": _c(inp["xk_w"]),
              "xv": _c(inp["xv_w"]), "xo": _c(inp["xo_w"])})
    d["ln12"] = _c(np.stack([np.concatenate([_lnpack(inp["ln1_g"][l], inp["ln1_b"][l]), _lnpack(inp["ln2_g"][l], inp["ln2_b"][l])], 1) for l in range(L)]))
    d.update({"rw": _c(inp["router_w"]), "rb": _c(inp["router_bias"].reshape(1, 16)), "wg": _c(inp["moe_w_gate"]), "wu": _c(inp["moe_w_up"]),
              "wd": _c(inp["moe_w_down"])})
    d["ln3"] = _c(np.stack([_lnpack(inp["ln3_g"][l], inp["ln3_b"][l]) for l in range(L)]))
    d.update(moe_consts())
    return d


_PROG = {}


def kernel(**inp):
    inp = {k: np.asarray(v) for k, v in inp.items()}
    B, T, Dm = inp["x"].shape
    if T not in _PROG:
        _PROG[T] = build_fused(T)
    per_b = [fused_inputs(inp, b, T) for b in range(B)]
    maps = [per_b[c // 2] for c in range(8)]
    res = run_bass_kernel_spmd(_PROG[T], maps, core_ids=list(range(8))).results
    out = np.zeros((B, T, Dm), np.float32)
    half = T // 2
    for c in range(8):
        b, h = c // 2, c % 2
        out[b, h * half:(h + 1) * half] = res[c]["outT"][:, h * half:(h + 1) * half].T
    return out
```
